# Optimizing a Trainium2 kernel written in Bass

```python
import jax, jax.numpy as jnp
from jax import lax
import numpy as np

D_MODEL = 2048
BATCH = 8
SEQ = 2048
DEPTH = 1

GRID_W = 64
CTX_LEN = 256
D_MIX = D_MODEL
HG_W = D_MIX // 2
HG_DK = 128
HG_H = HG_W // HG_DK
HG_DV = HG_W // HG_H
GD_W = D_MIX - HG_W
GD_DK = 128
GD_H = GD_W // GD_DK
GD_DV = GD_W // GD_H
CONV_W = 3
CHUNK = 64
N_EXPERTS = 32
TOP_K = 4
D_FF = D_MODEL
SWIGLU_LIMIT = 7.0
SWIGLU_ALPHA = 1.702
MOE_BLOCK = 128
EPS = 1e-6
IN_SIZES = (HG_W, HG_W, HG_W, HG_W, HG_W, GD_W, GD_W, GD_W, GD_W, GD_H, GD_H, GD_H, GD_H)
IN_DIM = sum(IN_SIZES)

kernel_name = "hybrid_hgrn2_gdn_moe_flow_block"


def rmsnorm(x, w):
    xf = x.astype(jnp.float32)
    y = xf * lax.rsqrt(jnp.mean(xf * xf, axis=-1, keepdims=True) + EPS)
    return (y * w.astype(jnp.float32)).astype(x.dtype)


def l2norm(x):
    return x * lax.rsqrt(jnp.sum(x * x, axis=-1, keepdims=True) + EPS)


def split_last(t, sizes):
    return jnp.split(t, np.cumsum(sizes)[:-1].tolist(), axis=-1)


def to_heads(t, h):
    b, s, w = t.shape
    return t.reshape(b, s, h, w // h).transpose(0, 2, 1, 3)


def short_conv(x, w):
    pad = CONV_W // 2
    n = x.shape[1]
    xp = jnp.pad(x, ((0, 0), (pad, pad), (0, 0)))
    return sum(xp[:, j:j + n, :] * w[j] for j in range(CONV_W))


def gla_chunked(q, k, v, logf, s0, with_out):
    b, h, t, dk = k.shape
    dv = v.shape[-1]
    n = t // CHUNK
    blk = lambda a: a.reshape(b, h, n, CHUNK, a.shape[-1])
    k, v, logf = blk(k), blk(v), blk(logf)
    cum = jnp.cumsum(logf, axis=3)
    cum_last = cum[:, :, :, -1:, :]
    u = jnp.einsum('bhncd,bhnce->bhnde', k * jnp.exp(cum_last - cum), v)
    decay = jnp.exp(cum_last[:, :, :, 0, :])

    def step(s, inp):
        dec, un = inp
        return dec[..., None] * s + un, s

    s_fin, s_start = lax.scan(step, s0, (jnp.moveaxis(decay, 2, 0), jnp.moveaxis(u, 2, 0)))
    if not with_out:
        return None, s_fin
    q = blk(q)
    s_start = jnp.moveaxis(s_start, 0, 2)
    ref = cum[:, :, :, CHUNK // 2:CHUNK // 2 + 1, :]
    scores = jnp.einsum('bhncd,bhnsd->bhncs', q * jnp.exp(cum - ref), k * jnp.exp(ref - cum))
    lower = np.tril(np.ones((CHUNK, CHUNK), bool))
    scores = jnp.where(lower, scores, 0.0)
    o = (jnp.einsum('bhncs,bhnse->bhnce', scores, v)
         + jnp.einsum('bhncd,bhnde->bhnce', q * jnp.exp(cum), s_start))
    return o.reshape(b, h, t, dv), s_fin


def gdn_chunked(q, k, v, g, beta, s0, with_out):
    b, h, t, dk = k.shape
    dv = v.shape[-1]
    n = t // CHUNK
    k = k.reshape(b, h, n, CHUNK, dk)
    v = v.reshape(b, h, n, CHUNK, dv)
    g = g.reshape(b, h, n, CHUNK)
    beta = beta.reshape(b, h, n, CHUNK)
    cum = jnp.cumsum(g, axis=-1)
    lower = np.tril(np.ones((CHUNK, CHUNK), bool))
    strict = np.tril(np.ones((CHUNK, CHUNK), bool), -1)
    dmask = jnp.exp(jnp.where(lower, cum[..., :, None] - cum[..., None, :], -jnp.inf))
    kb = k * beta[..., None]
    tri = jnp.where(strict, jnp.einsum('bhnid,bhnjd->bhnij', kb, k) * dmask, 0.0) + np.eye(CHUNK, dtype=np.float32)
    u = lax.linalg.triangular_solve(tri, v * beta[..., None], left_side=True, lower=True, unit_diagonal=True)
    w = lax.linalg.triangular_solve(tri, kb * jnp.exp(cum)[..., None], left_side=True, lower=True, unit_diagonal=True)
    cum_last = cum[..., -1]
    k_end = k * jnp.exp(cum_last[..., None] - cum)[..., None]
    mv = lambda a: jnp.moveaxis(a, 2, 0)

    def advance(s, w_n, u_n, ke_n, gl_n):
        v_new = u_n - jnp.einsum('bhcd,bhde->bhce', w_n, s)
        s_new = s * jnp.exp(gl_n)[..., None, None] + jnp.einsum('bhcd,bhce->bhde', ke_n, v_new)
        return v_new, s_new

    if not with_out:
        def step_state(s, inp):
            _, s_new = advance(s, *inp)
            return s_new, None
        s_fin, _ = lax.scan(step_state, s0, tuple(mv(a) for a in (w, u, k_end, cum_last)))
        return None, s_fin
    q = q.reshape(b, h, n, CHUNK, dk)
    attn = jnp.einsum('bhnid,bhnjd->bhnij', q, k) * dmask
    q_dec = q * jnp.exp(cum)[..., None]

    def step(s, inp):
        w_n, u_n, ke_n, gl_n, qd_n, at_n = inp
        v_new, s_new = advance(s, w_n, u_n, ke_n, gl_n)
        o_n = jnp.einsum('bhcd,bhde->bhce', qd_n, s) + jnp.einsum('bhcs,bhse->bhce', at_n, v_new)
        return s_new, o_n

    s_fin, o = lax.scan(step, s0, tuple(mv(a) for a in (w, u, k_end, cum_last, q_dec, attn)))
    return jnp.moveaxis(o, 0, 2).reshape(b, h, t, dv), s_fin


def hgrn2_direction(q, zf, v, lb, s0, reverse, with_out):
    if reverse:
        zf, v = jnp.flip(zf, 1), jnp.flip(v, 1)
        q = None if q is None else jnp.flip(q, 1)
    logf = jnp.log(lb + (1.0 - lb) * jax.nn.sigmoid(zf))
    k = (1.0 - lb) * jax.nn.sigmoid(-zf)
    qh = to_heads(q, HG_H) if with_out else None
    o, s = gla_chunked(qh, to_heads(k, HG_H), to_heads(v, HG_H), to_heads(logf, HG_H), s0, with_out)
    if with_out and reverse:
        o = jnp.flip(o, 2)
    return o, s


def gdn_inputs(q, k, v, conv_w, grid):
    wq, wk, wv = split_last(conv_w, (GD_W, GD_W, GD_W))

    def prep(a, w):
        bsz, t, ch = a.shape
        if grid:
            rows = t // GRID_W
            a = short_conv(a.reshape(bsz * rows, GRID_W, ch), w).reshape(bsz, t, ch)
        else:
            a = short_conv(a, w)
        return to_heads(jax.nn.silu(a), GD_H)

    qh = None if q is None else l2norm(prep(q, wq)) * GD_DK ** -0.5
    return qh, l2norm(prep(k, wk)), prep(v, wv)


def gdn_direction(q, k, v, a, bb, a_log, dt_bias, s0, reverse, with_out):
    g = (-jnp.exp(a_log) * jax.nn.softplus(a + dt_bias)).transpose(0, 2, 1)
    beta = jax.nn.sigmoid(bb).transpose(0, 2, 1)
    if reverse:
        k, v, g, beta = (jnp.flip(t_, 2) for t_ in (k, v, g, beta))
        q = None if q is None else jnp.flip(q, 2)
    o, s = gdn_chunked(q, k, v, g, beta, s0, with_out)
    if with_out and reverse:
        o = jnp.flip(o, 2)
    return o, s


def gated_head_norm(o, z, w, h):
    bsz, _, t, d = o.shape
    on = rmsnorm(o.transpose(0, 2, 1, 3), w)
    return (on * jax.nn.silu(z.reshape(bsz, t, h, d))).reshape(bsz, t, h * d)


def token_mixers(px, pc, lb_f, lb_b, hg_norm_w, conv_w, a_log_f, a_log_b,
                 dt_bias_f, dt_bias_b, gd_norm_w, ctx_out):
    f32 = jnp.float32
    bsz = px.shape[0]
    (hq_x, hff_x, hfb_x, hi_x, hg_x, gq_x, gk_x, gv_x, gz_x,
     gaf_x, gab_x, gbf_x, gbb_x) = split_last(px.astype(f32), IN_SIZES)
    (hq_c, hff_c, hfb_c, hi_c, hg_c, gq_c, gk_c, gv_c, gz_c,
     gaf_c, gab_c, gbf_c, gbb_c) = split_last(pc.astype(f32), IN_SIZES)
    s0 = jnp.zeros((bsz, HG_H, HG_DK, HG_DV), f32)
    hq_c_use = hq_c if ctx_out else None
    oc_f, sc_f = hgrn2_direction(hq_c_use, hff_c, hi_c, lb_f, s0, False, ctx_out)
    oc_b, sc_b = hgrn2_direction(hq_c_use, hfb_c, hi_c, lb_b, s0, True, ctx_out)
    ox_f, _ = hgrn2_direction(hq_x, hff_x, hi_x, lb_f, sc_f, False, True)
    ox_b, _ = hgrn2_direction(hq_x, hfb_x, hi_x, lb_b, sc_b, True, True)
    hg_out_x = gated_head_norm(ox_f + ox_b, hg_x, hg_norm_w, HG_H)
    qx, kx, vx = gdn_inputs(gq_x, gk_x, gv_x, conv_w, True)
    qc, kc, vc = gdn_inputs(gq_c if ctx_out else None, gk_c, gv_c, conv_w, False)
    z0 = jnp.zeros((bsz, GD_H, GD_DK, GD_DV), f32)
    pc_f, tc_f = gdn_direction(qc, kc, vc, gaf_c, gbf_c, a_log_f, dt_bias_f, z0, False, ctx_out)
    pc_b, tc_b = gdn_direction(qc, kc, vc, gab_c, gbb_c, a_log_b, dt_bias_b, z0, True, ctx_out)
    px_f, _ = gdn_direction(qx, kx, vx, gaf_x, gbf_x, a_log_f, dt_bias_f, tc_f, False, True)
    px_b, _ = gdn_direction(qx, kx, vx, gab_x, gbb_x, a_log_b, dt_bias_b, tc_b, True, True)
    gd_out_x = gated_head_norm(px_f + px_b, gz_x, gd_norm_w, GD_H)
    mix_x = jnp.concatenate([hg_out_x, gd_out_x], axis=-1).astype(px.dtype)
    if not ctx_out:
        return mix_x, None
    mix_c = jnp.concatenate([gated_head_norm(oc_f + oc_b, hg_c, hg_norm_w, HG_H),
                             gated_head_norm(pc_f + pc_b, gz_c, gd_norm_w, GD_H)], axis=-1)
    return mix_x, mix_c.astype(pc.dtype)


def moe(h, w_router, b_router, w_gate, b_gate, w_up, b_up, w_down, b_down):
    n_tok, d = h.shape
    logits = jnp.dot(h, w_router).astype(jnp.float32) + b_router
    top_val, top_idx = lax.top_k(logits, TOP_K)
    gates = jax.nn.softmax(top_val, axis=-1)
    m = n_tok * TOP_K
    e_flat = top_idx.reshape(m)
    order = jnp.argsort(e_flat)
    e_s = e_flat[order]
    tok_s = (order // TOP_K).astype(jnp.int32)
    g_s = gates.reshape(m)[order]
    counts = jax.ops.segment_sum(jnp.ones((m,), jnp.int32), e_flat, num_segments=N_EXPERTS)
    padded = (counts + MOE_BLOCK - 1) // MOE_BLOCK * MOE_BLOCK
    start = jnp.cumsum(counts) - counts
    pend = jnp.cumsum(padded)
    pstart = pend - padded
    dest = pstart[e_s] + (jnp.arange(m, dtype=jnp.int32) - start[e_s])
    n_blocks = -(-m // MOE_BLOCK) + N_EXPERTS
    row_tok = jnp.full((n_blocks * MOE_BLOCK,), n_tok, jnp.int32).at[dest].set(tok_s)
    h_pad = jnp.concatenate([h, jnp.zeros((1, d), h.dtype)], axis=0)
    xs = h_pad[row_tok].reshape(n_blocks, MOE_BLOCK, d)
    blk_exp = jnp.minimum(jnp.searchsorted(pend, jnp.arange(n_blocks, dtype=jnp.int32) * MOE_BLOCK,
                                           side='right'), N_EXPERTS - 1)

    def expert_block(args):
        xb, e = args
        gate = jnp.minimum(xb @ w_gate[e] + b_gate[e], SWIGLU_LIMIT)
        up = jnp.clip(xb @ w_up[e] + b_up[e], -SWIGLU_LIMIT, SWIGLU_LIMIT)
        act = (up + 1.0) * gate * jax.nn.sigmoid(SWIGLU_ALPHA * gate)
        return act @ w_down[e] + b_down[e]

    ys = lax.map(expert_block, (xs, blk_exp)).reshape(n_blocks * MOE_BLOCK, d)
    out = jnp.zeros((n_tok, d), jnp.float32).at[tok_s].add(ys[dest].astype(jnp.float32) * g_s[:, None])
    return out.astype(h.dtype)


def setup_inputs(seed: int = 0) -> dict:
    key = jax.random.key(seed)
    ks = jax.random.split(key, 32)
    f32 = jnp.float32
    d = D_MODEL
    nrm = lambda k, shape, scale: jax.random.normal(k, shape, f32) * scale

    def inv_softplus_dt(k, shape):
        dt = jnp.exp(jax.random.uniform(k, shape, f32, np.log(1e-3), np.log(1e-1)))
        return dt + jnp.log(-jnp.expm1(-dt))

    return {
        "x": nrm(ks[0], (BATCH, SEQ, d), 1.0),
        "c": nrm(ks[1], (BATCH, d), 1.0),
        "ctx": nrm(ks[2], (BATCH, CTX_LEN, d), 1.0),
        "c_ctx": nrm(ks[3], (d,), 1.0),
        "w_ada": nrm(ks[4], (DEPTH, d, 6 * d), 0.5 * d ** -0.5),
        "b_ada": nrm(ks[5], (DEPTH, 6 * d), 0.02),
        "norm_mix_w": 1.0 + nrm(ks[6], (DEPTH, d), 0.02),
        "w_in": nrm(ks[7], (DEPTH, d, IN_DIM), d ** -0.5),
        "hg_lb_f": nrm(ks[8], (DEPTH + 1, HG_W), 0.1),
        "hg_lb_b": nrm(ks[9], (DEPTH + 1, HG_W), 0.1),
        "hg_norm_w": 1.0 + nrm(ks[10], (DEPTH, HG_DV), 0.02),
        "gd_conv_w": nrm(ks[11], (DEPTH, CONV_W, 3 * GD_W), CONV_W ** -0.5),
        "gd_a_log_f": jnp.log(jax.random.uniform(ks[12], (DEPTH, GD_H), f32, 1.0, 16.0)),
        "gd_a_log_b": jnp.log(jax.random.uniform(ks[13], (DEPTH, GD_H), f32, 1.0, 16.0)),
        "gd_dt_bias_f": inv_softplus_dt(ks[14], (DEPTH, GD_H)),
        "gd_dt_bias_b": inv_softplus_dt(ks[15], (DEPTH, GD_H)),
        "gd_norm_w": 1.0 + nrm(ks[16], (DEPTH, GD_DV), 0.02),
        "w_out": nrm(ks[17], (DEPTH, D_MIX, d), D_MIX ** -0.5),
        "norm_ffn_w": 1.0 + nrm(ks[18], (DEPTH, d), 0.02),
        "w_router": nrm(ks[19], (DEPTH, d, N_EXPERTS), d ** -0.5),
        "b_router": nrm(ks[20], (DEPTH, N_EXPERTS), 0.01),
        "w_gate": nrm(ks[21], (DEPTH, N_EXPERTS, d, D_FF), d ** -0.5),
        "b_gate": nrm(ks[22], (DEPTH, N_EXPERTS, D_FF), 0.02),
        "w_up": nrm(ks[23], (DEPTH, N_EXPERTS, d, D_FF), d ** -0.5),
        "b_up": nrm(ks[24], (DEPTH, N_EXPERTS, D_FF), 0.02),
        "w_down": nrm(ks[25], (DEPTH, N_EXPERTS, D_FF, d), D_FF ** -0.5),
        "b_down": nrm(ks[26], (DEPTH, N_EXPERTS, d), 0.02),
        "norm_out_w": 1.0 + nrm(ks[27], (d,), 0.02),
    }


def reference(x, c, ctx, c_ctx, w_ada, b_ada, norm_mix_w, w_in, hg_lb_f, hg_lb_b, hg_norm_w,
              gd_conv_w, gd_a_log_f, gd_a_log_b, gd_dt_bias_f, gd_dt_bias_b, gd_norm_w, w_out,
              norm_ffn_w, w_router, b_router, w_gate, b_gate, w_up, b_up, w_down, b_down, norm_out_w):
    bsz, t, d = x.shape
    lb_f_all = jnp.cumsum(jax.nn.softmax(hg_lb_f.astype(jnp.float32), axis=0), axis=0)
    lb_b_all = jnp.cumsum(jax.nn.softmax(hg_lb_b.astype(jnp.float32), axis=0), axis=0)
    for l in range(DEPTH):
        ctx_out = l < DEPTH - 1
        mod_x = jax.nn.silu(c) @ w_ada[l] + b_ada[l]
        mod_c = jax.nn.silu(c_ctx) @ w_ada[l] + b_ada[l]
        sh1, sc1, gt1, sh2, sc2, gt2 = jnp.split(mod_x[:, None, :], 6, axis=-1)
        csh1, csc1, cgt1, csh2, csc2, cgt2 = jnp.split(mod_c, 6, axis=-1)
        hx = rmsnorm(x, norm_mix_w[l]) * (1.0 + sc1) + sh1
        hc = rmsnorm(ctx, norm_mix_w[l]) * (1.0 + csc1) + csh1
        mix_x, mix_c = token_mixers(hx @ w_in[l], hc @ w_in[l], lb_f_all[l], lb_b_all[l], hg_norm_w[l],
                                    gd_conv_w[l], gd_a_log_f[l], gd_a_log_b[l], gd_dt_bias_f[l],
                                    gd_dt_bias_b[l], gd_norm_w[l], ctx_out)
        x = x + gt1 * (mix_x @ w_out[l])
        hx2 = rmsnorm(x, norm_ffn_w[l]) * (1.0 + sc2) + sh2
        x = x + gt2 * moe(hx2.reshape(bsz * t, d), w_router[l], b_router[l], w_gate[l], b_gate[l],
                          w_up[l], b_up[l], w_down[l], b_down[l]).reshape(bsz, t, d)
        if ctx_out:
            ctx = ctx + cgt1 * (mix_c @ w_out[l])
            hc2 = rmsnorm(ctx, norm_ffn_w[l]) * (1.0 + csc2) + csh2
            ctx = ctx + cgt2 * moe(hc2.reshape(-1, d), w_router[l], b_router[l], w_gate[l], b_gate[l],
                                   w_up[l], b_up[l], w_down[l], b_down[l]).reshape(ctx.shape)
    return rmsnorm(x, norm_out_w)
```

```python
import contextlib
import numpy as np
import concourse.bass as bass
import concourse.mybir as mybir
from concourse.bass_utils import run_bass_kernel_spmd

F32 = mybir.dt.float32
I32 = mybir.dt.int32
AF = mybir.ActivationFunctionType
ALU = mybir.AluOpType

D = 2048
SEQ = 2048
CTX = 256
NTOK = SEQ + CTX
NT = NTOK // 128
NTX = SEQ // 128
HGW = 1024
GDW = 1024
NH = 8
IN_DIM = 9248
TOPK = 4
EPS = 1e-6
LIMIT = 7.0
ALPHA = 1.702


class Sem:
    def __init__(self, handle, name):
        self.h = handle
        self.name = name
        self.count = 0


class Buf:
    def __init__(self, name):
        self.name = name
        self.ws = {}
        self.r = {}
        self.ld = None
        self.st = None


class T(Buf):
    def __init__(self, em, name, shape, dtype=F32, psum=False):
        super().__init__(name)
        self.is_tile = True
        self.is_psum = psum
        if psum:
            self.t = em.stack.enter_context(em.nc.psum_tensor(name, list(shape), dtype))
        else:
            self.t = em.stack.enter_context(em.nc.sbuf_tensor(name, list(shape), dtype))

    def __getitem__(self, idx):
        return self.t[idx]


class Emitter:
    def __init__(self, nc, stack, n_dma_sems=88):
        self.nc = nc
        self.gstack = stack
        self.stack = stack
        self.eng = {"pe": nc.tensor, "act": nc.scalar, "dve": nc.vector, "pool": nc.gpsimd, "sp": nc.sync}
        self.esem = {k: Sem(stack.enter_context(nc.semaphore("e_" + k)), k) for k in self.eng}
        self.seen = {k: {} for k in self.eng}
        self.pool = [Sem(stack.enter_context(nc.semaphore("d%d" % i)), "d%d" % i) for i in range(n_dma_sems)]
        self.used = []
        self.ninstr = 0
        self.phase_tiles = []
        self.log = None

    def get_sem(self):
        s = self.pool.pop()
        self.used.append(s)
        return s

    def tile(self, name, shape, dtype=F32):
        t = T(self, name, shape, dtype)
        self.phase_tiles.append(t)
        return t

    def psum(self, name, shape, dtype=F32):
        return T(self, name, shape, dtype, psum=True)

    def _waits(self, e, reads, writes, shared=False):
        need = {}

        def add(tk):
            if tk is None:
                return
            sem, val, is_dma = tk
            if is_dma:
                val = sem.count
            if e == "pe" and sem is self.esem["pe"]:
                return
            if need.get(sem, 0) < val:
                need[sem] = val

        for b in reads:
            for tk in b.ws.values():
                add(tk)
            if getattr(b, "is_psum", False):
                for tk in b.r.values():
                    if tk[0] is not self.esem.get(e):
                        add(tk)
        for b in writes:
            if not shared:
                for tk in b.ws.values():
                    add(tk)
            for tk in b.r.values():
                add(tk)
        for sem, val in need.items():
            if self.seen[e].get(sem, 0) < val:
                self.eng[e].wait_ge(sem.h, val)
                self.seen[e][sem] = val
                self.ninstr += 1
                if self.log is not None:
                    self.log.append((e, "wait", sem.name, val))

    def _record(self, tk, reads, writes, shared):
        sem = tk[0]
        for b in reads:
            b.r[sem] = tk
        for b in writes:
            if shared:
                b.ws[sem] = tk
            else:
                b.ws = {sem: tk}
                b.r = {}

    def op(self, e, fn, reads=(), writes=(), shared=False):
        self._waits(e, reads, writes, shared)
        ins = fn(self.eng[e])
        sem = self.esem[e]
        sem.count += 1
        ins.then_inc(sem.h, 1)
        tk = (sem, sem.count, False)
        if self.log is not None:
            self.log.append((e, "inc", sem.name, 1))
        self._record(tk, reads, writes, shared)
        self.ninstr += 1
        return ins

    def dma(self, q, fn, reads=(), writes=(), shared=False):
        self._waits(q, reads, writes, shared)
        sem = None
        for b in writes:
            if isinstance(b, T):
                if b.ld is None:
                    b.ld = self.get_sem()
                sem = b.ld
                break
        if sem is None:
            for b in reads:
                if isinstance(b, T):
                    if b.st is None:
                        b.st = self.get_sem()
                    sem = b.st
                    break
        assert sem is not None
        ins = fn(self.eng[q])
        sem.count += 16
        ins.then_inc(sem.h, 16)
        tk = (sem, sem.count, True)
        if self.log is not None:
            self.log.append((q, "inc", sem.name, 16))
        self._record(tk, reads, writes, shared)
        self.ninstr += 1
        return ins

    def barrier(self):
        sems = list(self.esem.values()) + list(self.used)
        for e in self.eng:
            for s in sems:
                if s is self.esem[e] or s.count == 0:
                    continue
                if self.seen[e].get(s, 0) < s.count:
                    self.eng[e].wait_ge(s.h, s.count)
                    self.seen[e][s] = s.count
                    self.ninstr += 1
                    if self.log is not None:
                        self.log.append((e, "wait", s.name, s.count))

    @contextlib.contextmanager
    def phase(self):
        old_stack, old_tiles = self.stack, self.phase_tiles
        st = contextlib.ExitStack()
        self.stack = st
        self.phase_tiles = []
        try:
            with st:
                yield
                self.barrier()
                for t in self.phase_tiles:
                    for s in (t.ld, t.st):
                        if s is not None:
                            self.used.remove(s)
                            self.pool.append(s)
        finally:
            self.stack = old_stack
            self.phase_tiles = old_tiles

    def mm(self, out, lhsT, rhs, start, stop, reads, writes, shared=False):
        return self.op("pe", lambda g: g.matmul(out, lhsT, rhs, start=start, stop=stop), reads, writes, shared)

    def tr(self, out, in_, ident, reads, writes, shared=False, k=128):
        return self.op("pe", lambda g: g.transpose(out, in_, ident[0:k, 0:k]), list(reads) + [ident], writes, shared)


def build_program(NE=32, dbg=False, upto=99, nhg=NH, ngd=NH):
    NBLK = (SEQ * TOPK) // 128 + NE
    nc = bass.Bass("TRN2", target_bir_lowering=False)

    def din(name, shape, dt=F32):
        return nc.dram_tensor(name, list(shape), dt, kind="ExternalInput").ap()

    def dscr(name, shape, dt=F32, out=False):
        return nc.dram_tensor(name, list(shape), dt, kind="ExternalOutput" if (out or dbg) else "Internal").ap()

    x_d = din("x", [SEQ, D]); ctx_d = din("ctx", [CTX, D]); c_d = din("c", [D]); cctx_d = din("c_ctx", [D])
    w_ada = din("w_ada", [D, 6 * D]); b_ada = din("b_ada", [6 * D]); nmw_d = din("norm_mix_w", [D])
    w_in = din("w_in", [D, IN_DIM]); lbf_d = din("hg_lb_f", [2, HGW]); lbb_d = din("hg_lb_b", [2, HGW])
    hgnw_d = din("hg_norm_w", [128]); conv_d = din("gd_conv_w", [3, 3 * GDW])
    alf_d = din("gd_a_log_f", [NH]); alb_d = din("gd_a_log_b", [NH])
    dtf_d = din("gd_dt_bias_f", [NH]); dtb_d = din("gd_dt_bias_b", [NH]); gdnw_d = din("gd_norm_w", [128])
    w_out = din("w_out", [D, D]); nfw_d = din("norm_ffn_w", [D]); w_rt = din("w_router", [D, NE])
    b_rt = din("b_router", [NE]); w_gate = din("w_gate", [NE * D, D]); b_gate = din("b_gate", [NE, D])
    w_up = din("w_up", [NE * D, D]); b_up = din("b_up", [NE, D]); w_down = din("w_down", [NE * D, D])
    b_down = din("b_down", [NE, D]); now_d = din("norm_out_w", [D])
    y_d = nc.dram_tensor("y", [SEQ, D], F32, kind="ExternalOutput").ap()

    modrows = dscr("modrows", [2, 6 * D])
    pfm = dscr("pfm", [6144, NTOK])
    ptm = dscr("ptm", [NTOK, 3104])
    mixT = dscr("mixT", [D, SEQ])
    x1_d = dscr("x1", [SEQ, D])
    xn2_d = dscr("xn2", [SEQ, D])
    xs_d = dscr("xs", [NBLK * 128, D])
    ys_d = dscr("ys", [NBLK * 128, D])
    B_modrows = Buf("modrows"); B_pfm = Buf("pfm"); B_ptm = Buf("ptm"); B_mixT = Buf("mixT")
    B_x1 = Buf("x1"); B_xn2 = Buf("xn2"); B_xs = Buf("xs"); B_ys = Buf("ys"); B_y = Buf("y")

    gst = contextlib.ExitStack()
    with gst:
        em = Emitter(nc, gst)
        em.log = [] if dbg else None
        nc._em = em
        PS = [em.psum("ps%d" % i, [128, 512]) for i in range(8)]
        ident = em.tile("ident", [128, 128]); ones = em.tile("ones", [128, 128])
        em.op("pool", lambda g: g.memset(ones[:], 1.0), writes=[ones])

        def aff_mask(out_t, cmp, sgn=1):
            em.op("pool", lambda g: g.affine_select(out=out_t[:], in_=ones[:], pattern=[[-sgn, 128]], compare_op=cmp,
                                                     fill=0.0, base=0, channel_multiplier=sgn), reads=[ones], writes=[out_t])

        aff_mask(ident, ALU.is_equal)
        cnt = {"rr": 0}
        dumps = {}

        def dump(name, tl, shape):
            if not dbg:
                return
            d = nc.dram_tensor("dbg_" + name, list(shape), F32, kind="ExternalOutput").ap()
            em.dma("sp", lambda g: g.dma_start(out=d, in_=tl[:]), reads=[tl], writes=[Buf("dbg_" + name)])

        def evac(out_ap, in_ap, reads, writes):
            cnt["rr"] += 1
            if cnt["rr"] % 2:
                em.op("act", lambda g: g.copy(out=out_ap, in_=in_ap), reads, writes)
            else:
                em.op("dve", lambda g: g.tensor_copy(out=out_ap, in_=in_ap), reads, writes)

        def rstd_from_ss(ss, n, width):
            em.op("dve", lambda g: g.tensor_scalar(out=ss[:, 0:n], in0=ss[:, 0:n], scalar1=1.0 / width, scalar2=EPS,
                                                   op0=ALU.mult, op1=ALU.add), reads=[ss], writes=[ss])
            em.op("act", lambda g: g.activation(out=ss[:, 0:n], in_=ss[:, 0:n], func=AF.Sqrt), reads=[ss], writes=[ss])
            em.op("dve", lambda g: g.reciprocal(out=ss[:, 0:n], in_=ss[:, 0:n]), reads=[ss], writes=[ss])

        A1x = em.tile("A1x", [128, 16]); B1x = em.tile("B1x", [128, 16])
        A1c = em.tile("A1c", [128, 16]); B1c = em.tile("B1c", [128, 16])
        A2x = em.tile("A2x", [128, 16]); B2x = em.tile("B2x", [128, 16])

        with em.phase():
            cs = em.tile("cs", [128, 16, 2]); craw = em.tile("craw", [128, 2, 16])
            em.dma("sp", lambda g: g.dma_start(out=craw[:, 0, :], in_=c_d.rearrange("(p q) -> p q", q=16)), writes=[craw], shared=True)
            em.dma("sp", lambda g: g.dma_start(out=craw[:, 1, :], in_=cctx_d.rearrange("(p q) -> p q", q=16)), writes=[craw], shared=True)
            for r in range(2):
                em.op("act", lambda g, r=r: g.activation(out=cs[:, :, r], in_=craw[:, r, :], func=AF.Silu), reads=[craw], writes=[cs], shared=True)
            brow = em.tile("brow", [1, 6 * D])
            em.dma("sp", lambda g: g.dma_start(out=brow[:], in_=b_ada.rearrange("(o n) -> o n", o=1)), writes=[brow])
            wv = w_ada.rearrange("(p q) c -> p q c", q=16)
            wring = [em.tile("adaw%d" % i, [128, 16, 512]) for i in range(3)]
            mrow = [em.tile("mrow%d" % i, [2, 512]) for i in range(2)]
            for s in range(24):
                wt = wring[s % 3]
                em.dma("sp" if s % 2 == 0 else "pool", lambda g, wt=wt, s=s: g.dma_start(out=wt[:], in_=wv[:, :, s * 512:(s + 1) * 512]), writes=[wt])
                ps = PS[s % 2]
                for q in range(16):
                    em.mm(ps[0:2, :], cs[:, q, :], wt[:, q, :], q == 0, False, reads=[cs, wt], writes=[ps])
                em.mm(ps[0:2, :], ones[0:1, 0:2], brow[0:1, s * 512:(s + 1) * 512], False, True, reads=[ones, brow], writes=[ps])
                mr = mrow[s % 2]
                evac(mr[:], ps[0:2, :], [ps], [mr])
                em.dma("sp", lambda g, mr=mr, s=s: g.dma_start(out=modrows[:, s * 512:(s + 1) * 512], in_=mr[:]), reads=[mr], writes=[B_modrows], shared=True)
            tmp = em.tile("modtmp", [128, 6, 16]); nw = em.tile("nw", [128, 2, 16])
            em.dma("sp", lambda g: g.dma_start(out=nw[:, 0, :], in_=nmw_d.rearrange("(p q) -> p q", q=16)), writes=[nw], shared=True)
            em.dma("sp", lambda g: g.dma_start(out=nw[:, 1, :], in_=nfw_d.rearrange("(p q) -> p q", q=16)), writes=[nw], shared=True)

            def col(row, chunk, dst):
                em.dma("sp", lambda g: g.dma_start(out=dst, in_=modrows[row, chunk * D:(chunk + 1) * D].rearrange("(p q) -> p q", q=16)),
                       reads=[B_modrows], writes=[tmp], shared=True)

            col(0, 0, tmp[:, 0, :]); col(0, 1, tmp[:, 1, :]); col(1, 0, tmp[:, 2, :]); col(1, 1, tmp[:, 3, :])
            col(0, 3, tmp[:, 4, :]); col(0, 4, tmp[:, 5, :])

            def mkA(dst, sc_idx, nwi):
                em.op("dve", lambda g: g.scalar_tensor_tensor(out=dst[:], in0=tmp[:, sc_idx, :], scalar=1.0, in1=nw[:, nwi, :],
                                                             op0=ALU.add, op1=ALU.mult), reads=[tmp, nw], writes=[dst])

            mkA(A1x, 1, 0); mkA(A1c, 3, 0); mkA(A2x, 5, 1)
            em.op("dve", lambda g: g.tensor_copy(out=B1x[:], in_=tmp[:, 0, :]), reads=[tmp], writes=[B1x])
            em.op("dve", lambda g: g.tensor_copy(out=B1c[:], in_=tmp[:, 2, :]), reads=[tmp], writes=[B1c])
            em.op("dve", lambda g: g.tensor_copy(out=B2x[:], in_=tmp[:, 4, :]), reads=[tmp], writes=[B2x])

        FM = [(0, 0), (512, 512), (1024, 1024), (1536, 1536), (2048, 2048), (2560, 2560),
              (5120, 3072), (5632, 3584), (6144, 4096), (6656, 4608), (7168, 5120), (7680, 5632)]
        TM = [(3072, 0, 512), (3584, 512, 512), (4096, 1024, 512), (4608, 1536, 512),
              (8192, 2048, 512), (8704, 2560, 512), (9216, 3072, 32)]
        with em.phase():
            wv = w_in.rearrange("(p q) c -> p q c", q=16)
            hxT = em.tile("hxT", [128, 16, 512])
            xt2 = [em.tile("xt%d" % i, [128, D]) for i in range(2)]
            xn = em.tile("xn", [128, D]); junk = em.tile("junk1", [128, D]); ss = em.tile("ss1", [128, 1])
            wring = [em.tile("winw%d" % i, [128, 16, 512]) for i in range(3)]
            ob = [em.tile("ob%d" % i, [128, 512]) for i in range(4)]
            groups = [(0, 2, ctx_d, A1c, B1c)] + [(2 + 4 * g, 4, x_d, A1x, B1x) for g in range(4)]
            ti = 0; wi = 0; oi = 0; pi = 0
            for (t0, ntile, src, A1, B1) in groups:
                ntok = ntile * 128
                for j in range(ntile):
                    row0 = (t0 + j) * 128 - (0 if src is ctx_d else CTX)
                    xt = xt2[ti % 2]; ti += 1
                    em.dma("sp", lambda g, xt=xt, row0=row0, src=src: g.dma_start(out=xt[:], in_=src[row0:row0 + 128, :]), writes=[xt])
                    em.op("act", lambda g, xt=xt: g.activation(out=junk[:], in_=xt[:], func=AF.Square, accum_out=ss[:]), reads=[xt], writes=[junk, ss])
                    rstd_from_ss(ss, 1, D)
                    em.op("dve", lambda g, xt=xt: g.tensor_scalar(out=xn[:], in0=xt[:], scalar1=ss[:, 0:1], scalar2=None, op0=ALU.mult),
                          reads=[xt, ss], writes=[xn])
                    for qq in range(4):
                        ps = PS[pi % 8]; pi += 1
                        for q4 in range(4):
                            q = qq * 4 + q4
                            em.tr(ps[:, q4 * 128:(q4 + 1) * 128], xn[:, q:D:16], ident, [xn], [ps])
                        for q4 in range(4):
                            q = qq * 4 + q4
                            em.op("act", lambda g, q=q, q4=q4, ps=ps, j=j, A1=A1, B1=B1: g.activation(
                                out=hxT[:, q, j * 128:(j + 1) * 128], in_=ps[:, q4 * 128:(q4 + 1) * 128], func=AF.Identity,
                                scale=A1[:, q:q + 1], bias=B1[:, q:q + 1]), reads=[ps, A1, B1], writes=[hxT], shared=True)
                tok0 = t0 * 128
                for (wc, prow) in FM:
                    wt = wring[wi % 3]; wi += 1
                    em.dma("sp" if wi % 2 else "pool", lambda g, wt=wt, wc=wc: g.dma_start(out=wt[:], in_=wv[:, :, wc:wc + 512]), writes=[wt])
                    for sub in range(4):
                        ps = PS[pi % 8]; pi += 1
                        for q in range(16):
                            em.mm(ps[:, 0:ntok], wt[:, q, sub * 128:(sub + 1) * 128], hxT[:, q, 0:ntok], q == 0, q == 15, [wt, hxT], [ps])
                        o = ob[oi % 4]; oi += 1
                        evac(o[:, 0:ntok], ps[:, 0:ntok], [ps], [o])
                        em.dma("pool" if oi % 2 else "sp", lambda g, o=o, prow=prow, sub=sub, tok0=tok0, ntok=ntok: g.dma_start(
                            out=pfm[prow + sub * 128:prow + (sub + 1) * 128, tok0:tok0 + ntok], in_=o[:, 0:ntok]), reads=[o], writes=[B_pfm], shared=True)
                for (wc, pcol, wd) in TM:
                    wt = wring[wi % 3]; wi += 1
                    em.dma("sp" if wi % 2 else "pool", lambda g, wt=wt, wc=wc, wd=wd: g.dma_start(out=wt[:, :, 0:wd], in_=wv[:, :, wc:wc + wd]), writes=[wt])
                    for j in range(ntile):
                        ps = PS[pi % 8]; pi += 1
                        for q in range(16):
                            em.mm(ps[:, 0:wd], hxT[:, q, j * 128:(j + 1) * 128], wt[:, q, 0:wd], q == 0, q == 15, [wt, hxT], [ps])
                        o = ob[oi % 4]; oi += 1
                        evac(o[:, 0:wd], ps[:, 0:wd], [ps], [o])
                        em.dma("pool" if oi % 2 else "sp", lambda g, o=o, pcol=pcol, wd=wd, r0=tok0 + j * 128: g.dma_start(
                            out=ptm[r0:r0 + 128, pcol:pcol + wd], in_=o[:, 0:wd]), reads=[o], writes=[B_ptm], shared=True)

        if upto <= 1:
            em.barrier()
            print("instructions:", em.ninstr)
            return nc

        pcnt = {"i": 0}

        def nextps():
            pcnt["i"] += 1
            return PS[pcnt["i"] % 8]

        MLE = em.tile("MLE", [128, 128]); MGE = em.tile("MGE", [128, 128])
        aff_mask(MLE, ALU.is_ge, -1); aff_mask(MGE, ALU.is_ge, 1)

        def gated_norm_store(o_acc, g_tm, w_bc, feat0, jk, ssn, t1, sg, mT_all):
            for n in range(NTX):
                em.op("act", lambda g, n=n: g.activation(out=jk[:], in_=o_acc[:, n, :], func=AF.Square, accum_out=ssn[:, n:n + 1]),
                      reads=[o_acc], writes=[jk, ssn], shared=True)
            rstd_from_ss(ssn, NTX, 128)
            for n in range(NTX):
                em.op("dve", lambda g, n=n: g.scalar_tensor_tensor(out=t1[:], in0=o_acc[:, n, :], scalar=ssn[:, n:n + 1], in1=w_bc[:],
                                                                  op0=ALU.mult, op1=ALU.mult), reads=[o_acc, ssn, w_bc], writes=[t1])
                em.op("act", lambda g, n=n: g.activation(out=sg[:], in_=g_tm[:, n, :], func=AF.Silu), reads=[g_tm], writes=[sg])
                em.op("dve", lambda g: g.tensor_tensor(out=t1[:], in0=t1[:], in1=sg[:], op=ALU.mult), reads=[t1, sg], writes=[t1])
                ps = nextps()
                em.tr(ps[:, 0:128], t1[:], ident, [t1], [ps])
                evac(mT_all[:, n * 128:(n + 1) * 128], ps[:, 0:128], [ps], [mT_all])
            em.dma("sp", lambda g: g.dma_start(out=mixT[feat0:feat0 + 128, :], in_=mT_all[:]), reads=[mT_all], writes=[B_mixT], shared=True)

        with em.phase():
            lbt = em.tile("lbt", [128, 2, 2, 8]); lb = em.tile("lb", [128, 2, 8]); oml = em.tile("oml", [128, 2, 8])
            lbrow = em.tile("lbrow", [32, 128])
            for di, src in enumerate((lbf_d, lbb_d)):
                em.dma("sp", lambda g, di=di, src=src: g.dma_start(out=lbrow[di * 16:(di + 1) * 16, :], in_=src.rearrange("l (h p) -> (l h) p", p=128)),
                       writes=[lbrow], shared=True)
            ps = nextps()
            em.tr(ps[:, 0:32], lbrow[:], ident, [lbrow], [ps], k=32)
            em.op("dve", lambda g: g.tensor_copy(out=lbt[:].rearrange("p d l h -> p (d l h)"), in_=ps[:, 0:32]), reads=[ps], writes=[lbt])
            em.op("dve", lambda g: g.tensor_tensor(out=lb[:], in0=lbt[:, :, 0, :], in1=lbt[:, :, 1, :], op=ALU.subtract), reads=[lbt], writes=[lb])
            em.op("act", lambda g: g.activation(out=lb[:], in_=lb[:], func=AF.Sigmoid), reads=[lb], writes=[lb])
            em.op("dve", lambda g: g.tensor_scalar(out=oml[:], in0=lb[:], scalar1=-1.0, scalar2=1.0, op0=ALU.mult, op1=ALU.add), reads=[lb], writes=[oml])
            hgw_bc = em.tile("hgw_bc", [128, 128])
            em.dma("sp", lambda g: g.dma_start(out=hgw_bc[:], in_=hgnw_d.partition_broadcast(128)), writes=[hgw_bc])
            rst = em.tile("rst", [128, NTOK])
            em.op("pool", lambda g: g.memset(rst[:], 1.0), writes=[rst])
            em.op("pool", lambda g: g.memset(rst[:, 0:NTOK:128], 0.0), writes=[rst])
            qT = em.tile("qT", [128, NTOK]); zz = em.tile("zz", [128, NTOK]); v_tm = em.tile("v_tm", [128, NT, 128])
            g_tm = em.tile("g_tm", [128, NTX, 128])
            Ft = em.tile("Ft", [128, NTOK]); LF = em.tile("LF", [128, NTOK]); Kt = em.tile("Kt", [128, NTOK])
            CUM = em.tile("CUM", [128, NTOK]); CUMB = em.tile("CUMB", [128, NTOK]); At = em.tile("At", [128, NTOK])
            EQ = em.tile("EQ", [128, NTOK]); EK = em.tile("EK", [128, NTOK]); QD = em.tile("QD", [128, NTOK])
            dec = em.tile("dec", [128, NT]); gend = em.tile("gend", [128, NT])
            S2 = [em.tile("S%d" % i, [128, 128]) for i in range(2)]
            scT = [em.tile("scT%d" % i, [128, 128]) for i in range(2)]; kit = [em.tile("kit%d" % i, [128, 128]) for i in range(2)]
            o_acc = em.tile("o_acc", [128, NTX, 128]); mT_all = em.tile("mT_all", [128, SEQ])
            jk = em.tile("jk", [128, 128]); ssn = em.tile("ssn", [128, NTX]); t1 = em.tile("t1", [128, 128]); sg = em.tile("sg", [128, 128])
            v3 = lambda t: t[:].rearrange("p (n w) -> p n w", w=128)
            for h in range(nhg):
                em.dma("sp", lambda g, h=h: g.dma_start(out=qT[:], in_=pfm[h * 128:(h + 1) * 128, :]), reads=[B_pfm], writes=[qT])
                em.dma("pool", lambda g, h=h: g.dma_start(out=v_tm[:], in_=ptm[:, h * 128:(h + 1) * 128].rearrange("(n p) e -> p n e", p=128)),
                       reads=[B_ptm], writes=[v_tm])
                em.dma("pool", lambda g, h=h: g.dma_start(out=g_tm[:], in_=ptm[CTX:NTOK, 1024 + h * 128:1024 + (h + 1) * 128].rearrange("(n p) e -> p n e", p=128)),
                       reads=[B_ptm], writes=[g_tm])
                for di in range(2):
                    zrow = 1024 + di * 1024 + h * 128
                    em.dma("sp", lambda g, zrow=zrow: g.dma_start(out=zz[:], in_=pfm[zrow:zrow + 128, :]), reads=[B_pfm], writes=[zz])
                    em.op("act", lambda g: g.activation(out=Ft[:], in_=zz[:], func=AF.Sigmoid), reads=[zz], writes=[Ft])
                    em.op("dve", lambda g, di=di, h=h: g.tensor_scalar(out=Ft[:], in0=Ft[:], scalar1=oml[:, di, h:h + 1], scalar2=lb[:, di, h:h + 1],
                                                                       op0=ALU.mult, op1=ALU.add), reads=[Ft, oml, lb], writes=[Ft])
                    em.op("act", lambda g: g.activation(out=LF[:], in_=Ft[:], func=AF.Ln), reads=[Ft], writes=[LF])
                    em.op("dve", lambda g: g.tensor_scalar(out=Kt[:], in0=Ft[:], scalar1=-1.0, scalar2=1.0, op0=ALU.mult, op1=ALU.add), reads=[Ft], writes=[Kt])
                    em.op("dve", lambda g: g.tensor_tensor_scan(out=CUM[:], data0=rst[:], data1=LF[:], initial=0.0, op0=ALU.mult, op1=ALU.add),
                          reads=[rst, LF], writes=[CUM])
                    if di == 0:
                        cum = CUM; last = 127
                    else:
                        em.op("dve", lambda g: g.tensor_tensor(out=CUMB[:], in0=LF[:], in1=CUM[:], op=ALU.subtract), reads=[LF, CUM], writes=[CUMB])
                        em.op("dve", lambda g: g.tensor_tensor(out=v3(CUMB), in0=v3(CUMB), in1=v3(CUM)[:, :, 127:128].to_broadcast([128, NT, 128]), op=ALU.add),
                              reads=[CUMB, CUM], writes=[CUMB])
                        cum = CUMB; last = 0
                    em.op("dve", lambda g, cum=cum: g.tensor_tensor(out=v3(At), in0=v3(cum), in1=v3(cum)[:, :, 64:65].to_broadcast([128, NT, 128]), op=ALU.subtract),
                          reads=[cum], writes=[At])
                    em.op("act", lambda g, last=last: g.activation(out=gend[:], in_=v3(At)[:, :, last], func=AF.Exp), reads=[At], writes=[gend])
                    em.op("act", lambda g, cum=cum, last=last: g.activation(out=dec[:], in_=v3(cum)[:, :, last], func=AF.Exp), reads=[cum], writes=[dec])
                    em.op("act", lambda g: g.activation(out=EQ[:], in_=At[:], func=AF.Exp), reads=[At], writes=[EQ])
                    em.op("dve", lambda g: g.tensor_tensor(out=EQ[:], in0=EQ[:], in1=qT[:], op=ALU.mult), reads=[EQ, qT], writes=[EQ])
                    em.op("act", lambda g: g.activation(out=EK[:], in_=At[:], func=AF.Exp, scale=-1.0), reads=[At], writes=[EK])
                    em.op("dve", lambda g: g.tensor_tensor(out=EK[:], in0=EK[:], in1=Kt[:], op=ALU.mult), reads=[EK, Kt], writes=[EK])
                    em.op("act", lambda g, cum=cum: g.activation(out=QD[:], in_=cum[:], func=AF.Exp), reads=[cum], writes=[QD])
                    em.op("dve", lambda g: g.tensor_tensor(out=QD[:], in0=QD[:], in1=qT[:], op=ALU.mult), reads=[QD, qT], writes=[QD])
                    order = [0, 1] + list(range(2, NT)) if di == 0 else [1, 0] + list(range(NT - 1, 1, -1))
                    MASK = MLE if di == 0 else MGE
                    si = 0
                    em.op("pool", lambda g: g.memset(S2[0][:], 0.0), writes=[S2[0]])
                    for it, n in enumerate(order):
                        Sc = S2[si % 2]; Sn = S2[(si + 1) % 2]; si += 1
                        sl = slice(n * 128, (n + 1) * 128)
                        if n >= 2:
                            ps = nextps(); sc = scT[it % 2]
                            em.mm(ps[:, 0:128], EK[:, sl], EQ[:, sl], True, True, [EK, EQ], [ps])
                            em.op("dve", lambda g, ps=ps, sc=sc: g.tensor_tensor(out=sc[:], in0=ps[:, 0:128], in1=MASK[:], op=ALU.mult), reads=[ps, MASK], writes=[sc])
                            po = nextps()
                            em.mm(po[:, 0:128], sc[:], v_tm[:, n, :], True, False, [sc, v_tm], [po])
                            em.mm(po[:, 0:128], QD[:, sl], Sc[:], False, True, [QD, Sc], [po])
                            if di == 0:
                                em.op("act", lambda g, po=po, n=n: g.copy(out=o_acc[:, n - 2, :], in_=po[:, 0:128]), reads=[po], writes=[o_acc], shared=True)
                            else:
                                em.op("dve", lambda g, po=po, n=n: g.tensor_tensor(out=o_acc[:, n - 2, :], in0=o_acc[:, n - 2, :], in1=po[:, 0:128], op=ALU.add),
                                      reads=[po, o_acc], writes=[o_acc], shared=True)
                        if it == len(order) - 1:
                            break
                        pt = nextps(); kt_ = kit[it % 2]
                        em.tr(pt[:, 0:128], EK[:, sl], ident, [EK], [pt])
                        em.op("act", lambda g, pt=pt, kt_=kt_: g.copy(out=kt_[:], in_=pt[:, 0:128]), reads=[pt], writes=[kt_])
                        pu = nextps()
                        em.mm(pu[:, 0:128], kt_[:], v_tm[:, n, :], True, True, [kt_, v_tm], [pu])
                        em.op("dve", lambda g, Sc=Sc, Sn=Sn, n=n: g.tensor_scalar(out=Sn[:], in0=Sc[:], scalar1=dec[:, n:n + 1], scalar2=None, op0=ALU.mult),
                              reads=[Sc, dec], writes=[Sn])
                        em.op("dve", lambda g, pu=pu, Sn=Sn, n=n: g.scalar_tensor_tensor(out=Sn[:], in0=pu[:, 0:128], scalar=gend[:, n:n + 1], in1=Sn[:],
                                                                                       op0=ALU.mult, op1=ALU.add), reads=[pu, gend, Sn], writes=[Sn])
                    S2 = S2 if si % 2 == 0 else S2[::-1]
                gated_norm_store(o_acc, g_tm, hgw_bc, h * 128, jk, ssn, t1, sg, mT_all)
        if upto <= 2:
            em.barrier()
            print("instructions:", em.ninstr)
            return nc

        with em.phase():
            SAME = em.tile("SAME", [128, 128])
            em.op("pool", lambda g: g.memset(SAME[:], 0.0), writes=[SAME])
            em.op("pool", lambda g: g.memset(SAME[0:64, 0:64], 1.0), writes=[SAME])
            em.op("pool", lambda g: g.memset(SAME[64:128, 64:128], 1.0), writes=[SAME])
            SEL = em.tile("SEL", [128, 2, 128])
            em.op("pool", lambda g: g.memset(SEL[:], 0.0), writes=[SEL])
            em.op("pool", lambda g: g.memset(SEL[0:64, 0, :], 1.0), writes=[SEL])
            em.op("pool", lambda g: g.memset(SEL[64:128, 1, :], 1.0), writes=[SEL])
            TRI = [em.tile("TRI%d" % i, [128, 128]) for i in range(2)]
            NEGS = [em.tile("NEGS%d" % i, [128, 128]) for i in range(2)]
            NEGI = [em.tile("NEGI%d" % i, [128, 128]) for i in range(2)]
            for di, M in enumerate((MLE, MGE)):
                em.op("dve", lambda g, di=di, M=M: g.tensor_tensor(out=TRI[di][:], in0=M[:], in1=SAME[:], op=ALU.mult), reads=[M, SAME], writes=[TRI[di]])
                em.op("dve", lambda g, di=di: g.tensor_scalar(out=NEGI[di][:], in0=TRI[di][:], scalar1=-1.0, scalar2=1e9, op0=ALU.add, op1=ALU.mult),
                      reads=[TRI[di]], writes=[NEGI[di]])
                em.op("dve", lambda g, di=di: g.tensor_tensor(out=NEGS[di][:], in0=TRI[di][:], in1=ident[:], op=ALU.subtract), reads=[TRI[di], ident], writes=[NEGS[di]])
                em.op("dve", lambda g, di=di: g.tensor_scalar(out=NEGS[di][:], in0=NEGS[di][:], scalar1=-1.0, scalar2=1e9, op0=ALU.add, op1=ALU.mult),
                      reads=[NEGS[di]], writes=[NEGS[di]])
            cwt = em.tile("cwt", [128, 3, 24]); cwrow = em.tile("cwrow", [72, 128])
            em.dma("sp", lambda g: g.dma_start(out=cwrow[:], in_=conv_d.rearrange("j (g p) -> (j g) p", p=128)), writes=[cwrow])
            ps = nextps()
            em.tr(ps[:, 0:72], cwrow[:], ident, [cwrow], [ps], k=72)
            em.op("dve", lambda g: g.tensor_copy(out=cwt[:].rearrange("p j g -> p (j g)"), in_=ps[:, 0:72]), reads=[ps], writes=[cwt])
            gdw_bc = em.tile("gdw_bc", [128, 128])
            em.dma("sp", lambda g: g.dma_start(out=gdw_bc[:], in_=gdnw_d.partition_broadcast(128)), writes=[gdw_bc])
            alg = em.tile("alg", [128, 2, 8]); dtb = em.tile("dtb", [128, 2, 8])
            for di, (a_, d_) in enumerate(((alf_d, dtf_d), (alb_d, dtb_d))):
                em.dma("sp", lambda g, di=di, a_=a_: g.dma_start(out=alg[:, di, :], in_=a_.partition_broadcast(128)), writes=[alg], shared=True)
                em.dma("sp", lambda g, di=di, d_=d_: g.dma_start(out=dtb[:, di, :], in_=d_.partition_broadcast(128)), writes=[dtb], shared=True)
            em.op("act", lambda g: g.activation(out=alg[:], in_=alg[:], func=AF.Exp), reads=[alg], writes=[alg])
            em.op("dve", lambda g: g.tensor_scalar(out=alg[:], in0=alg[:], scalar1=-1.0, scalar2=None, op0=ALU.mult), reads=[alg], writes=[alg])
            gt = em.tile("gt", [128, NT, 32])
            em.dma("sp", lambda g: g.dma_start(out=gt[:], in_=ptm[:, 3072:3104].rearrange("(n p) c -> p n c", p=128)), reads=[B_ptm], writes=[gt])
            names = ["gcol", "lnb", "allg", "cumc", "ncum", "r1", "er1", "ecum", "send", "beta", "decb"]
            G = {}
            for di in range(2):
                for nm in names:
                    shp = [128, NT, 32] if nm == "allg" else ([128, NT, 16] if nm == "decb" else [128, NT, 8])
                    G[nm, di] = em.tile("%s%d" % (nm, di), shp)
                gcol, lnb, allg = G["gcol", di], G["lnb", di], G["allg", di]
                em.op("dve", lambda g, di=di, gcol=gcol: g.tensor_tensor(out=gcol[:], in0=gt[:, :, di * 8:(di + 1) * 8],
                                                                        in1=dtb[:, di:di + 1, :].to_broadcast([128, NT, 8]), op=ALU.add), reads=[gt, dtb], writes=[gcol])
                em.op("act", lambda g, gcol=gcol: g.activation(out=gcol[:], in_=gcol[:], func=AF.Exp), reads=[gcol], writes=[gcol])
                em.op("act", lambda g, gcol=gcol: g.activation(out=gcol[:], in_=gcol[:], func=AF.Ln, bias=1.0), reads=[gcol], writes=[gcol])
                em.op("dve", lambda g, di=di, gcol=gcol: g.tensor_tensor(out=gcol[:], in0=gcol[:], in1=alg[:, di:di + 1, :].to_broadcast([128, NT, 8]), op=ALU.mult),
                      reads=[gcol, alg], writes=[gcol])
                em.op("act", lambda g, di=di, lnb=lnb: g.activation(out=lnb[:], in_=gt[:, :, 16 + di * 8:16 + (di + 1) * 8], func=AF.Exp, scale=-1.0), reads=[gt], writes=[lnb])
                em.op("act", lambda g, lnb=lnb: g.activation(out=lnb[:], in_=lnb[:], func=AF.Ln, bias=1.0), reads=[lnb], writes=[lnb])
                em.op("dve", lambda g, lnb=lnb: g.tensor_scalar(out=lnb[:], in0=lnb[:], scalar1=-1.0, scalar2=None, op0=ALU.mult), reads=[lnb], writes=[lnb])
                for n in range(NT):
                    ps = nextps()
                    em.mm(ps[:, 0:8], TRI[di][:], gcol[:, n, :], True, True, [TRI[di], gcol], [ps])
                    em.mm(ps[:, 8:16], SAME[:], gcol[:, n, :], True, True, [SAME, gcol], [ps])
                    em.mm(ps[:, 16:24], SEL[:, 0, :], gcol[:, n, :], True, True, [SEL, gcol], [ps])
                    em.mm(ps[:, 24:32], SEL[:, 1, :], gcol[:, n, :], True, True, [SEL, gcol], [ps])
                    evac(allg[:, n, :], ps[:, 0:32], [ps], [allg])
                cumc, ncum, r1, er1, ecum, send, beta, decb = (G[k, di] for k in ("cumc", "ncum", "r1", "er1", "ecum", "send", "beta", "decb"))
                em.op("dve", lambda g, cumc=cumc, allg=allg: g.tensor_copy(out=cumc[:], in_=allg[:, :, 0:8]), reads=[allg], writes=[cumc])
                em.op("dve", lambda g, ncum=ncum, cumc=cumc: g.tensor_scalar(out=ncum[:], in0=cumc[:], scalar1=-1.0, scalar2=None, op0=ALU.mult), reads=[cumc], writes=[ncum])
                em.op("dve", lambda g, r1=r1, cumc=cumc, lnb=lnb: g.tensor_tensor(out=r1[:], in0=cumc[:], in1=lnb[:], op=ALU.add), reads=[cumc, lnb], writes=[r1])
                em.op("act", lambda g, er1=er1, r1=r1: g.activation(out=er1[:], in_=r1[:], func=AF.Exp), reads=[r1], writes=[er1])
                em.op("act", lambda g, ecum=ecum, cumc=cumc: g.activation(out=ecum[:], in_=cumc[:], func=AF.Exp), reads=[cumc], writes=[ecum])
                em.op("dve", lambda g, send=send, allg=allg, cumc=cumc: g.tensor_tensor(out=send[:], in0=allg[:, :, 8:16], in1=cumc[:], op=ALU.subtract), reads=[allg, cumc], writes=[send])
                em.op("act", lambda g, send=send: g.activation(out=send[:], in_=send[:], func=AF.Exp), reads=[send], writes=[send])
                em.op("act", lambda g, beta=beta, lnb=lnb: g.activation(out=beta[:], in_=lnb[:], func=AF.Exp), reads=[lnb], writes=[beta])
                em.op("act", lambda g, decb=decb, allg=allg: g.activation(out=decb[:], in_=allg[:, :, 16:32], func=AF.Exp), reads=[allg], writes=[decb])

            raw = em.tile("raw", [128, NTOK]); SQ = em.tile("SQ", [128, NTOK]); RI = em.tile("RI", [128, 512])
            YQ = em.tile("YQ", [128, NTOK]); YK = em.tile("YK", [128, NTOK]); YV = em.tile("YV", [128, NTOK])
            k_tm = em.tile("k_tm", [128, NT, 128]); vg_tm = em.tile("vg_tm", [128, NT, 128]); z_tm = em.tile("z_tm", [128, NTX, 128])
            o_fb = [em.tile("o_fb%d" % i, [128, NTX, 128]) for i in range(2)]
            mT_all = em.tile("mT_all2", [128, SEQ])
            jk = em.tile("jk2", [128, 128]); ssn = em.tile("ssn2", [128, NTX]); t1 = em.tile("t12", [128, 128]); sg = em.tile("sg2", [128, 128])
            WK = {}
            for di in range(2):
                for nm in ("DGr", "DGn", "DGc", "E1", "E2", "Bm", "Bt", "AT", "P", "Pt", "Pn", "Ptn", "R", "Rn", "vb", "kbd", "kend", "U", "WT", "VN", "O", "Sa", "Sb"):
                    WK[nm, di] = em.tile("%s_%d" % (nm, di), [128, 128])

            def conv_silu(Y, gi, h):
                cj = gi * 8 + h
                em.op("dve", lambda g: g.tensor_scalar(out=Y[:], in0=raw[:], scalar1=cwt[:, 1, cj:cj + 1], scalar2=None, op0=ALU.mult), reads=[raw, cwt], writes=[Y])
                segs = [(Y[:, 0:CTX].rearrange("p (r w) -> p r w", w=CTX), raw[:, 0:CTX].rearrange("p (r w) -> p r w", w=CTX), CTX),
                        (Y[:, CTX:NTOK].rearrange("p (r w) -> p r w", w=64), raw[:, CTX:NTOK].rearrange("p (r w) -> p r w", w=64), 64)]
                for (y3, a3, w) in segs:
                    em.op("dve", lambda g, y3=y3, a3=a3, w=w: g.scalar_tensor_tensor(out=y3[:, :, 1:w], in0=a3[:, :, 0:w - 1], scalar=cwt[:, 0, cj:cj + 1], in1=y3[:, :, 1:w],
                                                                                   op0=ALU.mult, op1=ALU.add), reads=[raw, cwt, Y], writes=[Y])
                    em.op("dve", lambda g, y3=y3, a3=a3, w=w: g.scalar_tensor_tensor(out=y3[:, :, 0:w - 1], in0=a3[:, :, 1:w], scalar=cwt[:, 2, cj:cj + 1], in1=y3[:, :, 0:w - 1],
                                                                                   op0=ALU.mult, op1=ALU.add), reads=[raw, cwt, Y], writes=[Y])
                em.op("act", lambda g: g.activation(out=Y[:], in_=Y[:], func=AF.Silu), reads=[Y], writes=[Y])

            def l2norm(Y, mult):
                em.op("dve", lambda g: g.tensor_tensor(out=SQ[:], in0=Y[:], in1=Y[:], op=ALU.mult), reads=[Y], writes=[SQ])
                for c0 in range(0, NTOK, 512):
                    w = min(512, NTOK - c0)
                    ps = nextps()
                    em.mm(ps[:, 0:w], ones[:], SQ[:, c0:c0 + w], True, True, [ones, SQ], [ps])
                    em.op("dve", lambda g, ps=ps, w=w: g.tensor_scalar(out=RI[:, 0:w], in0=ps[:, 0:w], scalar1=EPS, scalar2=None, op0=ALU.add), reads=[ps], writes=[RI])
                    em.op("act", lambda g, w=w: g.activation(out=RI[:, 0:w], in_=RI[:, 0:w], func=AF.Sqrt), reads=[RI], writes=[RI])
                    em.op("dve", lambda g, w=w: g.reciprocal(out=RI[:, 0:w], in_=RI[:, 0:w]), reads=[RI], writes=[RI])
                    em.op("dve", lambda g, c0=c0, w=w: g.scalar_tensor_tensor(out=Y[:, c0:c0 + w], in0=Y[:, c0:c0 + w], scalar=mult, in1=RI[:, 0:w], op0=ALU.mult, op1=ALU.mult),
                          reads=[Y, RI], writes=[Y])

            def gdn_chain(h, di):
                W = lambda nm: WK[nm, di]
                cumc, ncum, r1, er1, ecum, send, beta, decb = (G[k, di] for k in ("cumc", "ncum", "r1", "er1", "ecum", "send", "beta", "decb"))
                Sc, Sn = W("Sa"), W("Sb")
                em.op("pool", lambda g: g.memset(Sc[:], 0.0), writes=[Sc])
                order = [0, 1] + list(range(2, NT)) if di == 0 else [1, 0] + list(range(NT - 1, 1, -1))
                corder = (0, 1) if di == 0 else (1, 0)
                o_out = o_fb[di]
                for n in order:
                    sl = slice(n * 128, (n + 1) * 128)
                    isx = n >= 2
                    col = lambda t: t[:, n, h:h + 1]
                    psA = nextps()
                    em.mm(psA[:, 0:128], YK[:, sl], YK[:, sl], True, True, [YK], [psA])
                    if isx:
                        em.mm(psA[:, 128:256], YK[:, sl], YQ[:, sl], True, True, [YK, YQ], [psA])
                    em.op("dve", lambda g: g.tensor_scalar(out=W("DGr")[:], in0=ident[:], scalar1=col(r1), scalar2=None, op0=ALU.mult), reads=[ident, r1], writes=[W("DGr")])
                    em.op("dve", lambda g: g.tensor_scalar(out=W("DGn")[:], in0=ident[:], scalar1=col(ncum), scalar2=None, op0=ALU.mult), reads=[ident, ncum], writes=[W("DGn")])
                    psD = nextps()
                    em.mm(psD[:, 0:128], ones[:], W("DGr")[:], True, False, [ones, W("DGr")], [psD])
                    em.mm(psD[:, 0:128], W("DGn")[:], ones[:], False, True, [ones, W("DGn")], [psD])
                    if isx:
                        em.op("dve", lambda g: g.tensor_scalar(out=W("DGc")[:], in0=ident[:], scalar1=col(cumc), scalar2=None, op0=ALU.mult), reads=[ident, cumc], writes=[W("DGc")])
                        em.mm(psD[:, 128:256], ones[:], W("DGc")[:], True, False, [ones, W("DGc")], [psD])
                        em.mm(psD[:, 128:256], W("DGn")[:], ones[:], False, True, [ones, W("DGn")], [psD])
                    em.op("dve", lambda g: g.tensor_tensor(out=W("E1")[:], in0=psD[:, 0:128], in1=NEGS[di][:], op=ALU.add), reads=[psD, NEGS[di]], writes=[W("E1")])
                    em.op("act", lambda g: g.activation(out=W("E1")[:], in_=W("E1")[:], func=AF.Exp), reads=[W("E1")], writes=[W("E1")])
                    em.op("dve", lambda g: g.tensor_tensor(out=W("Bm")[:], in0=psA[:, 0:128], in1=W("E1")[:], op=ALU.mult), reads=[psA, W("E1")], writes=[W("Bm")])
                    if isx:
                        em.op("dve", lambda g: g.tensor_tensor(out=W("E2")[:], in0=psD[:, 128:256], in1=NEGI[di][:], op=ALU.add), reads=[psD, NEGI[di]], writes=[W("E2")])
                        em.op("act", lambda g: g.activation(out=W("E2")[:], in_=W("E2")[:], func=AF.Exp), reads=[W("E2")], writes=[W("E2")])
                        em.op("dve", lambda g: g.tensor_tensor(out=W("AT")[:], in0=psA[:, 128:256], in1=W("E2")[:], op=ALU.mult), reads=[psA, W("E2")], writes=[W("AT")])
                    yield
                    if upto == 2.6:
                        continue
                    pst = nextps()
                    em.tr(pst[:, 0:128], W("Bm")[:], ident, [W("Bm")], [pst])
                    em.op("act", lambda g: g.copy(out=W("Bt")[:], in_=pst[:, 0:128]), reads=[pst], writes=[W("Bt")])
                    em.op("dve", lambda g: g.tensor_tensor(out=W("R")[:], in0=ident[:], in1=W("Bm")[:], op=ALU.subtract), reads=[ident, W("Bm")], writes=[W("R")])
                    P, Pt, Pn, Ptn, R, Rn = W("Bm"), W("Bt"), W("Pn"), W("Ptn"), W("R"), W("Rn")
                    for lvl in range(5):
                        yield
                        ps2 = nextps()
                        em.mm(ps2[:, 0:128], P[:], Pt[:], True, True, [P, Pt], [ps2])
                        em.op("act", lambda g, ps2=ps2, Ptn=Ptn: g.copy(out=Ptn[:], in_=ps2[:, 0:128]), reads=[ps2], writes=[Ptn])
                        if lvl < 4:
                            em.mm(ps2[:, 128:256], Pt[:], P[:], True, True, [P, Pt], [ps2])
                            em.op("dve", lambda g, ps2=ps2, Pn=Pn: g.tensor_copy(out=Pn[:], in_=ps2[:, 128:256]), reads=[ps2], writes=[Pn])
                        ps3 = nextps()
                        em.mm(ps3[:, 0:128], Ptn[:], R[:], True, True, [Ptn, R], [ps3])
                        em.op("dve", lambda g, ps3=ps3, R=R, Rn=Rn: g.tensor_tensor(out=Rn[:], in0=ps3[:, 0:128], in1=R[:], op=ALU.add), reads=[ps3, R], writes=[Rn])
                        P, Pn = Pn, P
                        Pt, Ptn = Ptn, Pt
                        R, Rn = Rn, R
                    yield
                    if upto == 2.7:
                        continue
                    em.op("dve", lambda g: g.tensor_scalar(out=W("vb")[:], in0=vg_tm[:, n, :], scalar1=col(beta), scalar2=None, op0=ALU.mult), reads=[vg_tm, beta], writes=[W("vb")])
                    em.op("dve", lambda g: g.tensor_scalar(out=W("kbd")[:], in0=k_tm[:, n, :], scalar1=col(er1), scalar2=None, op0=ALU.mult), reads=[k_tm, er1], writes=[W("kbd")])
                    em.op("dve", lambda g: g.tensor_scalar(out=W("kend")[:], in0=k_tm[:, n, :], scalar1=col(send), scalar2=None, op0=ALU.mult), reads=[k_tm, send], writes=[W("kend")])
                    if n == 2 and h == 0:
                        for nm in ("DGr", "DGn", "E1", "Bm", "Bt", "vb", "kbd"):
                            dump("%s_%d" % (nm, di), W(nm), [128, 128])
                        dump("R_%d" % di, R, [128, 128])
                        if di == 0:
                            for nm in ("cumc", "r1", "ncum", "er1", "beta", "send", "gcol", "lnb"):
                                for dd in range(2):
                                    dump("%s%d" % (nm, dd), G[nm, dd], [128, NT, 8])
                            dump("k_tm", k_tm, [128, NT, 128]); dump("vg_tm", vg_tm, [128, NT, 128])
                    if upto == 2.71:
                        continue
                    psu = nextps()
                    em.mm(psu[:, 0:128], R[:], W("vb")[:], True, True, [R, W("vb")], [psu])
                    em.mm(psu[:, 128:256], W("kbd")[:], R[:], True, True, [R, W("kbd")], [psu])
                    if upto == 2.72:
                        continue
                    em.op("act", lambda g, psu=psu: g.copy(out=W("U")[:], in_=psu[:, 0:128]), reads=[psu], writes=[W("U")])
                    em.op("dve", lambda g, psu=psu: g.tensor_copy(out=W("WT")[:], in_=psu[:, 128:256]), reads=[psu], writes=[W("WT")])
                    if upto == 2.8:
                        continue
                    for c in corder:
                        yield
                        rs = slice(c * 64, (c + 1) * 64)
                        psv = nextps()
                        em.mm(psv[:, 0:128], W("WT")[:], Sc[:], True, True, [W("WT"), Sc], [psv])
                        if isx:
                            em.mm(psv[:, 128:256], YQ[:, sl], Sc[:], True, True, [YQ, Sc], [psv])
                        em.op("dve", lambda g, rs=rs, psv=psv: g.tensor_tensor(out=W("VN")[rs, :], in0=W("U")[rs, :], in1=psv[rs, 0:128], op=ALU.subtract),
                              reads=[W("U"), psv], writes=[W("VN")], shared=True)
                        if isx:
                            em.op("act", lambda g, rs=rs, psv=psv: g.activation(out=W("O")[rs, :], in_=psv[rs, 128:256], func=AF.Identity, scale=ecum[rs, n, h:h + 1]),
                                  reads=[psv, ecum], writes=[W("O")], shared=True)
                        pss = nextps()
                        em.mm(pss[:, 0:128], W("kend")[rs, :], W("VN")[rs, :], True, True, [W("kend"), W("VN")], [pss])
                        em.op("dve", lambda g, c=c, Sc=Sc, Sn=Sn: g.tensor_scalar(out=Sn[:], in0=Sc[:], scalar1=decb[:, n, c * 8 + h:c * 8 + h + 1], scalar2=None, op0=ALU.mult),
                              reads=[Sc, decb], writes=[Sn])
                        em.op("dve", lambda g, pss=pss, Sn=Sn: g.tensor_tensor(out=Sn[:], in0=Sn[:], in1=pss[:, 0:128], op=ALU.add), reads=[pss, Sn], writes=[Sn])
                        Sc, Sn = Sn, Sc
                    if isx:
                        yield
                        pso = nextps()
                        em.mm(pso[:, 0:128], W("AT")[:], W("VN")[:], True, True, [W("AT"), W("VN")], [pso])
                        em.op("dve", lambda g, pso=pso: g.tensor_tensor(out=o_out[:, n - 2, :], in0=W("O")[:], in1=pso[:, 0:128], op=ALU.add),
                              reads=[W("O"), pso], writes=[o_out], shared=True)
                    yield

            for h in range(ngd):
                for gi, Y in enumerate((YQ, YK, YV)):
                    r0 = 3072 + gi * 1024 + h * 128
                    em.dma("sp", lambda g, r0=r0: g.dma_start(out=raw[:], in_=pfm[r0:r0 + 128, :]), reads=[B_pfm], writes=[raw])
                    conv_silu(Y, gi, h)
                em.dma("pool", lambda g, h=h: g.dma_start(out=z_tm[:], in_=ptm[CTX:NTOK, 2048 + h * 128:2048 + (h + 1) * 128].rearrange("(n p) e -> p n e", p=128)),
                       reads=[B_ptm], writes=[z_tm])
                l2norm(YQ, 128 ** -0.5); l2norm(YK, 1.0)
                for n in range(NT):
                    for (Y, dst) in ((YK, k_tm), (YV, vg_tm)):
                        ps = nextps()
                        em.tr(ps[:, 0:128], Y[:, n * 128:(n + 1) * 128], ident, [Y], [ps])
                        evac(dst[:, n, :], ps[:, 0:128], [ps], [dst])
                if upto < 3:
                    for i in range(2):
                        em.op("pool", lambda g, i=i: g.memset(o_fb[i][:], 1.0), writes=[o_fb[i]])
                if upto == 2.5:
                    continue
                gens = [gdn_chain(h, 0), gdn_chain(h, 1)]
                alive = [True, True]
                while any(alive):
                    for i, gen in enumerate(gens):
                        if alive[i]:
                            try:
                                next(gen)
                            except StopIteration:
                                alive[i] = False
                em.op("dve", lambda g: g.tensor_tensor(out=o_fb[0][:], in0=o_fb[0][:], in1=o_fb[1][:], op=ALU.add), reads=[o_fb[0], o_fb[1]], writes=[o_fb[0]])
                gated_norm_store(o_fb[0], z_tm, gdw_bc, 1024 + h * 128, jk, ssn, t1, sg, mT_all)
        if upto <= 3:
            em.barrier()
            print("instructions:", em.ninstr)
            return nc

        bc_blk = nc.gpsimd.to_reg(NBLK * 128 - 1); bc_w = nc.gpsimd.to_reg(NE * 128 * 4 - 1); bc_b = nc.gpsimd.to_reg(NE - 1)
        icols = [em.tile("icol%d" % i, [128, 1], I32) for i in range(8)]
        icnt = {"i": 0}

        def probe_ind(tag, src=None, ic_=None):
            import os
            if os.environ.get("PROBE_IND") != "1":
                return
            try:
                tt = icols[0] if ic_ is None else ic_
                src = zt if src is None else src
                nc.gpsimd.indirect_dma_start(out=xs_d[:, :], out_offset=bass.IndirectOffsetOnAxis(ap=tt[:, :], axis=0), in_=src[:, :], in_offset=None,
                                             bounds_check=bc_blk, oob_is_err=False).then_inc(em.esem["pool"].h, 16)
                print("PROBE_IND", tag, "ok")
            except Exception as ex:
                print("PROBE_IND", tag, "ERR", ex)

        def idxcol(src, c):
            t_ = icols[icnt["i"] % 8]; icnt["i"] += 1
            em.op("pool", lambda g: g.tensor_copy(out=t_[:], in_=src[:, c:c + 1]), reads=[src], writes=[t_])
            return t_

        desti = em.tile("desti", [128, NTX * TOPK], I32); gates = em.tile("gates", [128, NTX, TOPK])
        widx = em.tile("widx", [128, NBLK * 4], I32); bidx = em.tile("bidx", [128, NBLK], I32)
        zt = em.tile("zt", [128, D])
        em.op("pool", lambda g: g.memset(zt[:], 0.0), writes=[zt])
        probe_ind("before zero fill")
        for j in range(NBLK):
            em.dma("pool", lambda g, j=j: g.dma_start(out=xs_d[j * 128:(j + 1) * 128, :], in_=zt[:]), reads=[zt], writes=[B_xs], shared=True)
        probe_ind("after zero fill")
        with em.phase():
            probe_ind("in phase")
            gt1_bc = em.tile("gt1_bc", [128, D])
            em.dma("sp", lambda g: g.dma_start(out=gt1_bc[:], in_=modrows[0, 2 * D:3 * D].partition_broadcast(128)), reads=[B_modrows], writes=[gt1_bc])
            wrt = em.tile("wrt", [128, 16, NE]); brt = em.tile("brt", [1, NE])
            em.dma("sp", lambda g: g.dma_start(out=wrt[:], in_=w_rt.rearrange("(p q) e -> p q e", q=16)), writes=[wrt])
            em.dma("sp", lambda g: g.dma_start(out=brt[:], in_=b_rt.rearrange("(o n) -> o n", o=1)), writes=[brt])
            mg = em.tile("mg", [128, 16, 512]); wring = [em.tile("wo%d" % i, [128, 16, 512]) for i in range(2)]
            x1t = [em.tile("x1t%d" % i, [128, D]) for i in range(4)]
            tmpy = [em.tile("tmpy%d" % i, [128, 512]) for i in range(2)]
            junk = em.tile("junk3", [128, D]); ss = em.tile("ss3", [128, 1]); xn2 = em.tile("xn2t", [128, D]); h2T = em.tile("h2T", [128, 16, 128])
            lg = em.tile("lg", [128, NTX, NE]); mask_all = em.tile("mask_all", [128, NTX, NE]); rank = em.tile("rank", [128, NTX, NE])
            top8 = em.tile("top8", [128, NTX, 8])
            SLT = em.tile("SLT", [128, 128])
            em.op("dve", lambda g: g.tensor_tensor(out=SLT[:], in0=MLE[:], in1=ident[:], op=ALU.subtract), reads=[MLE, ident], writes=[SLT])
            wov = w_out.rearrange("(k p) c -> p k c", p=128); mxv = mixT.rearrange("(k p) t -> p k t", p=128)
            wi = 0; yi = 0
            for gI in range(4):
                tok0 = gI * 512
                em.dma("sp", lambda g, tok0=tok0: g.dma_start(out=mg[:], in_=mxv[:, :, tok0:tok0 + 512]), reads=[B_mixT], writes=[mg])
                for j in range(4):
                    em.dma("pool", lambda g, j=j, tok0=tok0: g.dma_start(out=x1t[j][:], in_=x_d[tok0 + j * 128:tok0 + (j + 1) * 128, :]), writes=[x1t[j]])
                for cg in range(4):
                    wt = wring[wi % 2]; wi += 1
                    em.dma("sp", lambda g, wt=wt, cg=cg: g.dma_start(out=wt[:], in_=wov[:, :, cg * 512:(cg + 1) * 512]), writes=[wt])
                    for j in range(4):
                        ps = nextps()
                        for k in range(16):
                            em.mm(ps[:, :], mg[:, k, j * 128:(j + 1) * 128], wt[:, k, :], k == 0, k == 15, [mg, wt], [ps])
                        ty = tmpy[yi % 2]; yi += 1
                        em.op("dve", lambda g, ps=ps, ty=ty, cg=cg: g.tensor_tensor(out=ty[:], in0=ps[:, :], in1=gt1_bc[:, cg * 512:(cg + 1) * 512], op=ALU.mult),
                              reads=[ps, gt1_bc], writes=[ty])
                        em.op("dve", lambda g, ty=ty, j=j, cg=cg: g.tensor_tensor(out=x1t[j][:, cg * 512:(cg + 1) * 512], in0=x1t[j][:, cg * 512:(cg + 1) * 512], in1=ty[:], op=ALU.add),
                              reads=[ty, x1t[j]], writes=[x1t[j]])
                for j in range(4):
                    t = gI * 4 + j
                    xt = x1t[j]
                    em.dma("sp", lambda g, xt=xt, t=t: g.dma_start(out=x1_d[t * 128:(t + 1) * 128, :], in_=xt[:]), reads=[xt], writes=[B_x1], shared=True)
                    em.op("act", lambda g, xt=xt: g.activation(out=junk[:], in_=xt[:], func=AF.Square, accum_out=ss[:]), reads=[xt], writes=[junk, ss])
                    rstd_from_ss(ss, 1, D)
                    em.op("dve", lambda g, xt=xt: g.tensor_scalar(out=xn2[:], in0=xt[:], scalar1=ss[:, 0:1], scalar2=None, op0=ALU.mult), reads=[xt, ss], writes=[xn2])
                    em.dma("sp", lambda g, t=t: g.dma_start(out=xn2_d[t * 128:(t + 1) * 128, :], in_=xn2[:]), reads=[xn2], writes=[B_xn2], shared=True)
                    for qq in range(4):
                        ps = nextps()
                        for q4 in range(4):
                            q = qq * 4 + q4
                            em.tr(ps[:, q4 * 128:(q4 + 1) * 128], xn2[:, q:D:16], ident, [xn2], [ps])
                        for q4 in range(4):
                            q = qq * 4 + q4
                            em.op("act", lambda g, q=q, q4=q4, ps=ps: g.activation(out=h2T[:, q, :], in_=ps[:, q4 * 128:(q4 + 1) * 128], func=AF.Identity,
                                                                                  scale=A2x[:, q:q + 1], bias=B2x[:, q:q + 1]), reads=[ps, A2x, B2x], writes=[h2T], shared=True)
                    ps = nextps()
                    for q in range(16):
                        em.mm(ps[:, 0:NE], h2T[:, q, :], wrt[:, q, :], q == 0, False, [h2T, wrt], [ps])
                    em.mm(ps[:, 0:NE], ones[0:1, 0:128], brt[0:1, :], False, True, [ones, brt], [ps])
                    em.op("dve", lambda g, ps=ps, t=t: g.tensor_copy(out=lg[:, t, :], in_=ps[:, 0:NE]), reads=[ps], writes=[lg], shared=True)
            probe_ind("before routing")
            nm = em.tile("nm", [128, 1]); e4 = em.tile("e4", [128, 4]); es = em.tile("es", [128, 1])
            for t in range(NTX):
                em.op("dve", lambda g, t=t: g.max(out=top8[:, t, :], in_=lg[:, t, :]), reads=[lg], writes=[top8], shared=True)
                em.op("dve", lambda g, t=t: g.tensor_scalar(out=mask_all[:, t, :], in0=lg[:, t, :], scalar1=top8[:, t, 3:4], scalar2=None, op0=ALU.is_ge),
                      reads=[lg, top8], writes=[mask_all], shared=True)
                em.op("dve", lambda g, t=t: g.tensor_scalar(out=nm[:], in0=top8[:, t, 0:1], scalar1=-1.0, scalar2=None, op0=ALU.mult), reads=[top8], writes=[nm])
                em.op("act", lambda g, t=t: g.activation(out=e4[:], in_=top8[:, t, 0:4], func=AF.Exp, bias=nm[:, 0:1], accum_out=es[:]), reads=[top8, nm], writes=[e4, es])
                em.op("dve", lambda g: g.reciprocal(out=es[:], in_=es[:]), reads=[es], writes=[es])
                em.op("dve", lambda g, t=t: g.tensor_scalar(out=gates[:, t, :], in0=e4[:], scalar1=es[:, 0:1], scalar2=None, op0=ALU.mult), reads=[e4, es], writes=[gates], shared=True)
                ps = nextps()
                em.mm(ps[:, 0:NE], SLT[:], mask_all[:, t, :], True, t == 0, [SLT, mask_all], [ps])
                for tp in range(t):
                    em.mm(ps[:, 0:NE], ones[:], mask_all[:, tp, :], False, tp == t - 1, [ones, mask_all], [ps])
                em.op("dve", lambda g, ps=ps, t=t: g.tensor_copy(out=rank[:, t, :], in_=ps[:, 0:NE]), reads=[ps], writes=[rank], shared=True)
            cntb = em.tile("cntb", [128, NE]); nblk = em.tile("nblk", [128, NE]); cmp = em.tile("cmp", [128, NE]); pend = em.tile("pend", [128, NE]); pst = em.tile("pst", [128, NE])
            ps = nextps()
            for t in range(NTX):
                em.mm(ps[:, 0:NE], ones[:], mask_all[:, t, :], t == 0, t == NTX - 1, [ones, mask_all], [ps])
            em.op("dve", lambda g: g.tensor_copy(out=cntb[:], in_=ps[:, 0:NE]), reads=[ps], writes=[cntb])
            em.op("dve", lambda g: g.tensor_scalar(out=nblk[:], in0=cntb[:], scalar1=0.5, scalar2=None, op0=ALU.is_gt), reads=[cntb], writes=[nblk])
            for m in range(1, 16):
                em.op("dve", lambda g, m=m: g.tensor_scalar(out=cmp[:], in0=cntb[:], scalar1=128.0 * m + 0.5, scalar2=None, op0=ALU.is_gt), reads=[cntb], writes=[cmp])
                em.op("dve", lambda g: g.tensor_tensor(out=nblk[:], in0=nblk[:], in1=cmp[:], op=ALU.add), reads=[nblk, cmp], writes=[nblk])
            em.op("dve", lambda g: g.tensor_tensor_scan(out=pend[:], data0=ones[:, 0:NE], data1=nblk[:], initial=0.0, op0=ALU.mult, op1=ALU.add), reads=[ones, nblk], writes=[pend])
            em.op("dve", lambda g: g.tensor_tensor(out=pst[:], in0=pend[:], in1=nblk[:], op=ALU.subtract), reads=[pend, nblk], writes=[pst])
            em.op("dve", lambda g: g.tensor_scalar(out=pst[:], in0=pst[:], scalar1=128.0, scalar2=None, op0=ALU.mult), reads=[pst], writes=[pst])
            em.op("dve", lambda g: g.tensor_scalar(out=pend[:], in0=pend[:], scalar1=128.0, scalar2=None, op0=ALU.mult), reads=[pend], writes=[pend])
            em.op("dve", lambda g: g.tensor_tensor(out=rank[:], in0=rank[:], in1=pst[:].unsqueeze(1).to_broadcast([128, NTX, NE]), op=ALU.add), reads=[rank, pst], writes=[rank])
            destf = em.tile("destf", [128, NTX, TOPK]); eqt = em.tile("eqt", [128, NE])
            for t in range(NTX):
                for k in range(TOPK):
                    em.op("dve", lambda g, t=t, k=k: g.tensor_scalar(out=eqt[:], in0=lg[:, t, :], scalar1=top8[:, t, k:k + 1], scalar2=None, op0=ALU.is_equal), reads=[lg, top8], writes=[eqt])
                    em.op("dve", lambda g, t=t: g.tensor_tensor(out=eqt[:], in0=eqt[:], in1=rank[:, t, :], op=ALU.mult), reads=[eqt, rank], writes=[eqt])
                    em.op("dve", lambda g, t=t, k=k: g.reduce_sum(out=destf[:, t, k:k + 1], in_=eqt[:], axis=mybir.AxisListType.X), reads=[eqt], writes=[destf], shared=True)
            em.op("dve", lambda g: g.tensor_copy(out=desti[:], in_=destf[:].rearrange("p t k -> p (t k)")), reads=[destf], writes=[desti])
            jv = em.tile("jv", [128, NBLK]); bexp = em.tile("bexp", [128, NBLK]); cmpj = em.tile("cmpj", [128, NBLK]); pio = em.tile("pio", [128, 1])
            em.op("pool", lambda g: g.iota(jv[:], pattern=[[128, NBLK]], base=0, channel_multiplier=0, allow_small_or_imprecise_dtypes=True), writes=[jv])
            em.op("pool", lambda g: g.iota(pio[:], pattern=[[0, 1]], base=0, channel_multiplier=1, allow_small_or_imprecise_dtypes=True), writes=[pio])
            em.op("pool", lambda g: g.memset(bexp[:], 0.0), writes=[bexp])
            for e_ in range(NE):
                em.op("dve", lambda g, e_=e_: g.tensor_scalar(out=cmpj[:], in0=jv[:], scalar1=pend[:, e_:e_ + 1], scalar2=None, op0=ALU.is_ge), reads=[jv, pend], writes=[cmpj])
                em.op("dve", lambda g: g.tensor_tensor(out=bexp[:], in0=bexp[:], in1=cmpj[:], op=ALU.add), reads=[bexp, cmpj], writes=[bexp])
            em.op("dve", lambda g: g.tensor_scalar(out=bexp[:], in0=bexp[:], scalar1=float(NE - 1), scalar2=None, op0=ALU.min), reads=[bexp], writes=[bexp])
            em.op("dve", lambda g: g.tensor_copy(out=bidx[:], in_=bexp[:]), reads=[bexp], writes=[bidx])
            em.op("dve", lambda g: g.tensor_scalar(out=bexp[:], in0=bexp[:], scalar1=128.0, scalar2=None, op0=ALU.mult), reads=[bexp], writes=[bexp])
            em.op("dve", lambda g: g.tensor_scalar(out=bexp[:], in0=bexp[:], scalar1=pio[:, 0:1], scalar2=None, op0=ALU.add), reads=[bexp, pio], writes=[bexp])
            bexp4 = em.tile("bexp4", [128, NBLK, 4])
            for quad in range(4):
                em.op("dve", lambda g, quad=quad: g.tensor_scalar(out=bexp4[:, :, quad], in0=bexp[:], scalar1=4.0, scalar2=float(quad), op0=ALU.mult, op1=ALU.add),
                      reads=[bexp], writes=[bexp4], shared=True)
            em.op("dve", lambda g: g.tensor_copy(out=widx[:], in_=bexp4[:].rearrange("p j q -> p (j q)")), reads=[bexp4], writes=[widx])
            if dbg:
                dump("lg", lg, [128, NTX, NE]); dump("destf", destf, [128, NTX, TOPK]); dump("gates", gates, [128, NTX, TOPK]); dump("bexp", bexp, [128, NBLK])
            probe_ind("before scatter")
            for t in range(NTX):
                em.dma("sp", lambda g, t=t: g.dma_start(out=xn2[:], in_=xn2_d[t * 128:(t + 1) * 128, :]), reads=[B_xn2], writes=[xn2])
                for k in range(TOPK):
                    probe_ind("pre-idxcol xn2", src=xn2)
                    ic = idxcol(desti, t * TOPK + k)
                    probe_ind("post-idxcol zt", ic_=ic)
                    probe_ind("post-idxcol xn2", src=xn2, ic_=ic)
                    em.dma("pool", lambda g, ic=ic: g.indirect_dma_start(out=xs_d[:, :], out_offset=bass.IndirectOffsetOnAxis(ap=ic[:, :], axis=0),
                                                                        in_=xn2[:, :], in_offset=None, bounds_check=bc_blk, oob_is_err=False),
                           reads=[xn2, ic], writes=[B_xs], shared=True)
        if upto <= 4:
            em.barrier()
            print("instructions:", em.ninstr)
            return nc

        with em.phase():
            w2 = [w.rearrange("(e p q4 ql) c -> (e p q4) (ql c)", p=128, q4=4, ql=4) for w in (w_gate, w_up, w_down)]
            bsrc = (b_gate, b_up, b_down)
            wq = [em.tile("wq%d" % i, [128, 4 * D]) for i in range(3)]
            xs_t = em.tile("xs_t", [128, D]); xsT = em.tile("xsT", [128, 16, 128]); actv = em.tile("actv", [128, D]); actT = em.tile("actT", [128, 16, 128])
            ysb = em.tile("ysb", [128, D]); gsb = em.tile("gsb", [128, 512]); usb = em.tile("usb", [128, 512]); sgm = em.tile("sgm", [128, 512])
            brow3 = [em.tile("brow3_%d" % i, [2, D]) for i in range(3)]
            wqi = 0
            for j in range(NBLK):
                em.dma("sp", lambda g, j=j: g.dma_start(out=xs_t[:], in_=xs_d[j * 128:(j + 1) * 128, :]), reads=[B_xs], writes=[xs_t])
                bic = idxcol(bidx, j)
                for i in range(3):
                    em.dma("pool", lambda g, i=i: g.indirect_dma_start(out=brow3[i][0:2, :], out_offset=None, in_=bsrc[i][:, :],
                                                                     in_offset=bass.IndirectOffsetOnAxis(ap=bic[0:2, :], axis=0),
                                                                     bounds_check=bc_b, oob_is_err=False), reads=[bic], writes=[brow3[i]])
                for qq in range(4):
                    ps = nextps()
                    for q4 in range(4):
                        q = qq * 4 + q4
                        em.tr(ps[:, q4 * 128:(q4 + 1) * 128], xs_t[:, q:D:16], ident, [xs_t], [ps])
                    for q4 in range(4):
                        q = qq * 4 + q4
                        em.op("act", lambda g, q=q, q4=q4, ps=ps: g.activation(out=xsT[:, q, :], in_=ps[:, q4 * 128:(q4 + 1) * 128], func=AF.Identity,
                                                                              scale=A2x[:, q:q + 1], bias=B2x[:, q:q + 1]), reads=[ps, A2x, B2x], writes=[xsT], shared=True)
                for quad in range(4):
                    wts = []
                    wic = idxcol(widx, j * 4 + quad)
                    for i in range(2):
                        wt = wq[wqi % 3]; wqi += 1
                        em.dma("pool", lambda g, wt=wt, i=i, j=j, quad=quad: g.indirect_dma_start(
                            out=wt[:, :], out_offset=None, in_=w2[i][:, :],
                            in_offset=bass.IndirectOffsetOnAxis(ap=wic[:, :], axis=0), bounds_check=bc_w, oob_is_err=False), reads=[wic], writes=[wt])
                        wts.append(wt)
                    for i in range(2):
                        for cg in range(4):
                            ps = PS[i * 4 + cg]
                            for ql in range(4):
                                q = quad * 4 + ql
                                em.mm(ps[:, :], xsT[:, q, :], wts[i][:, ql * D + cg * 512:ql * D + (cg + 1) * 512], q == 0, False, [xsT, wts[i]], [ps])
                for i in range(2):
                    for cg in range(4):
                        ps = PS[i * 4 + cg]
                        em.mm(ps[:, :], ones[0:1, 0:128], brow3[i][0:1, cg * 512:(cg + 1) * 512], False, True, [ones, brow3[i]], [ps])
                for cg in range(4):
                    cs_ = slice(cg * 512, (cg + 1) * 512)
                    em.op("dve", lambda g, cg=cg: g.tensor_scalar(out=gsb[:], in0=PS[cg][:, :], scalar1=LIMIT, scalar2=None, op0=ALU.min), reads=[PS[cg]], writes=[gsb])
                    em.op("act", lambda g: g.activation(out=sgm[:], in_=gsb[:], func=AF.Sigmoid, scale=ALPHA), reads=[gsb], writes=[sgm])
                    em.op("dve", lambda g, cg=cg: g.tensor_scalar(out=usb[:], in0=PS[4 + cg][:, :], scalar1=LIMIT, scalar2=-LIMIT, op0=ALU.min, op1=ALU.max), reads=[PS[4 + cg]], writes=[usb])
                    em.op("dve", lambda g: g.scalar_tensor_tensor(out=usb[:], in0=usb[:], scalar=1.0, in1=gsb[:], op0=ALU.add, op1=ALU.mult), reads=[usb, gsb], writes=[usb])
                    em.op("dve", lambda g, cs_=cs_: g.tensor_tensor(out=actv[:, cs_], in0=usb[:], in1=sgm[:], op=ALU.mult), reads=[usb, sgm], writes=[actv], shared=True)
                for qq in range(4):
                    ps = nextps()
                    for q4 in range(4):
                        q = qq * 4 + q4
                        em.tr(ps[:, q4 * 128:(q4 + 1) * 128], actv[:, q:D:16], ident, [actv], [ps])
                    for q4 in range(4):
                        q = qq * 4 + q4
                        evac(actT[:, q, :], ps[:, q4 * 128:(q4 + 1) * 128], [ps], [actT])
                for quad in range(4):
                    wt = wq[wqi % 3]; wqi += 1
                    wic = idxcol(widx, j * 4 + quad)
                    em.dma("pool", lambda g, wt=wt, j=j, quad=quad: g.indirect_dma_start(
                        out=wt[:, :], out_offset=None, in_=w2[2][:, :],
                        in_offset=bass.IndirectOffsetOnAxis(ap=wic[:, :], axis=0), bounds_check=bc_w, oob_is_err=False), reads=[wic], writes=[wt])
                    for cg in range(4):
                        ps = PS[cg]
                        for ql in range(4):
                            q = quad * 4 + ql
                            em.mm(ps[:, :], actT[:, q, :], wt[:, ql * D + cg * 512:ql * D + (cg + 1) * 512], q == 0, False, [actT, wt], [ps])
                for cg in range(4):
                    em.mm(PS[cg][:, :], ones[0:1, 0:128], brow3[2][0:1, cg * 512:(cg + 1) * 512], False, True, [ones, brow3[2]], [PS[cg]])
                    evac(ysb[:, cg * 512:(cg + 1) * 512], PS[cg][:, :], [PS[cg]], [ysb])
                em.dma("sp", lambda g, j=j: g.dma_start(out=ys_d[j * 128:(j + 1) * 128, :], in_=ysb[:]), reads=[ysb], writes=[B_ys], shared=True)

        with em.phase():
            gt2_bc = em.tile("gt2_bc", [128, D]); now_bc = em.tile("now_bc", [128, D])
            em.dma("sp", lambda g: g.dma_start(out=gt2_bc[:], in_=modrows[0, 5 * D:6 * D].partition_broadcast(128)), reads=[B_modrows], writes=[gt2_bc])
            em.dma("sp", lambda g: g.dma_start(out=now_bc[:], in_=now_d.partition_broadcast(128)), writes=[now_bc])
            yk = [em.tile("yk%d" % i, [128, D]) for i in range(4)]
            x1b = [em.tile("x1b%d" % i, [128, D]) for i in range(2)]; acc = em.tile("acc", [128, D]); junk = em.tile("junk5", [128, D]); ss = em.tile("ss5", [128, 1])
            ob5 = [em.tile("ob5_%d" % i, [128, D]) for i in range(2)]
            for t in range(NTX):
                xb = x1b[t % 2]; ob = ob5[t % 2]
                em.dma("sp", lambda g, xb=xb, t=t: g.dma_start(out=xb[:], in_=x1_d[t * 128:(t + 1) * 128, :]), reads=[B_x1], writes=[xb])
                for k in range(TOPK):
                    ic = idxcol(desti, t * TOPK + k)
                    em.dma("pool", lambda g, k=k, ic=ic: g.indirect_dma_start(out=yk[k][:, :], out_offset=None, in_=ys_d[:, :],
                                                                            in_offset=bass.IndirectOffsetOnAxis(ap=ic[:, :], axis=0),
                                                                            bounds_check=bc_blk, oob_is_err=False), reads=[B_ys, ic], writes=[yk[k]])
                em.op("dve", lambda g, t=t: g.tensor_scalar(out=acc[:], in0=yk[0][:], scalar1=gates[:, t, 0:1], scalar2=None, op0=ALU.mult), reads=[yk[0], gates], writes=[acc])
                for k in range(1, TOPK):
                    em.op("dve", lambda g, t=t, k=k: g.scalar_tensor_tensor(out=acc[:], in0=yk[k][:], scalar=gates[:, t, k:k + 1], in1=acc[:], op0=ALU.mult, op1=ALU.add),
                          reads=[yk[k], gates, acc], writes=[acc])
                em.op("dve", lambda g: g.tensor_tensor(out=acc[:], in0=acc[:], in1=gt2_bc[:], op=ALU.mult), reads=[acc, gt2_bc], writes=[acc])
                em.op("dve", lambda g, xb=xb: g.tensor_tensor(out=acc[:], in0=acc[:], in1=xb[:], op=ALU.add), reads=[acc, xb], writes=[acc])
                em.op("act", lambda g: g.activation(out=junk[:], in_=acc[:], func=AF.Square, accum_out=ss[:]), reads=[acc], writes=[junk, ss])
                rstd_from_ss(ss, 1, D)
                em.op("dve", lambda g, ob=ob: g.scalar_tensor_tensor(out=ob[:], in0=acc[:], scalar=ss[:, 0:1], in1=now_bc[:], op0=ALU.mult, op1=ALU.mult),
                      reads=[acc, ss, now_bc], writes=[ob])
                em.dma("sp", lambda g, ob=ob, t=t: g.dma_start(out=y_d[t * 128:(t + 1) * 128, :], in_=ob[:]), reads=[ob], writes=[B_y], shared=True)
        em.barrier()
        print("instructions:", em.ninstr)
    return nc


_W_KEYS = ["w_ada", "b_ada", "norm_mix_w", "w_in", "hg_lb_f", "hg_lb_b", "hg_norm_w", "gd_conv_w", "gd_a_log_f", "gd_a_log_b",
           "gd_dt_bias_f", "gd_dt_bias_b", "gd_norm_w", "w_out", "norm_ffn_w", "w_router", "b_router", "w_gate", "b_gate",
           "w_up", "b_up", "w_down", "b_down"]


def kernel(**inputs):
    f32 = lambda a: np.ascontiguousarray(np.asarray(a, dtype=np.float32))
    x = f32(inputs["x"]); c = f32(inputs["c"]); ctx = f32(inputs["ctx"])
    nb = x.shape[0]
    ne = int(np.asarray(inputs["w_router"]).shape[-1])
    shared = {"c_ctx": f32(inputs["c_ctx"]), "norm_out_w": f32(inputs["norm_out_w"])}
    for k in _W_KEYS:
        a = f32(inputs[k])[0]
        if k in ("w_gate", "w_up", "w_down"):
            a = a.reshape(ne * D, D)
        shared[k] = np.ascontiguousarray(a)
    shared["hg_lb_f"] = f32(inputs["hg_lb_f"]); shared["hg_lb_b"] = f32(inputs["hg_lb_b"])
    nc = build_program(NE=ne)
    in_maps = []
    for b in range(nb):
        m = dict(shared)
        m["x"] = x[b]; m["c"] = c[b]; m["ctx"] = ctx[b]
        in_maps.append(m)
    res = run_bass_kernel_spmd(nc, in_maps, core_ids=list(range(nb)))
    return np.stack([r["y"] for r in res.results], axis=0).astype(np.float32)
```

```python
import contextlib
import numpy as np
import concourse.bass as bass
import concourse.mybir as mybir
from concourse.bass_utils import run_bass_kernel_spmd

F32 = mybir.dt.float32
I32 = mybir.dt.int32
F32R = mybir.dt.float32r
AF = mybir.ActivationFunctionType
ALU = mybir.AluOpType

D = 2048
SEQ = 2048
CTX = 256
NTOK = SEQ + CTX
NT = NTOK // 128
NTX = SEQ // 128
HGW = 1024
GDW = 1024
NH = 8
IN_DIM = 9248
TOPK = 4
EPS = 1e-6
LIMIT = 7.0
ALPHA = 1.702


class Sem:
    def __init__(self, handle, name):
        self.h = handle
        self.name = name
        self.count = 0


class Buf:
    def __init__(self, name):
        self.name = name
        self.ws = {}
        self.r = {}
        self.ld = None
        self.st = None


class T(Buf):
    def __init__(self, em, name, shape, dtype=F32, psum=False):
        super().__init__(name)
        self.is_tile = True
        self.is_psum = psum
        if psum:
            self.t = em.stack.enter_context(em.nc.psum_tensor(name, list(shape), dtype))
        else:
            self.t = em.stack.enter_context(em.nc.sbuf_tensor(name, list(shape), dtype))

    def __getitem__(self, idx):
        return self.t[idx]


class Emitter:
    def __init__(self, nc, stack, n_dma_sems=88):
        self.nc = nc
        self.gstack = stack
        self.stack = stack
        self.eng = {"pe": nc.tensor, "act": nc.scalar, "dve": nc.vector, "pool": nc.gpsimd, "sp": nc.sync}
        self.esem = {k: Sem(stack.enter_context(nc.semaphore("e_" + k)), k) for k in self.eng}
        self.seen = {k: {} for k in self.eng}
        self.pool = [Sem(stack.enter_context(nc.semaphore("d%d" % i)), "d%d" % i) for i in range(n_dma_sems)]
        self.used = []
        self.ninstr = 0
        self.phase_tiles = []
        self.log = None

    def get_sem(self):
        s = self.pool.pop()
        self.used.append(s)
        return s

    def tile(self, name, shape, dtype=F32):
        t = T(self, name, shape, dtype)
        self.phase_tiles.append(t)
        return t

    def psum(self, name, shape, dtype=F32):
        return T(self, name, shape, dtype, psum=True)

    def _waits(self, e, reads, writes, shared=False):
        need = {}

        def add(tk):
            if tk is None:
                return
            sem, val, is_dma = tk
            if is_dma:
                val = sem.count
            if e == "pe" and sem is self.esem["pe"]:
                return
            if need.get(sem, 0) < val:
                need[sem] = val

        for b in reads:
            for tk in b.ws.values():
                add(tk)
            if getattr(b, "is_psum", False):
                for tk in b.r.values():
                    if tk[0] is not self.esem.get(e):
                        add(tk)
        for b in writes:
            if not shared:
                for tk in b.ws.values():
                    add(tk)
            for tk in b.r.values():
                add(tk)
        for sem, val in need.items():
            if self.seen[e].get(sem, 0) < val:
                self.eng[e].wait_ge(sem.h, val)
                self.seen[e][sem] = val
                self.ninstr += 1
                if self.log is not None:
                    self.log.append((e, "wait", sem.name, val))

    def _record(self, tk, reads, writes, shared):
        sem = tk[0]
        for b in reads:
            b.r[sem] = tk
        for b in writes:
            if shared:
                b.ws[sem] = tk
            else:
                b.ws = {sem: tk}
                b.r = {}

    def op(self, e, fn, reads=(), writes=(), shared=False):
        self._waits(e, reads, writes, shared)
        ins = fn(self.eng[e])
        sem = self.esem[e]
        sem.count += 1
        ins.then_inc(sem.h, 1)
        tk = (sem, sem.count, False)
        if self.log is not None:
            self.log.append((e, "inc", sem.name, 1))
        self._record(tk, reads, writes, shared)
        self.ninstr += 1
        return ins

    def dma(self, q, fn, reads=(), writes=(), shared=False):
        self._waits(q, reads, writes, shared)
        sem = None
        for b in writes:
            if isinstance(b, T):
                if b.ld is None:
                    b.ld = self.get_sem()
                sem = b.ld
                break
        if sem is None:
            for b in reads:
                if isinstance(b, T):
                    if b.st is None:
                        b.st = self.get_sem()
                    sem = b.st
                    break
        assert sem is not None
        ins = fn(self.eng[q])
        sem.count += 16
        ins.then_inc(sem.h, 16)
        tk = (sem, sem.count, True)
        if self.log is not None:
            self.log.append((q, "inc", sem.name, 16))
        self._record(tk, reads, writes, shared)
        self.ninstr += 1
        return ins

    def barrier(self):
        sems = list(self.esem.values()) + list(self.used)
        for e in self.eng:
            for s in sems:
                if s is self.esem[e] or s.count == 0:
                    continue
                if self.seen[e].get(s, 0) < s.count:
                    self.eng[e].wait_ge(s.h, s.count)
                    self.seen[e][s] = s.count
                    self.ninstr += 1
                    if self.log is not None:
                        self.log.append((e, "wait", s.name, s.count))

    @contextlib.contextmanager
    def phase(self):
        old_stack, old_tiles = self.stack, self.phase_tiles
        st = contextlib.ExitStack()
        self.stack = st
        self.phase_tiles = []
        try:
            with st:
                yield
                self.barrier()
                for t in self.phase_tiles:
                    for s in (t.ld, t.st):
                        if s is not None:
                            self.used.remove(s)
                            self.pool.append(s)
        finally:
            self.stack = old_stack
            self.phase_tiles = old_tiles

    def mm(self, out, lhsT, rhs, start, stop, reads, writes, shared=False, r=False):
        if r:
            if lhsT.dtype != F32R:
                lhsT = lhsT.bitcast(F32R)
            if rhs.dtype != F32R:
                rhs = rhs.bitcast(F32R)
        return self.op("pe", lambda g: g.matmul(out, lhsT, rhs, start=start, stop=stop), reads, writes, shared)

    def tr(self, out, in_, ident, reads, writes, shared=False, k=128):
        return self.op("pe", lambda g: g.transpose(out, in_, ident[0:k, 0:k]), list(reads) + [ident], writes, shared)


def build_program(NE=32, dbg=False, upto=99, nhg=NH, ngd=NH):
    NBLK = (SEQ * TOPK) // 128 + NE
    nc = bass.Bass("TRN2", target_bir_lowering=False)

    def din(name, shape, dt=F32):
        return nc.dram_tensor(name, list(shape), dt, kind="ExternalInput").ap()

    def dscr(name, shape, dt=F32, out=False):
        return nc.dram_tensor(name, list(shape), dt, kind="ExternalOutput" if (out or dbg) else "Internal").ap()

    x_d = din("x", [SEQ, D]); ctx_d = din("ctx", [CTX, D]); c_d = din("c", [D]); cctx_d = din("c_ctx", [D])
    w_ada = din("w_ada", [D, 6 * D], F32R); b_ada = din("b_ada", [6 * D], F32R); nmw_d = din("norm_mix_w", [D])
    w_in = din("w_in", [D, IN_DIM], F32R); lbf_d = din("hg_lb_f", [2, HGW]); lbb_d = din("hg_lb_b", [2, HGW])
    hgnw_d = din("hg_norm_w", [128]); conv_d = din("gd_conv_w", [3, 3 * GDW])
    alf_d = din("gd_a_log_f", [NH]); alb_d = din("gd_a_log_b", [NH])
    dtf_d = din("gd_dt_bias_f", [NH]); dtb_d = din("gd_dt_bias_b", [NH]); gdnw_d = din("gd_norm_w", [128])
    w_out = din("w_out", [D, D], F32R); nfw_d = din("norm_ffn_w", [D]); w_rt = din("w_router", [D, NE])
    b_rt = din("b_router", [NE]); w_gate = din("w_gate", [NE * D, D], F32R); b_gate = din("b_gate", [NE, D], F32R)
    w_up = din("w_up", [NE * D, D], F32R); b_up = din("b_up", [NE, D], F32R); w_down = din("w_down", [NE * D, D], F32R)
    b_down = din("b_down", [NE, D], F32R); now_d = din("norm_out_w", [D])
    y_d = nc.dram_tensor("y", [SEQ, D], F32, kind="ExternalOutput").ap()

    modrows = dscr("modrows", [2, 6 * D])
    pfm = dscr("pfm", [6144, NTOK])
    ptm = dscr("ptm", [NTOK, 3104])
    mixT = dscr("mixT", [D, SEQ], F32R)
    x1_d = dscr("x1", [SEQ, D])
    xn2_d = dscr("xn2", [SEQ, D])
    xs_d = dscr("xs", [NBLK * 128, D])
    ys_d = dscr("ys", [NBLK * 128, D])
    B_modrows = Buf("modrows"); B_pfm = Buf("pfm"); B_ptm = Buf("ptm"); B_mixT = Buf("mixT")
    B_x1 = Buf("x1"); B_xn2 = Buf("xn2"); B_xs = Buf("xs"); B_ys = Buf("ys"); B_y = Buf("y")

    gst = contextlib.ExitStack()
    with gst:
        em = Emitter(nc, gst)
        em.log = [] if dbg else None
        nc._em = em
        PS = [em.psum("ps%d" % i, [128, 512]) for i in range(8)]
        ident = em.tile("ident", [128, 128]); ones = em.tile("ones", [128, 128])
        em.op("pool", lambda g: g.memset(ones[:], 1.0), writes=[ones])
        ones_r = em.tile("ones_r", [1, 128])
        em.op("dve", lambda g: g.tensor_copy(out=ones_r[:].bitcast(F32R), in_=ones[0:1, :]), reads=[ones], writes=[ones_r])

        def aff_mask(out_t, cmp, sgn=1):
            em.op("pool", lambda g: g.affine_select(out=out_t[:], in_=ones[:], pattern=[[-sgn, 128]], compare_op=cmp,
                                                     fill=0.0, base=0, channel_multiplier=sgn), reads=[ones], writes=[out_t])

        aff_mask(ident, ALU.is_equal)
        cnt = {"rr": 0}
        dumps = {}

        def dump(name, tl, shape):
            if not dbg:
                return
            d = nc.dram_tensor("dbg_" + name, list(shape), F32, kind="ExternalOutput").ap()
            em.dma("sp", lambda g: g.dma_start(out=d, in_=tl[:]), reads=[tl], writes=[Buf("dbg_" + name)])

        def evac(out_ap, in_ap, reads, writes):
            cnt["rr"] += 1
            if cnt["rr"] % 2:
                em.op("act", lambda g: g.copy(out=out_ap, in_=in_ap), reads, writes)
            else:
                em.op("dve", lambda g: g.tensor_copy(out=out_ap, in_=in_ap), reads, writes)

        def rstd_from_ss(ss, n, width):
            em.op("dve", lambda g: g.tensor_scalar(out=ss[:, 0:n], in0=ss[:, 0:n], scalar1=1.0 / width, scalar2=EPS,
                                                   op0=ALU.mult, op1=ALU.add), reads=[ss], writes=[ss])
            em.op("act", lambda g: g.activation(out=ss[:, 0:n], in_=ss[:, 0:n], func=AF.Sqrt), reads=[ss], writes=[ss])
            em.op("dve", lambda g: g.reciprocal(out=ss[:, 0:n], in_=ss[:, 0:n]), reads=[ss], writes=[ss])

        A1x = em.tile("A1x", [128, 16]); B1x = em.tile("B1x", [128, 16])
        A1c = em.tile("A1c", [128, 16]); B1c = em.tile("B1c", [128, 16])
        A2x = em.tile("A2x", [128, 16]); B2x = em.tile("B2x", [128, 16])

        with em.phase():
            cs = em.tile("cs", [128, 16, 2]); craw = em.tile("craw", [128, 2, 16])
            em.dma("sp", lambda g: g.dma_start(out=craw[:, 0, :], in_=c_d.rearrange("(p q) -> p q", q=16)), writes=[craw], shared=True)
            em.dma("sp", lambda g: g.dma_start(out=craw[:, 1, :], in_=cctx_d.rearrange("(p q) -> p q", q=16)), writes=[craw], shared=True)
            for r in range(2):
                em.op("act", lambda g, r=r: g.activation(out=cs[:, :, r].bitcast(F32R), in_=craw[:, r, :], func=AF.Silu), reads=[craw], writes=[cs], shared=True)
            brow = em.tile("brow", [1, 6 * D], F32R)
            em.dma("pool", lambda g: g.dma_start(out=brow[:], in_=b_ada.rearrange("(o n) -> o n", o=1)), writes=[brow])
            wv = w_ada.rearrange("(p q) c -> p q c", q=16)
            wring = [em.tile("adaw%d" % i, [128, 16, 512], F32R) for i in range(3)]
            mrow = [em.tile("mrow%d" % i, [2, 512]) for i in range(2)]
            for s in range(24):
                wt = wring[s % 3]
                em.dma("pool", lambda g, wt=wt, s=s: g.dma_start(out=wt[:], in_=wv[:, :, s * 512:(s + 1) * 512]), writes=[wt])
                ps = PS[s % 2]
                for q in range(16):
                    em.mm(ps[0:2, :], cs[:, q, :], wt[:, q, :], q == 0, False, reads=[cs, wt], writes=[ps], r=True)
                em.mm(ps[0:2, :], ones_r[0:1, 0:2], brow[0:1, s * 512:(s + 1) * 512], False, True, reads=[ones_r, brow], writes=[ps], r=True)
                mr = mrow[s % 2]
                evac(mr[:], ps[0:2, :], [ps], [mr])
                em.dma("sp", lambda g, mr=mr, s=s: g.dma_start(out=modrows[:, s * 512:(s + 1) * 512], in_=mr[:]), reads=[mr], writes=[B_modrows], shared=True)
            tmp = em.tile("modtmp", [128, 6, 16]); nw = em.tile("nw", [128, 2, 16])
            em.dma("sp", lambda g: g.dma_start(out=nw[:, 0, :], in_=nmw_d.rearrange("(p q) -> p q", q=16)), writes=[nw], shared=True)
            em.dma("sp", lambda g: g.dma_start(out=nw[:, 1, :], in_=nfw_d.rearrange("(p q) -> p q", q=16)), writes=[nw], shared=True)

            def col(row, chunk, dst):
                em.dma("sp", lambda g: g.dma_start(out=dst, in_=modrows[row, chunk * D:(chunk + 1) * D].rearrange("(p q) -> p q", q=16)),
                       reads=[B_modrows], writes=[tmp], shared=True)

            col(0, 0, tmp[:, 0, :]); col(0, 1, tmp[:, 1, :]); col(1, 0, tmp[:, 2, :]); col(1, 1, tmp[:, 3, :])
            col(0, 3, tmp[:, 4, :]); col(0, 4, tmp[:, 5, :])

            def mkA(dst, sc_idx, nwi):
                em.op("dve", lambda g: g.scalar_tensor_tensor(out=dst[:], in0=tmp[:, sc_idx, :], scalar=1.0, in1=nw[:, nwi, :],
                                                             op0=ALU.add, op1=ALU.mult), reads=[tmp, nw], writes=[dst])

            mkA(A1x, 1, 0); mkA(A1c, 3, 0); mkA(A2x, 5, 1)
            em.op("dve", lambda g: g.tensor_copy(out=B1x[:], in_=tmp[:, 0, :]), reads=[tmp], writes=[B1x])
            em.op("dve", lambda g: g.tensor_copy(out=B1c[:], in_=tmp[:, 2, :]), reads=[tmp], writes=[B1c])
            em.op("dve", lambda g: g.tensor_copy(out=B2x[:], in_=tmp[:, 4, :]), reads=[tmp], writes=[B2x])

        FM = [(0, 0), (512, 512), (1024, 1024), (1536, 1536), (2048, 2048), (2560, 2560),
              (5120, 3072), (5632, 3584), (6144, 4096), (6656, 4608), (7168, 5120), (7680, 5632)]
        TM = [(3072, 0, 512), (3584, 512, 512), (4096, 1024, 512), (4608, 1536, 512),
              (8192, 2048, 512), (8704, 2560, 512), (9216, 3072, 32)]
        with em.phase():
            wv = w_in.rearrange("(p q) c -> p q c", q=16)
            hxT = em.tile("hxT", [128, 16, 512])
            xt2 = [em.tile("xt%d" % i, [128, D]) for i in range(2)]
            xn = em.tile("xn", [128, D]); junk = em.tile("junk1", [128, D]); ss = em.tile("ss1", [128, 1])
            wring = [em.tile("winw%d" % i, [128, 16, 512], F32R) for i in range(3)]
            ob = [em.tile("ob%d" % i, [128, 512]) for i in range(4)]
            groups = [(0, 2, ctx_d, A1c, B1c)] + [(2 + 4 * g, 4, x_d, A1x, B1x) for g in range(4)]
            ti = 0; wi = 0; oi = 0; pi = 0
            for (t0, ntile, src, A1, B1) in groups:
                ntok = ntile * 128
                for j in range(ntile):
                    row0 = (t0 + j) * 128 - (0 if src is ctx_d else CTX)
                    xt = xt2[ti % 2]; ti += 1
                    em.dma("sp", lambda g, xt=xt, row0=row0, src=src: g.dma_start(out=xt[:], in_=src[row0:row0 + 128, :]), writes=[xt])
                    em.op("act", lambda g, xt=xt: g.activation(out=junk[:], in_=xt[:], func=AF.Square, accum_out=ss[:]), reads=[xt], writes=[junk, ss])
                    rstd_from_ss(ss, 1, D)
                    em.op("dve", lambda g, xt=xt: g.tensor_scalar(out=xn[:], in0=xt[:], scalar1=ss[:, 0:1], scalar2=None, op0=ALU.mult),
                          reads=[xt, ss], writes=[xn])
                    for qq in range(4):
                        ps = PS[pi % 8]; pi += 1
                        for q4 in range(4):
                            q = qq * 4 + q4
                            em.tr(ps[:, q4 * 128:(q4 + 1) * 128], xn[:, q:D:16], ident, [xn], [ps])
                        for q4 in range(4):
                            q = qq * 4 + q4
                            em.op("act", lambda g, q=q, q4=q4, ps=ps, j=j, A1=A1, B1=B1: g.activation(
                                out=hxT[:, q, j * 128:(j + 1) * 128].bitcast(F32R), in_=ps[:, q4 * 128:(q4 + 1) * 128], func=AF.Identity,
                                scale=A1[:, q:q + 1], bias=B1[:, q:q + 1]), reads=[ps, A1, B1], writes=[hxT], shared=True)
                tok0 = t0 * 128
                for (wc, prow) in FM:
                    wt = wring[wi % 3]; wi += 1
                    em.dma("pool", lambda g, wt=wt, wc=wc: g.dma_start(out=wt[:], in_=wv[:, :, wc:wc + 512]), writes=[wt])
                    for sub in range(4):
                        ps = PS[pi % 8]; pi += 1
                        for q in range(16):
                            em.mm(ps[:, 0:ntok], wt[:, q, sub * 128:(sub + 1) * 128], hxT[:, q, 0:ntok], q == 0, q == 15, [wt, hxT], [ps], r=True)
                        o = ob[oi % 4]; oi += 1
                        evac(o[:, 0:ntok], ps[:, 0:ntok], [ps], [o])
                        em.dma("sp", lambda g, o=o, prow=prow, sub=sub, tok0=tok0, ntok=ntok: g.dma_start(
                            out=pfm[prow + sub * 128:prow + (sub + 1) * 128, tok0:tok0 + ntok], in_=o[:, 0:ntok]), reads=[o], writes=[B_pfm], shared=True)
                for (wc, pcol, wd) in TM:
                    wt = wring[wi % 3]; wi += 1
                    em.dma("pool", lambda g, wt=wt, wc=wc, wd=wd: g.dma_start(out=wt[:, :, 0:wd], in_=wv[:, :, wc:wc + wd]), writes=[wt])
                    for j in range(ntile):
                        ps = PS[pi % 8]; pi += 1
                        for q in range(16):
                            em.mm(ps[:, 0:wd], hxT[:, q, j * 128:(j + 1) * 128], wt[:, q, 0:wd], q == 0, q == 15, [wt, hxT], [ps], r=True)
                        o = ob[oi % 4]; oi += 1
                        evac(o[:, 0:wd], ps[:, 0:wd], [ps], [o])
                        em.dma("sp", lambda g, o=o, pcol=pcol, wd=wd, r0=tok0 + j * 128: g.dma_start(
                            out=ptm[r0:r0 + 128, pcol:pcol + wd], in_=o[:, 0:wd]), reads=[o], writes=[B_ptm], shared=True)

        if upto <= 1:
            em.barrier()
            print("instructions:", em.ninstr)
            return nc

        pcnt = {"i": 0}

        def nextps():
            pcnt["i"] += 1
            return PS[pcnt["i"] % 8]

        MLE = em.tile("MLE", [128, 128]); MGE = em.tile("MGE", [128, 128])
        aff_mask(MLE, ALU.is_ge, -1); aff_mask(MGE, ALU.is_ge, 1)

        def gated_norm_store(o_acc, g_tm, w_bc, feat0, jk, ssn, t1, sg, mT_all):
            for n in range(NTX):
                em.op("act", lambda g, n=n: g.activation(out=jk[:], in_=o_acc[:, n, :], func=AF.Square, accum_out=ssn[:, n:n + 1]),
                      reads=[o_acc], writes=[jk, ssn], shared=True)
            rstd_from_ss(ssn, NTX, 128)
            for n in range(NTX):
                em.op("dve", lambda g, n=n: g.scalar_tensor_tensor(out=t1[:], in0=o_acc[:, n, :], scalar=ssn[:, n:n + 1], in1=w_bc[:],
                                                                  op0=ALU.mult, op1=ALU.mult), reads=[o_acc, ssn, w_bc], writes=[t1])
                em.op("act", lambda g, n=n: g.activation(out=sg[:], in_=g_tm[:, n, :], func=AF.Silu), reads=[g_tm], writes=[sg])
                em.op("dve", lambda g: g.tensor_tensor(out=t1[:], in0=t1[:], in1=sg[:], op=ALU.mult), reads=[t1, sg], writes=[t1])
                ps = nextps()
                em.tr(ps[:, 0:128], t1[:], ident, [t1], [ps])
                evac(mT_all[:, n * 128:(n + 1) * 128], ps[:, 0:128], [ps], [mT_all])
            em.dma("pool", lambda g: g.dma_start(out=mixT[feat0:feat0 + 128, :], in_=mT_all[:]), reads=[mT_all], writes=[B_mixT], shared=True)

        with em.phase():
            lbt = em.tile("lbt", [128, 2, 2, 8]); lb = em.tile("lb", [128, 2, 8]); oml = em.tile("oml", [128, 2, 8])
            lbrow = em.tile("lbrow", [32, 128])
            for di, src in enumerate((lbf_d, lbb_d)):
                em.dma("sp", lambda g, di=di, src=src: g.dma_start(out=lbrow[di * 16:(di + 1) * 16, :], in_=src.rearrange("l (h p) -> (l h) p", p=128)),
                       writes=[lbrow], shared=True)
            ps = nextps()
            em.tr(ps[:, 0:32], lbrow[:], ident, [lbrow], [ps], k=32)
            em.op("dve", lambda g: g.tensor_copy(out=lbt[:].rearrange("p d l h -> p (d l h)"), in_=ps[:, 0:32]), reads=[ps], writes=[lbt])
            em.op("dve", lambda g: g.tensor_tensor(out=lb[:], in0=lbt[:, :, 0, :], in1=lbt[:, :, 1, :], op=ALU.subtract), reads=[lbt], writes=[lb])
            em.op("act", lambda g: g.activation(out=lb[:], in_=lb[:], func=AF.Sigmoid), reads=[lb], writes=[lb])
            em.op("dve", lambda g: g.tensor_scalar(out=oml[:], in0=lb[:], scalar1=-1.0, scalar2=1.0, op0=ALU.mult, op1=ALU.add), reads=[lb], writes=[oml])
            hgw_bc = em.tile("hgw_bc", [128, 128])
            em.dma("sp", lambda g: g.dma_start(out=hgw_bc[:], in_=hgnw_d.partition_broadcast(128)), writes=[hgw_bc])
            rst = em.tile("rst", [128, NTOK])
            em.op("pool", lambda g: g.memset(rst[:], 1.0), writes=[rst])
            em.op("pool", lambda g: g.memset(rst[:, 0:NTOK:128], 0.0), writes=[rst])
            qT = em.tile("qT", [128, NTOK]); zz = em.tile("zz", [128, NTOK]); v_tm = em.tile("v_tm", [128, NT, 128])
            g_tm = em.tile("g_tm", [128, NTX, 128])
            Ft = em.tile("Ft", [128, NTOK]); LF = em.tile("LF", [128, NTOK]); Kt = em.tile("Kt", [128, NTOK])
            CUM = em.tile("CUM", [128, NTOK]); CUMB = em.tile("CUMB", [128, NTOK]); At = em.tile("At", [128, NTOK])
            EQ = em.tile("EQ", [128, NTOK]); EK = em.tile("EK", [128, NTOK]); QD = em.tile("QD", [128, NTOK])
            dec = em.tile("dec", [128, NT]); gend = em.tile("gend", [128, NT])
            S2 = [em.tile("S%d" % i, [128, 128]) for i in range(2)]
            scT = [em.tile("scT%d" % i, [128, 128]) for i in range(2)]; kit = [em.tile("kit%d" % i, [128, 128]) for i in range(2)]
            o_acc = em.tile("o_acc", [128, NTX, 128]); mT_all = em.tile("mT_all", [128, SEQ], F32R)
            jk = em.tile("jk", [128, 128]); ssn = em.tile("ssn", [128, NTX]); t1 = em.tile("t1", [128, 128]); sg = em.tile("sg", [128, 128])
            v3 = lambda t: t[:].rearrange("p (n w) -> p n w", w=128)
            for h in range(nhg):
                em.dma("sp", lambda g, h=h: g.dma_start(out=qT[:], in_=pfm[h * 128:(h + 1) * 128, :]), reads=[B_pfm], writes=[qT])
                em.dma("pool", lambda g, h=h: g.dma_start(out=v_tm[:], in_=ptm[:, h * 128:(h + 1) * 128].rearrange("(n p) e -> p n e", p=128)),
                       reads=[B_ptm], writes=[v_tm])
                em.dma("pool", lambda g, h=h: g.dma_start(out=g_tm[:], in_=ptm[CTX:NTOK, 1024 + h * 128:1024 + (h + 1) * 128].rearrange("(n p) e -> p n e", p=128)),
                       reads=[B_ptm], writes=[g_tm])
                for di in range(2):
                    zrow = 1024 + di * 1024 + h * 128
                    em.dma("sp", lambda g, zrow=zrow: g.dma_start(out=zz[:], in_=pfm[zrow:zrow + 128, :]), reads=[B_pfm], writes=[zz])
                    em.op("act", lambda g: g.activation(out=Ft[:], in_=zz[:], func=AF.Sigmoid), reads=[zz], writes=[Ft])
                    em.op("dve", lambda g, di=di, h=h: g.tensor_scalar(out=Ft[:], in0=Ft[:], scalar1=oml[:, di, h:h + 1], scalar2=lb[:, di, h:h + 1],
                                                                       op0=ALU.mult, op1=ALU.add), reads=[Ft, oml, lb], writes=[Ft])
                    em.op("act", lambda g: g.activation(out=LF[:], in_=Ft[:], func=AF.Ln), reads=[Ft], writes=[LF])
                    em.op("dve", lambda g: g.tensor_scalar(out=Kt[:], in0=Ft[:], scalar1=-1.0, scalar2=1.0, op0=ALU.mult, op1=ALU.add), reads=[Ft], writes=[Kt])
                    em.op("dve", lambda g: g.tensor_tensor_scan(out=CUM[:], data0=rst[:], data1=LF[:], initial=0.0, op0=ALU.mult, op1=ALU.add),
                          reads=[rst, LF], writes=[CUM])
                    if di == 0:
                        cum = CUM; last = 127
                    else:
                        em.op("dve", lambda g: g.tensor_tensor(out=CUMB[:], in0=LF[:], in1=CUM[:], op=ALU.subtract), reads=[LF, CUM], writes=[CUMB])
                        em.op("dve", lambda g: g.tensor_tensor(out=v3(CUMB), in0=v3(CUMB), in1=v3(CUM)[:, :, 127:128].to_broadcast([128, NT, 128]), op=ALU.add),
                              reads=[CUMB, CUM], writes=[CUMB])
                        cum = CUMB; last = 0
                    em.op("dve", lambda g, cum=cum: g.tensor_tensor(out=v3(At), in0=v3(cum), in1=v3(cum)[:, :, 64:65].to_broadcast([128, NT, 128]), op=ALU.subtract),
                          reads=[cum], writes=[At])
                    em.op("act", lambda g, last=last: g.activation(out=gend[:], in_=v3(At)[:, :, last], func=AF.Exp), reads=[At], writes=[gend])
                    em.op("act", lambda g, cum=cum, last=last: g.activation(out=dec[:], in_=v3(cum)[:, :, last], func=AF.Exp), reads=[cum], writes=[dec])
                    em.op("act", lambda g: g.activation(out=EQ[:], in_=At[:], func=AF.Exp), reads=[At], writes=[EQ])
                    em.op("dve", lambda g: g.tensor_tensor(out=EQ[:], in0=EQ[:], in1=qT[:], op=ALU.mult), reads=[EQ, qT], writes=[EQ])
                    em.op("act", lambda g: g.activation(out=EK[:], in_=At[:], func=AF.Exp, scale=-1.0), reads=[At], writes=[EK])
                    em.op("dve", lambda g: g.tensor_tensor(out=EK[:], in0=EK[:], in1=Kt[:], op=ALU.mult), reads=[EK, Kt], writes=[EK])
                    em.op("act", lambda g, cum=cum: g.activation(out=QD[:], in_=cum[:], func=AF.Exp), reads=[cum], writes=[QD])
                    em.op("dve", lambda g: g.tensor_tensor(out=QD[:], in0=QD[:], in1=qT[:], op=ALU.mult), reads=[QD, qT], writes=[QD])
                    order = [0, 1] + list(range(2, NT)) if di == 0 else [1, 0] + list(range(NT - 1, 1, -1))
                    MASK = MLE if di == 0 else MGE
                    si = 0
                    em.op("pool", lambda g: g.memset(S2[0][:], 0.0), writes=[S2[0]])
                    for it, n in enumerate(order):
                        Sc = S2[si % 2]; Sn = S2[(si + 1) % 2]; si += 1
                        sl = slice(n * 128, (n + 1) * 128)
                        if n >= 2:
                            ps = nextps(); sc = scT[it % 2]
                            em.mm(ps[:, 0:128], EK[:, sl], EQ[:, sl], True, True, [EK, EQ], [ps])
                            em.op("dve", lambda g, ps=ps, sc=sc: g.tensor_tensor(out=sc[:], in0=ps[:, 0:128], in1=MASK[:], op=ALU.mult), reads=[ps, MASK], writes=[sc])
                            po = nextps()
                            em.mm(po[:, 0:128], sc[:], v_tm[:, n, :], True, False, [sc, v_tm], [po])
                            em.mm(po[:, 0:128], QD[:, sl], Sc[:], False, True, [QD, Sc], [po])
                            if di == 0:
                                em.op("act", lambda g, po=po, n=n: g.copy(out=o_acc[:, n - 2, :], in_=po[:, 0:128]), reads=[po], writes=[o_acc], shared=True)
                            else:
                                em.op("dve", lambda g, po=po, n=n: g.tensor_tensor(out=o_acc[:, n - 2, :], in0=o_acc[:, n - 2, :], in1=po[:, 0:128], op=ALU.add),
                                      reads=[po, o_acc], writes=[o_acc], shared=True)
                        if it == len(order) - 1:
                            break
                        pt = nextps(); kt_ = kit[it % 2]
                        em.tr(pt[:, 0:128], EK[:, sl], ident, [EK], [pt])
                        em.op("act", lambda g, pt=pt, kt_=kt_: g.copy(out=kt_[:], in_=pt[:, 0:128]), reads=[pt], writes=[kt_])
                        pu = nextps()
                        em.mm(pu[:, 0:128], kt_[:], v_tm[:, n, :], True, True, [kt_, v_tm], [pu])
                        em.op("dve", lambda g, Sc=Sc, Sn=Sn, n=n: g.tensor_scalar(out=Sn[:], in0=Sc[:], scalar1=dec[:, n:n + 1], scalar2=None, op0=ALU.mult),
                              reads=[Sc, dec], writes=[Sn])
                        em.op("dve", lambda g, pu=pu, Sn=Sn, n=n: g.scalar_tensor_tensor(out=Sn[:], in0=pu[:, 0:128], scalar=gend[:, n:n + 1], in1=Sn[:],
                                                                                       op0=ALU.mult, op1=ALU.add), reads=[pu, gend, Sn], writes=[Sn])
                    S2 = S2 if si % 2 == 0 else S2[::-1]
                gated_norm_store(o_acc, g_tm, hgw_bc, h * 128, jk, ssn, t1, sg, mT_all)
        if upto <= 2:
            em.barrier()
            print("instructions:", em.ninstr)
            return nc

        with em.phase():
            SAME = em.tile("SAME", [128, 128])
            em.op("pool", lambda g: g.memset(SAME[:], 0.0), writes=[SAME])
            em.op("pool", lambda g: g.memset(SAME[0:64, 0:64], 1.0), writes=[SAME])
            em.op("pool", lambda g: g.memset(SAME[64:128, 64:128], 1.0), writes=[SAME])
            SEL = em.tile("SEL", [128, 2, 128])
            em.op("pool", lambda g: g.memset(SEL[:], 0.0), writes=[SEL])
            em.op("pool", lambda g: g.memset(SEL[0:64, 0, :], 1.0), writes=[SEL])
            em.op("pool", lambda g: g.memset(SEL[64:128, 1, :], 1.0), writes=[SEL])
            TRI = [em.tile("TRI%d" % i, [128, 128]) for i in range(2)]
            NEGS = [em.tile("NEGS%d" % i, [128, 128]) for i in range(2)]
            NEGI = [em.tile("NEGI%d" % i, [128, 128]) for i in range(2)]
            for di, M in enumerate((MLE, MGE)):
                em.op("dve", lambda g, di=di, M=M: g.tensor_tensor(out=TRI[di][:], in0=M[:], in1=SAME[:], op=ALU.mult), reads=[M, SAME], writes=[TRI[di]])
                em.op("dve", lambda g, di=di: g.tensor_scalar(out=NEGI[di][:], in0=TRI[di][:], scalar1=-1.0, scalar2=1e9, op0=ALU.add, op1=ALU.mult),
                      reads=[TRI[di]], writes=[NEGI[di]])
                em.op("dve", lambda g, di=di: g.tensor_tensor(out=NEGS[di][:], in0=TRI[di][:], in1=ident[:], op=ALU.subtract), reads=[TRI[di], ident], writes=[NEGS[di]])
                em.op("dve", lambda g, di=di: g.tensor_scalar(out=NEGS[di][:], in0=NEGS[di][:], scalar1=-1.0, scalar2=1e9, op0=ALU.add, op1=ALU.mult),
                      reads=[NEGS[di]], writes=[NEGS[di]])
            cwt = em.tile("cwt", [128, 3, 24]); cwrow = em.tile("cwrow", [72, 128])
            em.dma("sp", lambda g: g.dma_start(out=cwrow[:], in_=conv_d.rearrange("j (g p) -> (j g) p", p=128)), writes=[cwrow])
            ps = nextps()
            em.tr(ps[:, 0:72], cwrow[:], ident, [cwrow], [ps], k=72)
            em.op("dve", lambda g: g.tensor_copy(out=cwt[:].rearrange("p j g -> p (j g)"), in_=ps[:, 0:72]), reads=[ps], writes=[cwt])
            gdw_bc = em.tile("gdw_bc", [128, 128])
            em.dma("sp", lambda g: g.dma_start(out=gdw_bc[:], in_=gdnw_d.partition_broadcast(128)), writes=[gdw_bc])
            alg = em.tile("alg", [128, 2, 8]); dtb = em.tile("dtb", [128, 2, 8])
            for di, (a_, d_) in enumerate(((alf_d, dtf_d), (alb_d, dtb_d))):
                em.dma("sp", lambda g, di=di, a_=a_: g.dma_start(out=alg[:, di, :], in_=a_.partition_broadcast(128)), writes=[alg], shared=True)
                em.dma("sp", lambda g, di=di, d_=d_: g.dma_start(out=dtb[:, di, :], in_=d_.partition_broadcast(128)), writes=[dtb], shared=True)
            em.op("act", lambda g: g.activation(out=alg[:], in_=alg[:], func=AF.Exp), reads=[alg], writes=[alg])
            em.op("dve", lambda g: g.tensor_scalar(out=alg[:], in0=alg[:], scalar1=-1.0, scalar2=None, op0=ALU.mult), reads=[alg], writes=[alg])
            gt = em.tile("gt", [128, NT, 32])
            em.dma("sp", lambda g: g.dma_start(out=gt[:], in_=ptm[:, 3072:3104].rearrange("(n p) c -> p n c", p=128)), reads=[B_ptm], writes=[gt])
            names = ["gcol", "lnb", "allg", "cumc", "ncum", "r1", "er1", "ecum", "send", "beta", "decb"]
            G = {}
            for di in range(2):
                for nm in names:
                    shp = [128, NT, 32] if nm == "allg" else ([128, NT, 16] if nm == "decb" else [128, NT, 8])
                    G[nm, di] = em.tile("%s%d" % (nm, di), shp)
                gcol, lnb, allg = G["gcol", di], G["lnb", di], G["allg", di]
                em.op("dve", lambda g, di=di, gcol=gcol: g.tensor_tensor(out=gcol[:], in0=gt[:, :, di * 8:(di + 1) * 8],
                                                                        in1=dtb[:, di:di + 1, :].to_broadcast([128, NT, 8]), op=ALU.add), reads=[gt, dtb], writes=[gcol])
                em.op("act", lambda g, gcol=gcol: g.activation(out=gcol[:], in_=gcol[:], func=AF.Exp), reads=[gcol], writes=[gcol])
                em.op("act", lambda g, gcol=gcol: g.activation(out=gcol[:], in_=gcol[:], func=AF.Ln, bias=1.0), reads=[gcol], writes=[gcol])
                em.op("dve", lambda g, di=di, gcol=gcol: g.tensor_tensor(out=gcol[:], in0=gcol[:], in1=alg[:, di:di + 1, :].to_broadcast([128, NT, 8]), op=ALU.mult),
                      reads=[gcol, alg], writes=[gcol])
                em.op("act", lambda g, di=di, lnb=lnb: g.activation(out=lnb[:], in_=gt[:, :, 16 + di * 8:16 + (di + 1) * 8], func=AF.Exp, scale=-1.0), reads=[gt], writes=[lnb])
                em.op("act", lambda g, lnb=lnb: g.activation(out=lnb[:], in_=lnb[:], func=AF.Ln, bias=1.0), reads=[lnb], writes=[lnb])
                em.op("dve", lambda g, lnb=lnb: g.tensor_scalar(out=lnb[:], in0=lnb[:], scalar1=-1.0, scalar2=None, op0=ALU.mult), reads=[lnb], writes=[lnb])
                for n in range(NT):
                    ps = nextps()
                    em.mm(ps[:, 0:8], TRI[di][:], gcol[:, n, :], True, True, [TRI[di], gcol], [ps])
                    em.mm(ps[:, 8:16], SAME[:], gcol[:, n, :], True, True, [SAME, gcol], [ps])
                    em.mm(ps[:, 16:24], SEL[:, 0, :], gcol[:, n, :], True, True, [SEL, gcol], [ps])
                    em.mm(ps[:, 24:32], SEL[:, 1, :], gcol[:, n, :], True, True, [SEL, gcol], [ps])
                    evac(allg[:, n, :], ps[:, 0:32], [ps], [allg])
                cumc, ncum, r1, er1, ecum, send, beta, decb = (G[k, di] for k in ("cumc", "ncum", "r1", "er1", "ecum", "send", "beta", "decb"))
                em.op("dve", lambda g, cumc=cumc, allg=allg: g.tensor_copy(out=cumc[:], in_=allg[:, :, 0:8]), reads=[allg], writes=[cumc])
                em.op("dve", lambda g, ncum=ncum, cumc=cumc: g.tensor_scalar(out=ncum[:], in0=cumc[:], scalar1=-1.0, scalar2=None, op0=ALU.mult), reads=[cumc], writes=[ncum])
                em.op("dve", lambda g, r1=r1, cumc=cumc, lnb=lnb: g.tensor_tensor(out=r1[:], in0=cumc[:], in1=lnb[:], op=ALU.add), reads=[cumc, lnb], writes=[r1])
                em.op("act", lambda g, er1=er1, r1=r1: g.activation(out=er1[:], in_=r1[:], func=AF.Exp), reads=[r1], writes=[er1])
                em.op("act", lambda g, ecum=ecum, cumc=cumc: g.activation(out=ecum[:], in_=cumc[:], func=AF.Exp), reads=[cumc], writes=[ecum])
                em.op("dve", lambda g, send=send, allg=allg, cumc=cumc: g.tensor_tensor(out=send[:], in0=allg[:, :, 8:16], in1=cumc[:], op=ALU.subtract), reads=[allg, cumc], writes=[send])
                em.op("act", lambda g, send=send: g.activation(out=send[:], in_=send[:], func=AF.Exp), reads=[send], writes=[send])
                em.op("act", lambda g, beta=beta, lnb=lnb: g.activation(out=beta[:], in_=lnb[:], func=AF.Exp), reads=[lnb], writes=[beta])
                em.op("act", lambda g, decb=decb, allg=allg: g.activation(out=decb[:], in_=allg[:, :, 16:32], func=AF.Exp), reads=[allg], writes=[decb])

            raw = em.tile("raw", [128, NTOK]); SQ = em.tile("SQ", [128, NTOK]); RI = em.tile("RI", [128, 512])
            YQ = em.tile("YQ", [128, NTOK]); YK = em.tile("YK", [128, NTOK]); YV = em.tile("YV", [128, NTOK])
            k_tm = em.tile("k_tm", [128, NT, 128]); vg_tm = em.tile("vg_tm", [128, NT, 128]); z_tm = em.tile("z_tm", [128, NTX, 128])
            o_fb = [em.tile("o_fb%d" % i, [128, NTX, 128]) for i in range(2)]
            mT_all = em.tile("mT_all2", [128, SEQ], F32R)
            jk = em.tile("jk2", [128, 128]); ssn = em.tile("ssn2", [128, NTX]); t1 = em.tile("t12", [128, 128]); sg = em.tile("sg2", [128, 128])
            WK = {}
            for di in range(2):
                for nm in ("DGr", "DGn", "DGc", "E1", "E2", "Bm", "Bt", "AT", "P", "Pt", "Pn", "Ptn", "R", "Rn", "vb", "kbd", "kend", "U", "WT", "VN", "O", "Sa", "Sb"):
                    WK[nm, di] = em.tile("%s_%d" % (nm, di), [128, 128])

            def conv_silu(Y, gi, h):
                cj = gi * 8 + h
                em.op("dve", lambda g: g.tensor_scalar(out=Y[:], in0=raw[:], scalar1=cwt[:, 1, cj:cj + 1], scalar2=None, op0=ALU.mult), reads=[raw, cwt], writes=[Y])
                segs = [(Y[:, 0:CTX].rearrange("p (r w) -> p r w", w=CTX), raw[:, 0:CTX].rearrange("p (r w) -> p r w", w=CTX), CTX),
                        (Y[:, CTX:NTOK].rearrange("p (r w) -> p r w", w=64), raw[:, CTX:NTOK].rearrange("p (r w) -> p r w", w=64), 64)]
                for (y3, a3, w) in segs:
                    em.op("dve", lambda g, y3=y3, a3=a3, w=w: g.scalar_tensor_tensor(out=y3[:, :, 1:w], in0=a3[:, :, 0:w - 1], scalar=cwt[:, 0, cj:cj + 1], in1=y3[:, :, 1:w],
                                                                                   op0=ALU.mult, op1=ALU.add), reads=[raw, cwt, Y], writes=[Y])
                    em.op("dve", lambda g, y3=y3, a3=a3, w=w: g.scalar_tensor_tensor(out=y3[:, :, 0:w - 1], in0=a3[:, :, 1:w], scalar=cwt[:, 2, cj:cj + 1], in1=y3[:, :, 0:w - 1],
                                                                                   op0=ALU.mult, op1=ALU.add), reads=[raw, cwt, Y], writes=[Y])
                em.op("act", lambda g: g.activation(out=Y[:], in_=Y[:], func=AF.Silu), reads=[Y], writes=[Y])

            def l2norm(Y, mult):
                em.op("dve", lambda g: g.tensor_tensor(out=SQ[:], in0=Y[:], in1=Y[:], op=ALU.mult), reads=[Y], writes=[SQ])
                for c0 in range(0, NTOK, 512):
                    w = min(512, NTOK - c0)
                    ps = nextps()
                    em.mm(ps[:, 0:w], ones[:], SQ[:, c0:c0 + w], True, True, [ones, SQ], [ps])
                    em.op("dve", lambda g, ps=ps, w=w: g.tensor_scalar(out=RI[:, 0:w], in0=ps[:, 0:w], scalar1=EPS, scalar2=None, op0=ALU.add), reads=[ps], writes=[RI])
                    em.op("act", lambda g, w=w: g.activation(out=RI[:, 0:w], in_=RI[:, 0:w], func=AF.Sqrt), reads=[RI], writes=[RI])
                    em.op("dve", lambda g, w=w: g.reciprocal(out=RI[:, 0:w], in_=RI[:, 0:w]), reads=[RI], writes=[RI])
                    em.op("dve", lambda g, c0=c0, w=w: g.scalar_tensor_tensor(out=Y[:, c0:c0 + w], in0=Y[:, c0:c0 + w], scalar=mult, in1=RI[:, 0:w], op0=ALU.mult, op1=ALU.mult),
                          reads=[Y, RI], writes=[Y])

            def gdn_chain(h, di):
                W = lambda nm: WK[nm, di]
                cumc, ncum, r1, er1, ecum, send, beta, decb = (G[k, di] for k in ("cumc", "ncum", "r1", "er1", "ecum", "send", "beta", "decb"))
                Sc, Sn = W("Sa"), W("Sb")
                em.op("pool", lambda g: g.memset(Sc[:], 0.0), writes=[Sc])
                order = [0, 1] + list(range(2, NT)) if di == 0 else [1, 0] + list(range(NT - 1, 1, -1))
                corder = (0, 1) if di == 0 else (1, 0)
                o_out = o_fb[di]
                for n in order:
                    sl = slice(n * 128, (n + 1) * 128)
                    isx = n >= 2
                    col = lambda t: t[:, n, h:h + 1]
                    psA = nextps()
                    em.mm(psA[:, 0:128], YK[:, sl], YK[:, sl], True, True, [YK], [psA])
                    if isx:
                        em.mm(psA[:, 128:256], YK[:, sl], YQ[:, sl], True, True, [YK, YQ], [psA])
                    em.op("dve", lambda g: g.tensor_scalar(out=W("DGr")[:], in0=ident[:], scalar1=col(r1), scalar2=None, op0=ALU.mult), reads=[ident, r1], writes=[W("DGr")])
                    em.op("dve", lambda g: g.tensor_scalar(out=W("DGn")[:], in0=ident[:], scalar1=col(ncum), scalar2=None, op0=ALU.mult), reads=[ident, ncum], writes=[W("DGn")])
                    psD = nextps()
                    em.mm(psD[:, 0:128], ones[:], W("DGr")[:], True, False, [ones, W("DGr")], [psD])
                    em.mm(psD[:, 0:128], W("DGn")[:], ones[:], False, True, [ones, W("DGn")], [psD])
                    if isx:
                        em.op("dve", lambda g: g.tensor_scalar(out=W("DGc")[:], in0=ident[:], scalar1=col(cumc), scalar2=None, op0=ALU.mult), reads=[ident, cumc], writes=[W("DGc")])
                        em.mm(psD[:, 128:256], ones[:], W("DGc")[:], True, False, [ones, W("DGc")], [psD])
                        em.mm(psD[:, 128:256], W("DGn")[:], ones[:], False, True, [ones, W("DGn")], [psD])
                    em.op("dve", lambda g: g.tensor_tensor(out=W("E1")[:], in0=psD[:, 0:128], in1=NEGS[di][:], op=ALU.add), reads=[psD, NEGS[di]], writes=[W("E1")])
                    em.op("act", lambda g: g.activation(out=W("E1")[:], in_=W("E1")[:], func=AF.Exp), reads=[W("E1")], writes=[W("E1")])
                    em.op("dve", lambda g: g.tensor_tensor(out=W("Bm")[:], in0=psA[:, 0:128], in1=W("E1")[:], op=ALU.mult), reads=[psA, W("E1")], writes=[W("Bm")])
                    if isx:
                        em.op("dve", lambda g: g.tensor_tensor(out=W("E2")[:], in0=psD[:, 128:256], in1=NEGI[di][:], op=ALU.add), reads=[psD, NEGI[di]], writes=[W("E2")])
                        em.op("act", lambda g: g.activation(out=W("E2")[:], in_=W("E2")[:], func=AF.Exp), reads=[W("E2")], writes=[W("E2")])
                        em.op("dve", lambda g: g.tensor_tensor(out=W("AT")[:], in0=psA[:, 128:256], in1=W("E2")[:], op=ALU.mult), reads=[psA, W("E2")], writes=[W("AT")])
                    yield
                    if upto == 2.6:
                        continue
                    pst = nextps()
                    em.tr(pst[:, 0:128], W("Bm")[:], ident, [W("Bm")], [pst])
                    em.op("act", lambda g: g.copy(out=W("Bt")[:], in_=pst[:, 0:128]), reads=[pst], writes=[W("Bt")])
                    em.op("dve", lambda g: g.tensor_tensor(out=W("R")[:], in0=ident[:], in1=W("Bm")[:], op=ALU.subtract), reads=[ident, W("Bm")], writes=[W("R")])
                    P, Pt, Pn, Ptn, R, Rn = W("Bm"), W("Bt"), W("Pn"), W("Ptn"), W("R"), W("Rn")
                    for lvl in range(5):
                        yield
                        ps2 = nextps()
                        em.mm(ps2[:, 0:128], P[:], Pt[:], True, True, [P, Pt], [ps2])
                        em.op("act", lambda g, ps2=ps2, Ptn=Ptn: g.copy(out=Ptn[:], in_=ps2[:, 0:128]), reads=[ps2], writes=[Ptn])
                        if lvl < 4:
                            em.mm(ps2[:, 128:256], Pt[:], P[:], True, True, [P, Pt], [ps2])
                            em.op("dve", lambda g, ps2=ps2, Pn=Pn: g.tensor_copy(out=Pn[:], in_=ps2[:, 128:256]), reads=[ps2], writes=[Pn])
                        ps3 = nextps()
                        em.mm(ps3[:, 0:128], Ptn[:], R[:], True, True, [Ptn, R], [ps3])
                        em.op("dve", lambda g, ps3=ps3, R=R, Rn=Rn: g.tensor_tensor(out=Rn[:], in0=ps3[:, 0:128], in1=R[:], op=ALU.add), reads=[ps3, R], writes=[Rn])
                        P, Pn = Pn, P
                        Pt, Ptn = Ptn, Pt
                        R, Rn = Rn, R
                    yield
                    if upto == 2.7:
                        continue
                    em.op("dve", lambda g: g.tensor_scalar(out=W("vb")[:], in0=vg_tm[:, n, :], scalar1=col(beta), scalar2=None, op0=ALU.mult), reads=[vg_tm, beta], writes=[W("vb")])
                    em.op("dve", lambda g: g.tensor_scalar(out=W("kbd")[:], in0=k_tm[:, n, :], scalar1=col(er1), scalar2=None, op0=ALU.mult), reads=[k_tm, er1], writes=[W("kbd")])
                    em.op("dve", lambda g: g.tensor_scalar(out=W("kend")[:], in0=k_tm[:, n, :], scalar1=col(send), scalar2=None, op0=ALU.mult), reads=[k_tm, send], writes=[W("kend")])
                    if n == 2 and h == 0:
                        for nm in ("DGr", "DGn", "E1", "Bm", "Bt", "vb", "kbd"):
                            dump("%s_%d" % (nm, di), W(nm), [128, 128])
                        dump("R_%d" % di, R, [128, 128])
                        if di == 0:
                            for nm in ("cumc", "r1", "ncum", "er1", "beta", "send", "gcol", "lnb"):
                                for dd in range(2):
                                    dump("%s%d" % (nm, dd), G[nm, dd], [128, NT, 8])
                            dump("k_tm", k_tm, [128, NT, 128]); dump("vg_tm", vg_tm, [128, NT, 128])
                    if upto == 2.71:
                        continue
                    psu = nextps()
                    em.mm(psu[:, 0:128], R[:], W("vb")[:], True, True, [R, W("vb")], [psu])
                    em.mm(psu[:, 128:256], W("kbd")[:], R[:], True, True, [R, W("kbd")], [psu])
                    if upto == 2.72:
                        continue
                    em.op("act", lambda g, psu=psu: g.copy(out=W("U")[:], in_=psu[:, 0:128]), reads=[psu], writes=[W("U")])
                    em.op("dve", lambda g, psu=psu: g.tensor_copy(out=W("WT")[:], in_=psu[:, 128:256]), reads=[psu], writes=[W("WT")])
                    if upto == 2.8:
                        continue
                    for c in corder:
                        yield
                        rs = slice(c * 64, (c + 1) * 64)
                        psv = nextps()
                        em.mm(psv[:, 0:128], W("WT")[:], Sc[:], True, True, [W("WT"), Sc], [psv])
                        if isx:
                            em.mm(psv[:, 128:256], YQ[:, sl], Sc[:], True, True, [YQ, Sc], [psv])
                        em.op("dve", lambda g, rs=rs, psv=psv: g.tensor_tensor(out=W("VN")[rs, :], in0=W("U")[rs, :], in1=psv[rs, 0:128], op=ALU.subtract),
                              reads=[W("U"), psv], writes=[W("VN")], shared=True)
                        if isx:
                            em.op("act", lambda g, rs=rs, psv=psv: g.activation(out=W("O")[rs, :], in_=psv[rs, 128:256], func=AF.Identity, scale=ecum[rs, n, h:h + 1]),
                                  reads=[psv, ecum], writes=[W("O")], shared=True)
                        pss = nextps()
                        em.mm(pss[:, 0:128], W("kend")[rs, :], W("VN")[rs, :], True, True, [W("kend"), W("VN")], [pss])
                        em.op("dve", lambda g, c=c, Sc=Sc, Sn=Sn: g.tensor_scalar(out=Sn[:], in0=Sc[:], scalar1=decb[:, n, c * 8 + h:c * 8 + h + 1], scalar2=None, op0=ALU.mult),
                              reads=[Sc, decb], writes=[Sn])
                        em.op("dve", lambda g, pss=pss, Sn=Sn: g.tensor_tensor(out=Sn[:], in0=Sn[:], in1=pss[:, 0:128], op=ALU.add), reads=[pss, Sn], writes=[Sn])
                        Sc, Sn = Sn, Sc
                    if isx:
                        yield
                        pso = nextps()
                        em.mm(pso[:, 0:128], W("AT")[:], W("VN")[:], True, True, [W("AT"), W("VN")], [pso])
                        em.op("dve", lambda g, pso=pso: g.tensor_tensor(out=o_out[:, n - 2, :], in0=W("O")[:], in1=pso[:, 0:128], op=ALU.add),
                              reads=[W("O"), pso], writes=[o_out], shared=True)
                    yield

            for h in range(ngd):
                for gi, Y in enumerate((YQ, YK, YV)):
                    r0 = 3072 + gi * 1024 + h * 128
                    em.dma("sp", lambda g, r0=r0: g.dma_start(out=raw[:], in_=pfm[r0:r0 + 128, :]), reads=[B_pfm], writes=[raw])
                    conv_silu(Y, gi, h)
                em.dma("pool", lambda g, h=h: g.dma_start(out=z_tm[:], in_=ptm[CTX:NTOK, 2048 + h * 128:2048 + (h + 1) * 128].rearrange("(n p) e -> p n e", p=128)),
                       reads=[B_ptm], writes=[z_tm])
                l2norm(YQ, 128 ** -0.5); l2norm(YK, 1.0)
                for n in range(NT):
                    for (Y, dst) in ((YK, k_tm), (YV, vg_tm)):
                        ps = nextps()
                        em.tr(ps[:, 0:128], Y[:, n * 128:(n + 1) * 128], ident, [Y], [ps])
                        evac(dst[:, n, :], ps[:, 0:128], [ps], [dst])
                if upto < 3:
                    for i in range(2):
                        em.op("pool", lambda g, i=i: g.memset(o_fb[i][:], 1.0), writes=[o_fb[i]])
                if upto == 2.5:
                    continue
                gens = [gdn_chain(h, 0), gdn_chain(h, 1)]
                alive = [True, True]
                while any(alive):
                    for i, gen in enumerate(gens):
                        if alive[i]:
                            try:
                                next(gen)
                            except StopIteration:
                                alive[i] = False
                em.op("dve", lambda g: g.tensor_tensor(out=o_fb[0][:], in0=o_fb[0][:], in1=o_fb[1][:], op=ALU.add), reads=[o_fb[0], o_fb[1]], writes=[o_fb[0]])
                gated_norm_store(o_fb[0], z_tm, gdw_bc, 1024 + h * 128, jk, ssn, t1, sg, mT_all)
        if upto <= 3:
            em.barrier()
            print("instructions:", em.ninstr)
            return nc

        bc_blk = nc.gpsimd.to_reg(NBLK * 128 - 1); bc_w = nc.gpsimd.to_reg(NE * 128 * 4 - 1); bc_b = nc.gpsimd.to_reg(NE - 1)
        icols = [em.tile("icol%d" % i, [128, 1], I32) for i in range(8)]
        icnt = {"i": 0}

        def probe_ind(tag, src=None, ic_=None):
            import os
            if os.environ.get("PROBE_IND") != "1":
                return
            try:
                tt = icols[0] if ic_ is None else ic_
                src = zt if src is None else src
                nc.gpsimd.indirect_dma_start(out=xs_d[:, :], out_offset=bass.IndirectOffsetOnAxis(ap=tt[:, :], axis=0), in_=src[:, :], in_offset=None,
                                             bounds_check=bc_blk, oob_is_err=False).then_inc(em.esem["pool"].h, 16)
                print("PROBE_IND", tag, "ok")
            except Exception as ex:
                print("PROBE_IND", tag, "ERR", ex)

        def idxcol(src, c):
            t_ = icols[icnt["i"] % 8]; icnt["i"] += 1
            em.op("pool", lambda g: g.tensor_copy(out=t_[:], in_=src[:, c:c + 1]), reads=[src], writes=[t_])
            return t_

        desti = em.tile("desti", [128, NTX * TOPK], I32); gates = em.tile("gates", [128, NTX, TOPK])
        widx = em.tile("widx", [128, NBLK * 4], I32); bidx = em.tile("bidx", [128, NBLK], I32)
        zt = em.tile("zt", [128, D])
        em.op("pool", lambda g: g.memset(zt[:], 0.0), writes=[zt])
        probe_ind("before zero fill")
        for j in range(NBLK):
            em.dma("pool", lambda g, j=j: g.dma_start(out=xs_d[j * 128:(j + 1) * 128, :], in_=zt[:]), reads=[zt], writes=[B_xs], shared=True)
        probe_ind("after zero fill")
        with em.phase():
            probe_ind("in phase")
            gt1_bc = em.tile("gt1_bc", [128, D])
            em.dma("sp", lambda g: g.dma_start(out=gt1_bc[:], in_=modrows[0, 2 * D:3 * D].partition_broadcast(128)), reads=[B_modrows], writes=[gt1_bc])
            wrt = em.tile("wrt", [128, 16, NE]); brt = em.tile("brt", [1, NE])
            em.dma("sp", lambda g: g.dma_start(out=wrt[:], in_=w_rt.rearrange("(p q) e -> p q e", q=16)), writes=[wrt])
            em.dma("sp", lambda g: g.dma_start(out=brt[:], in_=b_rt.rearrange("(o n) -> o n", o=1)), writes=[brt])
            mg = em.tile("mg", [128, 16, 512], F32R); wring = [em.tile("wo%d" % i, [128, 16, 512], F32R) for i in range(2)]
            x1t = [em.tile("x1t%d" % i, [128, D]) for i in range(4)]
            tmpy = [em.tile("tmpy%d" % i, [128, 512]) for i in range(2)]
            junk = em.tile("junk3", [128, D]); ss = em.tile("ss3", [128, 1]); xn2 = em.tile("xn2t", [128, D]); h2T = em.tile("h2T", [128, 16, 128])
            lg = em.tile("lg", [128, NTX, NE]); mask_all = em.tile("mask_all", [128, NTX, NE]); rank = em.tile("rank", [128, NTX, NE])
            top8 = em.tile("top8", [128, NTX, 8])
            SLT = em.tile("SLT", [128, 128])
            em.op("dve", lambda g: g.tensor_tensor(out=SLT[:], in0=MLE[:], in1=ident[:], op=ALU.subtract), reads=[MLE, ident], writes=[SLT])
            wov = w_out.rearrange("(k p) c -> p k c", p=128); mxv = mixT.rearrange("(k p) t -> p k t", p=128)
            wi = 0; yi = 0
            for gI in range(4):
                tok0 = gI * 512
                em.dma("pool", lambda g, tok0=tok0: g.dma_start(out=mg[:], in_=mxv[:, :, tok0:tok0 + 512]), reads=[B_mixT], writes=[mg])
                for j in range(4):
                    em.dma("sp", lambda g, j=j, tok0=tok0: g.dma_start(out=x1t[j][:], in_=x_d[tok0 + j * 128:tok0 + (j + 1) * 128, :]), writes=[x1t[j]])
                for cg in range(4):
                    wt = wring[wi % 2]; wi += 1
                    em.dma("pool", lambda g, wt=wt, cg=cg: g.dma_start(out=wt[:], in_=wov[:, :, cg * 512:(cg + 1) * 512]), writes=[wt])
                    for j in range(4):
                        ps = nextps()
                        for k in range(16):
                            em.mm(ps[:, :], mg[:, k, j * 128:(j + 1) * 128], wt[:, k, :], k == 0, k == 15, [mg, wt], [ps], r=True)
                        ty = tmpy[yi % 2]; yi += 1
                        em.op("dve", lambda g, ps=ps, ty=ty, cg=cg: g.tensor_tensor(out=ty[:], in0=ps[:, :], in1=gt1_bc[:, cg * 512:(cg + 1) * 512], op=ALU.mult),
                              reads=[ps, gt1_bc], writes=[ty])
                        em.op("dve", lambda g, ty=ty, j=j, cg=cg: g.tensor_tensor(out=x1t[j][:, cg * 512:(cg + 1) * 512], in0=x1t[j][:, cg * 512:(cg + 1) * 512], in1=ty[:], op=ALU.add),
                              reads=[ty, x1t[j]], writes=[x1t[j]])
                for j in range(4):
                    t = gI * 4 + j
                    xt = x1t[j]
                    em.dma("sp", lambda g, xt=xt, t=t: g.dma_start(out=x1_d[t * 128:(t + 1) * 128, :], in_=xt[:]), reads=[xt], writes=[B_x1], shared=True)
                    em.op("act", lambda g, xt=xt: g.activation(out=junk[:], in_=xt[:], func=AF.Square, accum_out=ss[:]), reads=[xt], writes=[junk, ss])
                    rstd_from_ss(ss, 1, D)
                    em.op("dve", lambda g, xt=xt: g.tensor_scalar(out=xn2[:], in0=xt[:], scalar1=ss[:, 0:1], scalar2=None, op0=ALU.mult), reads=[xt, ss], writes=[xn2])
                    em.dma("sp", lambda g, t=t: g.dma_start(out=xn2_d[t * 128:(t + 1) * 128, :], in_=xn2[:]), reads=[xn2], writes=[B_xn2], shared=True)
                    for qq in range(4):
                        ps = nextps()
                        for q4 in range(4):
                            q = qq * 4 + q4
                            em.tr(ps[:, q4 * 128:(q4 + 1) * 128], xn2[:, q:D:16], ident, [xn2], [ps])
                        for q4 in range(4):
                            q = qq * 4 + q4
                            em.op("act", lambda g, q=q, q4=q4, ps=ps: g.activation(out=h2T[:, q, :], in_=ps[:, q4 * 128:(q4 + 1) * 128], func=AF.Identity,
                                                                                  scale=A2x[:, q:q + 1], bias=B2x[:, q:q + 1]), reads=[ps, A2x, B2x], writes=[h2T], shared=True)
                    ps = nextps()
                    for q in range(16):
                        em.mm(ps[:, 0:NE], h2T[:, q, :], wrt[:, q, :], q == 0, False, [h2T, wrt], [ps])
                    em.mm(ps[:, 0:NE], ones[0:1, 0:128], brt[0:1, :], False, True, [ones, brt], [ps])
                    em.op("dve", lambda g, ps=ps, t=t: g.tensor_copy(out=lg[:, t, :], in_=ps[:, 0:NE]), reads=[ps], writes=[lg], shared=True)
            probe_ind("before routing")
            nm = em.tile("nm", [128, 1]); e4 = em.tile("e4", [128, 4]); es = em.tile("es", [128, 1])
            for t in range(NTX):
                em.op("dve", lambda g, t=t: g.max(out=top8[:, t, :], in_=lg[:, t, :]), reads=[lg], writes=[top8], shared=True)
                em.op("dve", lambda g, t=t: g.tensor_scalar(out=mask_all[:, t, :], in0=lg[:, t, :], scalar1=top8[:, t, 3:4], scalar2=None, op0=ALU.is_ge),
                      reads=[lg, top8], writes=[mask_all], shared=True)
                em.op("dve", lambda g, t=t: g.tensor_scalar(out=nm[:], in0=top8[:, t, 0:1], scalar1=-1.0, scalar2=None, op0=ALU.mult), reads=[top8], writes=[nm])
                em.op("act", lambda g, t=t: g.activation(out=e4[:], in_=top8[:, t, 0:4], func=AF.Exp, bias=nm[:, 0:1], accum_out=es[:]), reads=[top8, nm], writes=[e4, es])
                em.op("dve", lambda g: g.reciprocal(out=es[:], in_=es[:]), reads=[es], writes=[es])
                em.op("dve", lambda g, t=t: g.tensor_scalar(out=gates[:, t, :], in0=e4[:], scalar1=es[:, 0:1], scalar2=None, op0=ALU.mult), reads=[e4, es], writes=[gates], shared=True)
                ps = nextps()
                em.mm(ps[:, 0:NE], SLT[:], mask_all[:, t, :], True, t == 0, [SLT, mask_all], [ps])
                for tp in range(t):
                    em.mm(ps[:, 0:NE], ones[:], mask_all[:, tp, :], False, tp == t - 1, [ones, mask_all], [ps])
                em.op("dve", lambda g, ps=ps, t=t: g.tensor_copy(out=rank[:, t, :], in_=ps[:, 0:NE]), reads=[ps], writes=[rank], shared=True)
            cntb = em.tile("cntb", [128, NE]); nblk = em.tile("nblk", [128, NE]); cmp = em.tile("cmp", [128, NE]); pend = em.tile("pend", [128, NE]); pst = em.tile("pst", [128, NE])
            ps = nextps()
            for t in range(NTX):
                em.mm(ps[:, 0:NE], ones[:], mask_all[:, t, :], t == 0, t == NTX - 1, [ones, mask_all], [ps])
            em.op("dve", lambda g: g.tensor_copy(out=cntb[:], in_=ps[:, 0:NE]), reads=[ps], writes=[cntb])
            em.op("dve", lambda g: g.tensor_scalar(out=nblk[:], in0=cntb[:], scalar1=0.5, scalar2=None, op0=ALU.is_gt), reads=[cntb], writes=[nblk])
            for m in range(1, 16):
                em.op("dve", lambda g, m=m: g.tensor_scalar(out=cmp[:], in0=cntb[:], scalar1=128.0 * m + 0.5, scalar2=None, op0=ALU.is_gt), reads=[cntb], writes=[cmp])
                em.op("dve", lambda g: g.tensor_tensor(out=nblk[:], in0=nblk[:], in1=cmp[:], op=ALU.add), reads=[nblk, cmp], writes=[nblk])
            em.op("dve", lambda g: g.tensor_tensor_scan(out=pend[:], data0=ones[:, 0:NE], data1=nblk[:], initial=0.0, op0=ALU.mult, op1=ALU.add), reads=[ones, nblk], writes=[pend])
            em.op("dve", lambda g: g.tensor_tensor(out=pst[:], in0=pend[:], in1=nblk[:], op=ALU.subtract), reads=[pend, nblk], writes=[pst])
            em.op("dve", lambda g: g.tensor_scalar(out=pst[:], in0=pst[:], scalar1=128.0, scalar2=None, op0=ALU.mult), reads=[pst], writes=[pst])
            em.op("dve", lambda g: g.tensor_scalar(out=pend[:], in0=pend[:], scalar1=128.0, scalar2=None, op0=ALU.mult), reads=[pend], writes=[pend])
            em.op("dve", lambda g: g.tensor_tensor(out=rank[:], in0=rank[:], in1=pst[:].unsqueeze(1).to_broadcast([128, NTX, NE]), op=ALU.add), reads=[rank, pst], writes=[rank])
            destf = em.tile("destf", [128, NTX, TOPK]); eqt = em.tile("eqt", [128, NE])
            for t in range(NTX):
                for k in range(TOPK):
                    em.op("dve", lambda g, t=t, k=k: g.tensor_scalar(out=eqt[:], in0=lg[:, t, :], scalar1=top8[:, t, k:k + 1], scalar2=None, op0=ALU.is_equal), reads=[lg, top8], writes=[eqt])
                    em.op("dve", lambda g, t=t: g.tensor_tensor(out=eqt[:], in0=eqt[:], in1=rank[:, t, :], op=ALU.mult), reads=[eqt, rank], writes=[eqt])
                    em.op("dve", lambda g, t=t, k=k: g.reduce_sum(out=destf[:, t, k:k + 1], in_=eqt[:], axis=mybir.AxisListType.X), reads=[eqt], writes=[destf], shared=True)
            em.op("dve", lambda g: g.tensor_copy(out=desti[:], in_=destf[:].rearrange("p t k -> p (t k)")), reads=[destf], writes=[desti])
            jv = em.tile("jv", [128, NBLK]); bexp = em.tile("bexp", [128, NBLK]); cmpj = em.tile("cmpj", [128, NBLK]); pio = em.tile("pio", [128, 1])
            em.op("pool", lambda g: g.iota(jv[:], pattern=[[128, NBLK]], base=0, channel_multiplier=0, allow_small_or_imprecise_dtypes=True), writes=[jv])
            em.op("pool", lambda g: g.iota(pio[:], pattern=[[0, 1]], base=0, channel_multiplier=1, allow_small_or_imprecise_dtypes=True), writes=[pio])
            em.op("pool", lambda g: g.memset(bexp[:], 0.0), writes=[bexp])
            for e_ in range(NE):
                em.op("dve", lambda g, e_=e_: g.tensor_scalar(out=cmpj[:], in0=jv[:], scalar1=pend[:, e_:e_ + 1], scalar2=None, op0=ALU.is_ge), reads=[jv, pend], writes=[cmpj])
                em.op("dve", lambda g: g.tensor_tensor(out=bexp[:], in0=bexp[:], in1=cmpj[:], op=ALU.add), reads=[bexp, cmpj], writes=[bexp])
            em.op("dve", lambda g: g.tensor_scalar(out=bexp[:], in0=bexp[:], scalar1=float(NE - 1), scalar2=None, op0=ALU.min), reads=[bexp], writes=[bexp])
            em.op("dve", lambda g: g.tensor_copy(out=bidx[:], in_=bexp[:]), reads=[bexp], writes=[bidx])
            em.op("dve", lambda g: g.tensor_scalar(out=bexp[:], in0=bexp[:], scalar1=128.0, scalar2=None, op0=ALU.mult), reads=[bexp], writes=[bexp])
            em.op("dve", lambda g: g.tensor_scalar(out=bexp[:], in0=bexp[:], scalar1=pio[:, 0:1], scalar2=None, op0=ALU.add), reads=[bexp, pio], writes=[bexp])
            bexp4 = em.tile("bexp4", [128, NBLK, 4])
            for quad in range(4):
                em.op("dve", lambda g, quad=quad: g.tensor_scalar(out=bexp4[:, :, quad], in0=bexp[:], scalar1=4.0, scalar2=float(quad), op0=ALU.mult, op1=ALU.add),
                      reads=[bexp], writes=[bexp4], shared=True)
            em.op("dve", lambda g: g.tensor_copy(out=widx[:], in_=bexp4[:].rearrange("p j q -> p (j q)")), reads=[bexp4], writes=[widx])
            if dbg:
                dump("lg", lg, [128, NTX, NE]); dump("destf", destf, [128, NTX, TOPK]); dump("gates", gates, [128, NTX, TOPK]); dump("bexp", bexp, [128, NBLK])
            probe_ind("before scatter")
            for t in range(NTX):
                em.dma("sp", lambda g, t=t: g.dma_start(out=xn2[:], in_=xn2_d[t * 128:(t + 1) * 128, :]), reads=[B_xn2], writes=[xn2])
                for k in range(TOPK):
                    probe_ind("pre-idxcol xn2", src=xn2)
                    ic = idxcol(desti, t * TOPK + k)
                    probe_ind("post-idxcol zt", ic_=ic)
                    probe_ind("post-idxcol xn2", src=xn2, ic_=ic)
                    em.dma("pool", lambda g, ic=ic: g.indirect_dma_start(out=xs_d[:, :], out_offset=bass.IndirectOffsetOnAxis(ap=ic[:, :], axis=0),
                                                                        in_=xn2[:, :], in_offset=None, bounds_check=bc_blk, oob_is_err=False),
                           reads=[xn2, ic], writes=[B_xs], shared=True)
        if upto <= 4:
            em.barrier()
            print("instructions:", em.ninstr)
            return nc

        with em.phase():
            w2 = [w.rearrange("(e p q4 ql) c -> (e p q4) (ql c)", p=128, q4=4, ql=4) for w in (w_gate, w_up, w_down)]
            bsrc = (b_gate, b_up, b_down)
            wq = [em.tile("wq%d" % i, [128, 4 * D], F32R) for i in range(3)]
            xs_t = em.tile("xs_t", [128, D]); xsT = em.tile("xsT", [128, 16, 128]); actv = em.tile("actv", [128, D]); actT = em.tile("actT", [128, 16, 128])
            ysb = em.tile("ysb", [128, D]); gsb = em.tile("gsb", [128, 512]); usb = em.tile("usb", [128, 512]); sgm = em.tile("sgm", [128, 512])
            brow3 = [em.tile("brow3_%d" % i, [2, D], F32R) for i in range(3)]
            wqi = 0
            for j in range(NBLK):
                em.dma("sp", lambda g, j=j: g.dma_start(out=xs_t[:], in_=xs_d[j * 128:(j + 1) * 128, :]), reads=[B_xs], writes=[xs_t])
                bic = idxcol(bidx, j)
                for i in range(3):
                    em.dma("pool", lambda g, i=i: g.indirect_dma_start(out=brow3[i][0:2, :], out_offset=None, in_=bsrc[i][:, :],
                                                                     in_offset=bass.IndirectOffsetOnAxis(ap=bic[0:2, :], axis=0),
                                                                     bounds_check=bc_b, oob_is_err=False), reads=[bic], writes=[brow3[i]])
                for qq in range(4):
                    ps = nextps()
                    for q4 in range(4):
                        q = qq * 4 + q4
                        em.tr(ps[:, q4 * 128:(q4 + 1) * 128], xs_t[:, q:D:16], ident, [xs_t], [ps])
                    for q4 in range(4):
                        q = qq * 4 + q4
                        em.op("act", lambda g, q=q, q4=q4, ps=ps: g.activation(out=xsT[:, q, :].bitcast(F32R), in_=ps[:, q4 * 128:(q4 + 1) * 128], func=AF.Identity,
                                                                              scale=A2x[:, q:q + 1], bias=B2x[:, q:q + 1]), reads=[ps, A2x, B2x], writes=[xsT], shared=True)
                for quad in range(4):
                    wts = []
                    wic = idxcol(widx, j * 4 + quad)
                    for i in range(2):
                        wt = wq[wqi % 3]; wqi += 1
                        em.dma("pool", lambda g, wt=wt, i=i, j=j, quad=quad: g.indirect_dma_start(
                            out=wt[:, :], out_offset=None, in_=w2[i][:, :],
                            in_offset=bass.IndirectOffsetOnAxis(ap=wic[:, :], axis=0), bounds_check=bc_w, oob_is_err=False), reads=[wic], writes=[wt])
                        wts.append(wt)
                    for i in range(2):
                        for cg in range(4):
                            ps = PS[i * 4 + cg]
                            for ql in range(4):
                                q = quad * 4 + ql
                                em.mm(ps[:, :], xsT[:, q, :], wts[i][:, ql * D + cg * 512:ql * D + (cg + 1) * 512], q == 0, False, [xsT, wts[i]], [ps], r=True)
                for i in range(2):
                    for cg in range(4):
                        ps = PS[i * 4 + cg]
                        em.mm(ps[:, :], ones_r[0:1, 0:128], brow3[i][0:1, cg * 512:(cg + 1) * 512], False, True, [ones_r, brow3[i]], [ps], r=True)
                for cg in range(4):
                    cs_ = slice(cg * 512, (cg + 1) * 512)
                    em.op("dve", lambda g, cg=cg: g.tensor_scalar(out=gsb[:], in0=PS[cg][:, :], scalar1=LIMIT, scalar2=None, op0=ALU.min), reads=[PS[cg]], writes=[gsb])
                    em.op("act", lambda g: g.activation(out=sgm[:], in_=gsb[:], func=AF.Sigmoid, scale=ALPHA), reads=[gsb], writes=[sgm])
                    em.op("dve", lambda g, cg=cg: g.tensor_scalar(out=usb[:], in0=PS[4 + cg][:, :], scalar1=LIMIT, scalar2=-LIMIT, op0=ALU.min, op1=ALU.max), reads=[PS[4 + cg]], writes=[usb])
                    em.op("dve", lambda g: g.scalar_tensor_tensor(out=usb[:], in0=usb[:], scalar=1.0, in1=gsb[:], op0=ALU.add, op1=ALU.mult), reads=[usb, gsb], writes=[usb])
                    em.op("dve", lambda g, cs_=cs_: g.tensor_tensor(out=actv[:, cs_], in0=usb[:], in1=sgm[:], op=ALU.mult), reads=[usb, sgm], writes=[actv], shared=True)
                for qq in range(4):
                    ps = nextps()
                    for q4 in range(4):
                        q = qq * 4 + q4
                        em.tr(ps[:, q4 * 128:(q4 + 1) * 128], actv[:, q:D:16], ident, [actv], [ps])
                    for q4 in range(4):
                        q = qq * 4 + q4
                        evac(actT[:, q, :].bitcast(F32R), ps[:, q4 * 128:(q4 + 1) * 128], [ps], [actT])
                for quad in range(4):
                    wt = wq[wqi % 3]; wqi += 1
                    wic = idxcol(widx, j * 4 + quad)
                    em.dma("pool", lambda g, wt=wt, j=j, quad=quad: g.indirect_dma_start(
                        out=wt[:, :], out_offset=None, in_=w2[2][:, :],
                        in_offset=bass.IndirectOffsetOnAxis(ap=wic[:, :], axis=0), bounds_check=bc_w, oob_is_err=False), reads=[wic], writes=[wt])
                    for cg in range(4):
                        ps = PS[cg]
                        for ql in range(4):
                            q = quad * 4 + ql
                            em.mm(ps[:, :], actT[:, q, :], wt[:, ql * D + cg * 512:ql * D + (cg + 1) * 512], q == 0, False, [actT, wt], [ps], r=True)
                for cg in range(4):
                    em.mm(PS[cg][:, :], ones_r[0:1, 0:128], brow3[2][0:1, cg * 512:(cg + 1) * 512], False, True, [ones_r, brow3[2]], [PS[cg]], r=True)
                    evac(ysb[:, cg * 512:(cg + 1) * 512], PS[cg][:, :], [PS[cg]], [ysb])
                em.dma("sp", lambda g, j=j: g.dma_start(out=ys_d[j * 128:(j + 1) * 128, :], in_=ysb[:]), reads=[ysb], writes=[B_ys], shared=True)

        with em.phase():
            gt2_bc = em.tile("gt2_bc", [128, D]); now_bc = em.tile("now_bc", [128, D])
            em.dma("sp", lambda g: g.dma_start(out=gt2_bc[:], in_=modrows[0, 5 * D:6 * D].partition_broadcast(128)), reads=[B_modrows], writes=[gt2_bc])
            em.dma("sp", lambda g: g.dma_start(out=now_bc[:], in_=now_d.partition_broadcast(128)), writes=[now_bc])
            yk = [em.tile("yk%d" % i, [128, D]) for i in range(4)]
            x1b = [em.tile("x1b%d" % i, [128, D]) for i in range(2)]; acc = em.tile("acc", [128, D]); junk = em.tile("junk5", [128, D]); ss = em.tile("ss5", [128, 1])
            ob5 = [em.tile("ob5_%d" % i, [128, D]) for i in range(2)]
            for t in range(NTX):
                xb = x1b[t % 2]; ob = ob5[t % 2]
                em.dma("sp", lambda g, xb=xb, t=t: g.dma_start(out=xb[:], in_=x1_d[t * 128:(t + 1) * 128, :]), reads=[B_x1], writes=[xb])
                for k in range(TOPK):
                    ic = idxcol(desti, t * TOPK + k)
                    em.dma("pool", lambda g, k=k, ic=ic: g.indirect_dma_start(out=yk[k][:, :], out_offset=None, in_=ys_d[:, :],
                                                                            in_offset=bass.IndirectOffsetOnAxis(ap=ic[:, :], axis=0),
                                                                            bounds_check=bc_blk, oob_is_err=False), reads=[B_ys, ic], writes=[yk[k]])
                em.op("dve", lambda g, t=t: g.tensor_scalar(out=acc[:], in0=yk[0][:], scalar1=gates[:, t, 0:1], scalar2=None, op0=ALU.mult), reads=[yk[0], gates], writes=[acc])
                for k in range(1, TOPK):
                    em.op("dve", lambda g, t=t, k=k: g.scalar_tensor_tensor(out=acc[:], in0=yk[k][:], scalar=gates[:, t, k:k + 1], in1=acc[:], op0=ALU.mult, op1=ALU.add),
                          reads=[yk[k], gates, acc], writes=[acc])
                em.op("dve", lambda g: g.tensor_tensor(out=acc[:], in0=acc[:], in1=gt2_bc[:], op=ALU.mult), reads=[acc, gt2_bc], writes=[acc])
                em.op("dve", lambda g, xb=xb: g.tensor_tensor(out=acc[:], in0=acc[:], in1=xb[:], op=ALU.add), reads=[acc, xb], writes=[acc])
                em.op("act", lambda g: g.activation(out=junk[:], in_=acc[:], func=AF.Square, accum_out=ss[:]), reads=[acc], writes=[junk, ss])
                rstd_from_ss(ss, 1, D)
                em.op("dve", lambda g, ob=ob: g.scalar_tensor_tensor(out=ob[:], in0=acc[:], scalar=ss[:, 0:1], in1=now_bc[:], op0=ALU.mult, op1=ALU.mult),
                      reads=[acc, ss, now_bc], writes=[ob])
                em.dma("sp", lambda g, ob=ob, t=t: g.dma_start(out=y_d[t * 128:(t + 1) * 128, :], in_=ob[:]), reads=[ob], writes=[B_y], shared=True)
        em.barrier()
        print("instructions:", em.ninstr)
    return nc


_W_KEYS = ["w_ada", "b_ada", "norm_mix_w", "w_in", "hg_lb_f", "hg_lb_b", "hg_norm_w", "gd_conv_w", "gd_a_log_f", "gd_a_log_b",
           "gd_dt_bias_f", "gd_dt_bias_b", "gd_norm_w", "w_out", "norm_ffn_w", "w_router", "b_router", "w_gate", "b_gate",
           "w_up", "b_up", "w_down", "b_down"]


def kernel(**inputs):
    f32 = lambda a: np.ascontiguousarray(np.asarray(a, dtype=np.float32))
    x = f32(inputs["x"]); c = f32(inputs["c"]); ctx = f32(inputs["ctx"])
    nb = x.shape[0]
    ne = int(np.asarray(inputs["w_router"]).shape[-1])
    shared = {"c_ctx": f32(inputs["c_ctx"]), "norm_out_w": f32(inputs["norm_out_w"])}
    for k in _W_KEYS:
        a = f32(inputs[k])[0]
        if k in ("w_gate", "w_up", "w_down"):
            a = a.reshape(ne * D, D)
        shared[k] = np.ascontiguousarray(a)
    shared["hg_lb_f"] = f32(inputs["hg_lb_f"]); shared["hg_lb_b"] = f32(inputs["hg_lb_b"])
    nc = build_program(NE=ne)
    in_maps = []
    for b in range(nb):
        m = dict(shared)
        m["x"] = x[b]; m["c"] = c[b]; m["ctx"] = ctx[b]
        in_maps.append(m)
    res = run_bass_kernel_spmd(nc, in_maps, core_ids=list(range(nb)))
    return np.stack([r["y"] for r in res.results], axis=0).astype(np.float32)
```

```python
import contextlib
import numpy as np
import concourse.bass as bass
import concourse.mybir as mybir
from concourse.bass_utils import run_bass_kernel_spmd

F32 = mybir.dt.float32
I32 = mybir.dt.int32
F32R = mybir.dt.float32r
AF = mybir.ActivationFunctionType
ALU = mybir.AluOpType

D = 2048
SEQ = 2048
CTX = 256
NTOK = SEQ + CTX
NT = NTOK // 128
NTX = SEQ // 128
HGW = 1024
GDW = 1024
NH = 8
IN_DIM = 9248
TOPK = 4
EPS = 1e-6
LIMIT = 7.0
ALPHA = 1.702


class Sem:
    def __init__(self, handle, name):
        self.h = handle
        self.name = name
        self.count = 0


class Buf:
    def __init__(self, name):
        self.name = name
        self.ws = {}
        self.r = {}
        self.ld = None
        self.st = None


class T(Buf):
    def __init__(self, em, name, shape, dtype=F32, psum=False):
        super().__init__(name)
        self.is_tile = True
        self.is_psum = psum
        if psum:
            self.t = em.stack.enter_context(em.nc.psum_tensor(name, list(shape), dtype))
        else:
            self.t = em.stack.enter_context(em.nc.sbuf_tensor(name, list(shape), dtype))

    def __getitem__(self, idx):
        return self.t[idx]


class Emitter:
    def __init__(self, nc, stack, n_dma_sems=88):
        self.nc = nc
        self.gstack = stack
        self.stack = stack
        self.eng = {"pe": nc.tensor, "act": nc.scalar, "dve": nc.vector, "pool": nc.gpsimd, "sp": nc.sync}
        self.esem = {k: Sem(stack.enter_context(nc.semaphore("e_" + k)), k) for k in self.eng}
        self.seen = {k: {} for k in self.eng}
        self.pool = [Sem(stack.enter_context(nc.semaphore("d%d" % i)), "d%d" % i) for i in range(n_dma_sems)]
        self.used = []
        self.ninstr = 0
        self.phase_tiles = []
        self.log = None

    def get_sem(self):
        s = self.pool.pop()
        self.used.append(s)
        return s

    def tile(self, name, shape, dtype=F32):
        t = T(self, name, shape, dtype)
        self.phase_tiles.append(t)
        return t

    def psum(self, name, shape, dtype=F32):
        return T(self, name, shape, dtype, psum=True)

    def _waits(self, e, reads, writes, shared=False):
        need = {}

        def add(tk):
            if tk is None:
                return
            sem, val, is_dma = tk
            if is_dma:
                val = sem.count
            if e == "pe" and sem is self.esem["pe"]:
                return
            if need.get(sem, 0) < val:
                need[sem] = val

        for b in reads:
            for tk in b.ws.values():
                add(tk)
            if getattr(b, "is_psum", False):
                for tk in b.r.values():
                    if tk[0] is not self.esem.get(e):
                        add(tk)
        for b in writes:
            if not shared:
                for tk in b.ws.values():
                    add(tk)
            for tk in b.r.values():
                add(tk)
        for sem, val in need.items():
            if self.seen[e].get(sem, 0) < val:
                self.eng[e].wait_ge(sem.h, val)
                self.seen[e][sem] = val
                self.ninstr += 1
                if self.log is not None:
                    self.log.append((e, "wait", sem.name, val))

    def _record(self, tk, reads, writes, shared):
        sem = tk[0]
        for b in reads:
            b.r[sem] = tk
        for b in writes:
            if shared:
                b.ws[sem] = tk
            else:
                b.ws = {sem: tk}
                b.r = {}

    def op(self, e, fn, reads=(), writes=(), shared=False):
        self._waits(e, reads, writes, shared)
        ins = fn(self.eng[e])
        sem = self.esem[e]
        sem.count += 1
        ins.then_inc(sem.h, 1)
        tk = (sem, sem.count, False)
        if self.log is not None:
            self.log.append((e, "inc", sem.name, 1))
        self._record(tk, reads, writes, shared)
        self.ninstr += 1
        return ins

    def dma(self, q, fn, reads=(), writes=(), shared=False):
        self._waits(q, reads, writes, shared)
        sem = None
        for b in writes:
            if isinstance(b, T):
                if b.ld is None:
                    b.ld = self.get_sem()
                sem = b.ld
                break
        if sem is None:
            for b in reads:
                if isinstance(b, T):
                    if b.st is None:
                        b.st = self.get_sem()
                    sem = b.st
                    break
        assert sem is not None
        ins = fn(self.eng[q])
        sem.count += 16
        ins.then_inc(sem.h, 16)
        tk = (sem, sem.count, True)
        if self.log is not None:
            self.log.append((q, "inc", sem.name, 16))
        self._record(tk, reads, writes, shared)
        self.ninstr += 1
        return ins

    def barrier(self):
        sems = list(self.esem.values()) + list(self.used)
        for e in self.eng:
            for s in sems:
                if s is self.esem[e] or s.count == 0:
                    continue
                if self.seen[e].get(s, 0) < s.count:
                    self.eng[e].wait_ge(s.h, s.count)
                    self.seen[e][s] = s.count
                    self.ninstr += 1
                    if self.log is not None:
                        self.log.append((e, "wait", s.name, s.count))

    @contextlib.contextmanager
    def phase(self):
        old_stack, old_tiles = self.stack, self.phase_tiles
        st = contextlib.ExitStack()
        self.stack = st
        self.phase_tiles = []
        try:
            with st:
                yield
                self.barrier()
                for t in self.phase_tiles:
                    for s in (t.ld, t.st):
                        if s is not None:
                            self.used.remove(s)
                            self.pool.append(s)
        finally:
            self.stack = old_stack
            self.phase_tiles = old_tiles

    def mm(self, out, lhsT, rhs, start, stop, reads, writes, shared=False, r=False):
        if r:
            if lhsT.dtype != F32R:
                lhsT = lhsT.bitcast(F32R)
            if rhs.dtype != F32R:
                rhs = rhs.bitcast(F32R)
        return self.op("pe", lambda g: g.matmul(out, lhsT, rhs, start=start, stop=stop), reads, writes, shared)

    def tr(self, out, in_, ident, reads, writes, shared=False, k=128):
        return self.op("pe", lambda g: g.transpose(out, in_, ident[0:k, 0:k]), list(reads) + [ident], writes, shared)


def build_program(NE=32, dbg=False, upto=99, nhg=NH, ngd=NH):
    NBLK = (SEQ * TOPK) // 128 + NE
    nc = bass.Bass("TRN2", target_bir_lowering=False)

    def din(name, shape, dt=F32):
        return nc.dram_tensor(name, list(shape), dt, kind="ExternalInput").ap()

    def dscr(name, shape, dt=F32, out=False):
        return nc.dram_tensor(name, list(shape), dt, kind="ExternalOutput" if (out or dbg) else "Internal").ap()

    x_d = din("x", [SEQ, D]); ctx_d = din("ctx", [CTX, D]); c_d = din("c", [D]); cctx_d = din("c_ctx", [D])
    w_ada = din("w_ada", [D, 6 * D], F32R); b_ada = din("b_ada", [6 * D], F32R); nmw_d = din("norm_mix_w", [D])
    w_in = din("w_in", [D, IN_DIM], F32R); lbf_d = din("hg_lb_f", [2, HGW]); lbb_d = din("hg_lb_b", [2, HGW])
    hgnw_d = din("hg_norm_w", [128]); conv_d = din("gd_conv_w", [3, 3 * GDW])
    alf_d = din("gd_a_log_f", [NH]); alb_d = din("gd_a_log_b", [NH])
    dtf_d = din("gd_dt_bias_f", [NH]); dtb_d = din("gd_dt_bias_b", [NH]); gdnw_d = din("gd_norm_w", [128])
    w_out = din("w_out", [D, D], F32R); nfw_d = din("norm_ffn_w", [D]); w_rt = din("w_router", [D, NE])
    b_rt = din("b_router", [NE]); w_gate = din("w_gate", [NE * D, D], F32R); b_gate = din("b_gate", [NE, D], F32R)
    w_up = din("w_up", [NE * D, D], F32R); b_up = din("b_up", [NE, D], F32R); w_down = din("w_down", [NE * D, D], F32R)
    b_down = din("b_down", [NE, D], F32R); now_d = din("norm_out_w", [D])
    y_d = nc.dram_tensor("y", [SEQ, D], F32, kind="ExternalOutput").ap()

    modrows = dscr("modrows", [2, 6 * D])
    pfm = dscr("pfm", [6144, NTOK])
    ptm = dscr("ptm", [NTOK, 3104])
    mixT = dscr("mixT", [D, SEQ], F32R)
    x1_d = dscr("x1", [SEQ, D])
    xn2_d = dscr("xn2", [SEQ, D])
    xs_d = dscr("xs", [NBLK * 128, D])
    ys_d = dscr("ys", [NBLK * 128, D])
    B_modrows = Buf("modrows"); B_pfm = Buf("pfm"); B_ptm = Buf("ptm"); B_mixT = Buf("mixT")
    B_x1 = Buf("x1"); B_xn2 = Buf("xn2"); B_xs = Buf("xs"); B_ys = Buf("ys"); B_y = Buf("y")

    gst = contextlib.ExitStack()
    with gst:
        em = Emitter(nc, gst)
        em.log = [] if dbg else None
        nc._em = em
        PS = [em.psum("ps%d" % i, [128, 512]) for i in range(8)]
        ident = em.tile("ident", [128, 128]); ones = em.tile("ones", [128, 128])
        em.op("pool", lambda g: g.memset(ones[:], 1.0), writes=[ones])
        ones_r = em.tile("ones_r", [1, 128])
        em.op("dve", lambda g: g.tensor_copy(out=ones_r[:].bitcast(F32R), in_=ones[0:1, :]), reads=[ones], writes=[ones_r])

        def aff_mask(out_t, cmp, sgn=1):
            em.op("pool", lambda g: g.affine_select(out=out_t[:], in_=ones[:], pattern=[[-sgn, 128]], compare_op=cmp,
                                                     fill=0.0, base=0, channel_multiplier=sgn), reads=[ones], writes=[out_t])

        aff_mask(ident, ALU.is_equal)
        cnt = {"rr": 0}
        dumps = {}

        def dump(name, tl, shape):
            if not dbg:
                return
            d = nc.dram_tensor("dbg_" + name, list(shape), F32, kind="ExternalOutput").ap()
            em.dma("sp", lambda g: g.dma_start(out=d, in_=tl[:]), reads=[tl], writes=[Buf("dbg_" + name)])

        def evac(out_ap, in_ap, reads, writes):
            cnt["rr"] += 1
            if cnt["rr"] % 2:
                em.op("act", lambda g: g.copy(out=out_ap, in_=in_ap), reads, writes)
            else:
                em.op("dve", lambda g: g.tensor_copy(out=out_ap, in_=in_ap), reads, writes)

        def rstd_from_ss(ss, n, width):
            em.op("dve", lambda g: g.tensor_scalar(out=ss[:, 0:n], in0=ss[:, 0:n], scalar1=1.0 / width, scalar2=EPS,
                                                   op0=ALU.mult, op1=ALU.add), reads=[ss], writes=[ss])
            em.op("act", lambda g: g.activation(out=ss[:, 0:n], in_=ss[:, 0:n], func=AF.Sqrt), reads=[ss], writes=[ss])
            em.op("dve", lambda g: g.reciprocal(out=ss[:, 0:n], in_=ss[:, 0:n]), reads=[ss], writes=[ss])

        A1x = em.tile("A1x", [128, 16]); B1x = em.tile("B1x", [128, 16])
        A1c = em.tile("A1c", [128, 16]); B1c = em.tile("B1c", [128, 16])
        A2x = em.tile("A2x", [128, 16]); B2x = em.tile("B2x", [128, 16])

        with em.phase():
            cs = em.tile("cs", [128, 16, 2]); craw = em.tile("craw", [128, 2, 16])
            em.dma("sp", lambda g: g.dma_start(out=craw[:, 0, :], in_=c_d.rearrange("(p q) -> p q", q=16)), writes=[craw], shared=True)
            em.dma("sp", lambda g: g.dma_start(out=craw[:, 1, :], in_=cctx_d.rearrange("(p q) -> p q", q=16)), writes=[craw], shared=True)
            for r in range(2):
                em.op("act", lambda g, r=r: g.activation(out=cs[:, :, r].bitcast(F32R), in_=craw[:, r, :], func=AF.Silu), reads=[craw], writes=[cs], shared=True)
            brow = em.tile("brow", [1, 6 * D], F32R)
            em.dma("pool", lambda g: g.dma_start(out=brow[:], in_=b_ada.rearrange("(o n) -> o n", o=1)), writes=[brow])
            wv = w_ada.rearrange("(p q) c -> p q c", q=16)
            wring = [em.tile("adaw%d" % i, [128, 16, 512], F32R) for i in range(3)]
            mrow = [em.tile("mrow%d" % i, [2, 512]) for i in range(2)]
            for s in range(24):
                wt = wring[s % 3]
                em.dma("pool", lambda g, wt=wt, s=s: g.dma_start(out=wt[:], in_=wv[:, :, s * 512:(s + 1) * 512]), writes=[wt])
                ps = PS[s % 2]
                for q in range(16):
                    em.mm(ps[0:2, :], cs[:, q, :], wt[:, q, :], q == 0, False, reads=[cs, wt], writes=[ps], r=True)
                em.mm(ps[0:2, :], ones_r[0:1, 0:2], brow[0:1, s * 512:(s + 1) * 512], False, True, reads=[ones_r, brow], writes=[ps], r=True)
                mr = mrow[s % 2]
                evac(mr[:], ps[0:2, :], [ps], [mr])
                em.dma("sp", lambda g, mr=mr, s=s: g.dma_start(out=modrows[:, s * 512:(s + 1) * 512], in_=mr[:]), reads=[mr], writes=[B_modrows], shared=True)
            tmp = em.tile("modtmp", [128, 6, 16]); nw = em.tile("nw", [128, 2, 16])
            em.dma("sp", lambda g: g.dma_start(out=nw[:, 0, :], in_=nmw_d.rearrange("(p q) -> p q", q=16)), writes=[nw], shared=True)
            em.dma("sp", lambda g: g.dma_start(out=nw[:, 1, :], in_=nfw_d.rearrange("(p q) -> p q", q=16)), writes=[nw], shared=True)

            def col(row, chunk, dst):
                em.dma("sp", lambda g: g.dma_start(out=dst, in_=modrows[row, chunk * D:(chunk + 1) * D].rearrange("(p q) -> p q", q=16)),
                       reads=[B_modrows], writes=[tmp], shared=True)

            col(0, 0, tmp[:, 0, :]); col(0, 1, tmp[:, 1, :]); col(1, 0, tmp[:, 2, :]); col(1, 1, tmp[:, 3, :])
            col(0, 3, tmp[:, 4, :]); col(0, 4, tmp[:, 5, :])

            def mkA(dst, sc_idx, nwi):
                em.op("dve", lambda g: g.scalar_tensor_tensor(out=dst[:], in0=tmp[:, sc_idx, :], scalar=1.0, in1=nw[:, nwi, :],
                                                             op0=ALU.add, op1=ALU.mult), reads=[tmp, nw], writes=[dst])

            mkA(A1x, 1, 0); mkA(A1c, 3, 0); mkA(A2x, 5, 1)
            em.op("dve", lambda g: g.tensor_copy(out=B1x[:], in_=tmp[:, 0, :]), reads=[tmp], writes=[B1x])
            em.op("dve", lambda g: g.tensor_copy(out=B1c[:], in_=tmp[:, 2, :]), reads=[tmp], writes=[B1c])
            em.op("dve", lambda g: g.tensor_copy(out=B2x[:], in_=tmp[:, 4, :]), reads=[tmp], writes=[B2x])

        FM = [(0, 0), (512, 512), (1024, 1024), (1536, 1536), (2048, 2048), (2560, 2560),
              (5120, 3072), (5632, 3584), (6144, 4096), (6656, 4608), (7168, 5120), (7680, 5632)]
        TM = [(3072, 0, 512), (3584, 512, 512), (4096, 1024, 512), (4608, 1536, 512),
              (8192, 2048, 512), (8704, 2560, 512), (9216, 3072, 32)]
        with em.phase():
            wv = w_in.rearrange("(p q) c -> p q c", q=16)
            hxT = em.tile("hxT", [128, 16, 512])
            xt2 = [em.tile("xt%d" % i, [128, D]) for i in range(2)]
            xn = em.tile("xn", [128, D]); junk = em.tile("junk1", [128, D]); ss = em.tile("ss1", [128, 1])
            wring = [em.tile("winw%d" % i, [128, 16, 512], F32R) for i in range(3)]
            ob = [em.tile("ob%d" % i, [128, 512]) for i in range(4)]
            groups = [(0, 2, ctx_d, A1c, B1c)] + [(2 + 4 * g, 4, x_d, A1x, B1x) for g in range(4)]
            ti = 0; wi = 0; oi = 0; pi = 0
            for (t0, ntile, src, A1, B1) in groups:
                ntok = ntile * 128
                for j in range(ntile):
                    row0 = (t0 + j) * 128 - (0 if src is ctx_d else CTX)
                    xt = xt2[ti % 2]; ti += 1
                    em.dma("sp", lambda g, xt=xt, row0=row0, src=src: g.dma_start(out=xt[:], in_=src[row0:row0 + 128, :]), writes=[xt])
                    em.op("act", lambda g, xt=xt: g.activation(out=junk[:], in_=xt[:], func=AF.Square, accum_out=ss[:]), reads=[xt], writes=[junk, ss])
                    rstd_from_ss(ss, 1, D)
                    em.op("dve", lambda g, xt=xt: g.tensor_scalar(out=xn[:], in0=xt[:], scalar1=ss[:, 0:1], scalar2=None, op0=ALU.mult),
                          reads=[xt, ss], writes=[xn])
                    for qq in range(4):
                        ps = PS[pi % 8]; pi += 1
                        for q4 in range(4):
                            q = qq * 4 + q4
                            em.tr(ps[:, q4 * 128:(q4 + 1) * 128], xn[:, q:D:16], ident, [xn], [ps])
                        for q4 in range(4):
                            q = qq * 4 + q4
                            em.op("act", lambda g, q=q, q4=q4, ps=ps, j=j, A1=A1, B1=B1: g.activation(
                                out=hxT[:, q, j * 128:(j + 1) * 128].bitcast(F32R), in_=ps[:, q4 * 128:(q4 + 1) * 128], func=AF.Identity,
                                scale=A1[:, q:q + 1], bias=B1[:, q:q + 1]), reads=[ps, A1, B1], writes=[hxT], shared=True)
                tok0 = t0 * 128
                for (wc, prow) in FM:
                    wt = wring[wi % 3]; wi += 1
                    em.dma("pool", lambda g, wt=wt, wc=wc: g.dma_start(out=wt[:], in_=wv[:, :, wc:wc + 512]), writes=[wt])
                    for sub in range(4):
                        ps = PS[pi % 8]; pi += 1
                        for q in range(16):
                            em.mm(ps[:, 0:ntok], wt[:, q, sub * 128:(sub + 1) * 128], hxT[:, q, 0:ntok], q == 0, q == 15, [wt, hxT], [ps], r=True)
                        o = ob[oi % 4]; oi += 1
                        evac(o[:, 0:ntok], ps[:, 0:ntok], [ps], [o])
                        em.dma("sp", lambda g, o=o, prow=prow, sub=sub, tok0=tok0, ntok=ntok: g.dma_start(
                            out=pfm[prow + sub * 128:prow + (sub + 1) * 128, tok0:tok0 + ntok], in_=o[:, 0:ntok]), reads=[o], writes=[B_pfm], shared=True)
                for (wc, pcol, wd) in TM:
                    wt = wring[wi % 3]; wi += 1
                    em.dma("pool", lambda g, wt=wt, wc=wc, wd=wd: g.dma_start(out=wt[:, :, 0:wd], in_=wv[:, :, wc:wc + wd]), writes=[wt])
                    for j in range(ntile):
                        ps = PS[pi % 8]; pi += 1
                        for q in range(16):
                            em.mm(ps[:, 0:wd], hxT[:, q, j * 128:(j + 1) * 128], wt[:, q, 0:wd], q == 0, q == 15, [wt, hxT], [ps], r=True)
                        o = ob[oi % 4]; oi += 1
                        evac(o[:, 0:wd], ps[:, 0:wd], [ps], [o])
                        em.dma("sp", lambda g, o=o, pcol=pcol, wd=wd, r0=tok0 + j * 128: g.dma_start(
                            out=ptm[r0:r0 + 128, pcol:pcol + wd], in_=o[:, 0:wd]), reads=[o], writes=[B_ptm], shared=True)

        if upto <= 1:
            em.barrier()
            print("instructions:", em.ninstr)
            return nc

        pcnt = {"i": 0}

        def nextps():
            pcnt["i"] += 1
            return PS[pcnt["i"] % 8]

        MLE = em.tile("MLE", [128, 128]); MGE = em.tile("MGE", [128, 128])
        aff_mask(MLE, ALU.is_ge, -1); aff_mask(MGE, ALU.is_ge, 1)

        def gated_norm_store(o_acc, g_tm, w_bc, feat0, jk, ssn, t1, sg, mT_all):
            for n in range(NTX):
                em.op("act", lambda g, n=n: g.activation(out=jk[:], in_=o_acc[:, n, :], func=AF.Square, accum_out=ssn[:, n:n + 1]),
                      reads=[o_acc], writes=[jk, ssn], shared=True)
            rstd_from_ss(ssn, NTX, 128)
            for n in range(NTX):
                em.op("dve", lambda g, n=n: g.scalar_tensor_tensor(out=t1[:], in0=o_acc[:, n, :], scalar=ssn[:, n:n + 1], in1=w_bc[:],
                                                                  op0=ALU.mult, op1=ALU.mult), reads=[o_acc, ssn, w_bc], writes=[t1])
                em.op("act", lambda g, n=n: g.activation(out=sg[:], in_=g_tm[:, n, :], func=AF.Silu), reads=[g_tm], writes=[sg])
                em.op("dve", lambda g: g.tensor_tensor(out=t1[:], in0=t1[:], in1=sg[:], op=ALU.mult), reads=[t1, sg], writes=[t1])
                ps = nextps()
                em.tr(ps[:, 0:128], t1[:], ident, [t1], [ps])
                evac(mT_all[:, n * 128:(n + 1) * 128], ps[:, 0:128], [ps], [mT_all])
            em.dma("pool", lambda g: g.dma_start(out=mixT[feat0:feat0 + 128, :], in_=mT_all[:]), reads=[mT_all], writes=[B_mixT], shared=True)

        with em.phase():
            lbt = em.tile("lbt", [128, 2, 2, 8]); lb = em.tile("lb", [128, 2, 8]); oml = em.tile("oml", [128, 2, 8])
            lbrow = em.tile("lbrow", [32, 128])
            for di, src in enumerate((lbf_d, lbb_d)):
                em.dma("sp", lambda g, di=di, src=src: g.dma_start(out=lbrow[di * 16:(di + 1) * 16, :], in_=src.rearrange("l (h p) -> (l h) p", p=128)),
                       writes=[lbrow], shared=True)
            ps = nextps()
            em.tr(ps[:, 0:32], lbrow[:], ident, [lbrow], [ps], k=32)
            em.op("dve", lambda g: g.tensor_copy(out=lbt[:].rearrange("p d l h -> p (d l h)"), in_=ps[:, 0:32]), reads=[ps], writes=[lbt])
            em.op("dve", lambda g: g.tensor_tensor(out=lb[:], in0=lbt[:, :, 0, :], in1=lbt[:, :, 1, :], op=ALU.subtract), reads=[lbt], writes=[lb])
            em.op("act", lambda g: g.activation(out=lb[:], in_=lb[:], func=AF.Sigmoid), reads=[lb], writes=[lb])
            em.op("dve", lambda g: g.tensor_scalar(out=oml[:], in0=lb[:], scalar1=-1.0, scalar2=1.0, op0=ALU.mult, op1=ALU.add), reads=[lb], writes=[oml])
            hgw_bc = em.tile("hgw_bc", [128, 128])
            em.dma("sp", lambda g: g.dma_start(out=hgw_bc[:], in_=hgnw_d.partition_broadcast(128)), writes=[hgw_bc])
            rst = em.tile("rst", [128, NTOK])
            em.op("pool", lambda g: g.memset(rst[:], 1.0), writes=[rst])
            em.op("pool", lambda g: g.memset(rst[:, 0:NTOK:128], 0.0), writes=[rst])
            qT = em.tile("qT", [128, NTOK]); zz = em.tile("zz", [128, NTOK]); v_tm = em.tile("v_tm", [128, NT, 128])
            g_tm = em.tile("g_tm", [128, NTX, 128])
            Ft = em.tile("Ft", [128, NTOK]); LF = em.tile("LF", [128, NTOK]); Kt = em.tile("Kt", [128, NTOK])
            CUM = em.tile("CUM", [128, NTOK]); CUMB = em.tile("CUMB", [128, NTOK]); At = em.tile("At", [128, NTOK])
            EQ = em.tile("EQ", [128, NTOK]); EK = em.tile("EK", [128, NTOK]); QD = em.tile("QD", [128, NTOK])
            dec = em.tile("dec", [128, NT]); gend = em.tile("gend", [128, NT])
            S2 = [em.tile("S%d" % i, [128, 128]) for i in range(2)]
            scT = [em.tile("scT%d" % i, [128, 128]) for i in range(2)]; kit = [em.tile("kit%d" % i, [128, 128]) for i in range(2)]
            o_acc = em.tile("o_acc", [128, NTX, 128]); mT_all = em.tile("mT_all", [128, SEQ], F32R)
            jk = em.tile("jk", [128, 128]); ssn = em.tile("ssn", [128, NTX]); t1 = em.tile("t1", [128, 128]); sg = em.tile("sg", [128, 128])
            v3 = lambda t: t[:].rearrange("p (n w) -> p n w", w=128)
            for h in range(nhg):
                em.dma("sp", lambda g, h=h: g.dma_start(out=qT[:], in_=pfm[h * 128:(h + 1) * 128, :]), reads=[B_pfm], writes=[qT])
                em.dma("pool", lambda g, h=h: g.dma_start(out=v_tm[:], in_=ptm[:, h * 128:(h + 1) * 128].rearrange("(n p) e -> p n e", p=128)),
                       reads=[B_ptm], writes=[v_tm])
                em.dma("pool", lambda g, h=h: g.dma_start(out=g_tm[:], in_=ptm[CTX:NTOK, 1024 + h * 128:1024 + (h + 1) * 128].rearrange("(n p) e -> p n e", p=128)),
                       reads=[B_ptm], writes=[g_tm])
                for di in range(2):
                    zrow = 1024 + di * 1024 + h * 128
                    em.dma("sp", lambda g, zrow=zrow: g.dma_start(out=zz[:], in_=pfm[zrow:zrow + 128, :]), reads=[B_pfm], writes=[zz])
                    em.op("act", lambda g: g.activation(out=Ft[:], in_=zz[:], func=AF.Sigmoid), reads=[zz], writes=[Ft])
                    em.op("dve", lambda g, di=di, h=h: g.tensor_scalar(out=Ft[:], in0=Ft[:], scalar1=oml[:, di, h:h + 1], scalar2=lb[:, di, h:h + 1],
                                                                       op0=ALU.mult, op1=ALU.add), reads=[Ft, oml, lb], writes=[Ft])
                    em.op("act", lambda g: g.activation(out=LF[:], in_=Ft[:], func=AF.Ln), reads=[Ft], writes=[LF])
                    em.op("dve", lambda g: g.tensor_scalar(out=Kt[:], in0=Ft[:], scalar1=-1.0, scalar2=1.0, op0=ALU.mult, op1=ALU.add), reads=[Ft], writes=[Kt])
                    em.op("dve", lambda g: g.tensor_tensor_scan(out=CUM[:], data0=rst[:], data1=LF[:], initial=0.0, op0=ALU.mult, op1=ALU.add),
                          reads=[rst, LF], writes=[CUM])
                    if di == 0:
                        cum = CUM; last = 127
                    else:
                        em.op("dve", lambda g: g.tensor_tensor(out=CUMB[:], in0=LF[:], in1=CUM[:], op=ALU.subtract), reads=[LF, CUM], writes=[CUMB])
                        em.op("dve", lambda g: g.tensor_tensor(out=v3(CUMB), in0=v3(CUMB), in1=v3(CUM)[:, :, 127:128].to_broadcast([128, NT, 128]), op=ALU.add),
                              reads=[CUMB, CUM], writes=[CUMB])
                        cum = CUMB; last = 0
                    em.op("dve", lambda g, cum=cum: g.tensor_tensor(out=v3(At), in0=v3(cum), in1=v3(cum)[:, :, 64:65].to_broadcast([128, NT, 128]), op=ALU.subtract),
                          reads=[cum], writes=[At])
                    em.op("act", lambda g, last=last: g.activation(out=gend[:], in_=v3(At)[:, :, last], func=AF.Exp), reads=[At], writes=[gend])
                    em.op("act", lambda g, cum=cum, last=last: g.activation(out=dec[:], in_=v3(cum)[:, :, last], func=AF.Exp), reads=[cum], writes=[dec])
                    em.op("act", lambda g: g.activation(out=EQ[:], in_=At[:], func=AF.Exp), reads=[At], writes=[EQ])
                    em.op("dve", lambda g: g.tensor_tensor(out=EQ[:], in0=EQ[:], in1=qT[:], op=ALU.mult), reads=[EQ, qT], writes=[EQ])
                    em.op("act", lambda g: g.activation(out=EK[:], in_=At[:], func=AF.Exp, scale=-1.0), reads=[At], writes=[EK])
                    em.op("dve", lambda g: g.tensor_tensor(out=EK[:], in0=EK[:], in1=Kt[:], op=ALU.mult), reads=[EK, Kt], writes=[EK])
                    em.op("act", lambda g, cum=cum: g.activation(out=QD[:], in_=cum[:], func=AF.Exp), reads=[cum], writes=[QD])
                    em.op("dve", lambda g: g.tensor_tensor(out=QD[:], in0=QD[:], in1=qT[:], op=ALU.mult), reads=[QD, qT], writes=[QD])
                    order = [0, 1] + list(range(2, NT)) if di == 0 else [1, 0] + list(range(NT - 1, 1, -1))
                    MASK = MLE if di == 0 else MGE
                    si = 0
                    em.op("pool", lambda g: g.memset(S2[0][:], 0.0), writes=[S2[0]])
                    for it, n in enumerate(order):
                        Sc = S2[si % 2]; Sn = S2[(si + 1) % 2]; si += 1
                        sl = slice(n * 128, (n + 1) * 128)
                        if n >= 2:
                            ps = nextps(); sc = scT[it % 2]
                            em.mm(ps[:, 0:128], EK[:, sl], EQ[:, sl], True, True, [EK, EQ], [ps])
                            em.op("dve", lambda g, ps=ps, sc=sc: g.tensor_tensor(out=sc[:], in0=ps[:, 0:128], in1=MASK[:], op=ALU.mult), reads=[ps, MASK], writes=[sc])
                            po = nextps()
                            em.mm(po[:, 0:128], sc[:], v_tm[:, n, :], True, False, [sc, v_tm], [po])
                            em.mm(po[:, 0:128], QD[:, sl], Sc[:], False, True, [QD, Sc], [po])
                            if di == 0:
                                em.op("act", lambda g, po=po, n=n: g.copy(out=o_acc[:, n - 2, :], in_=po[:, 0:128]), reads=[po], writes=[o_acc], shared=True)
                            else:
                                em.op("dve", lambda g, po=po, n=n: g.tensor_tensor(out=o_acc[:, n - 2, :], in0=o_acc[:, n - 2, :], in1=po[:, 0:128], op=ALU.add),
                                      reads=[po, o_acc], writes=[o_acc], shared=True)
                        if it == len(order) - 1:
                            break
                        pt = nextps(); kt_ = kit[it % 2]
                        em.tr(pt[:, 0:128], EK[:, sl], ident, [EK], [pt])
                        em.op("act", lambda g, pt=pt, kt_=kt_: g.copy(out=kt_[:], in_=pt[:, 0:128]), reads=[pt], writes=[kt_])
                        pu = nextps()
                        em.mm(pu[:, 0:128], kt_[:], v_tm[:, n, :], True, True, [kt_, v_tm], [pu])
                        em.op("dve", lambda g, Sc=Sc, Sn=Sn, n=n: g.tensor_scalar(out=Sn[:], in0=Sc[:], scalar1=dec[:, n:n + 1], scalar2=None, op0=ALU.mult),
                              reads=[Sc, dec], writes=[Sn])
                        em.op("dve", lambda g, pu=pu, Sn=Sn, n=n: g.scalar_tensor_tensor(out=Sn[:], in0=pu[:, 0:128], scalar=gend[:, n:n + 1], in1=Sn[:],
                                                                                       op0=ALU.mult, op1=ALU.add), reads=[pu, gend, Sn], writes=[Sn])
                    S2 = S2 if si % 2 == 0 else S2[::-1]
                gated_norm_store(o_acc, g_tm, hgw_bc, h * 128, jk, ssn, t1, sg, mT_all)
        if upto <= 2:
            em.barrier()
            print("instructions:", em.ninstr)
            return nc

        with em.phase():
            SAME = em.tile("SAME", [128, 128])
            em.op("pool", lambda g: g.memset(SAME[:], 0.0), writes=[SAME])
            em.op("pool", lambda g: g.memset(SAME[0:64, 0:64], 1.0), writes=[SAME])
            em.op("pool", lambda g: g.memset(SAME[64:128, 64:128], 1.0), writes=[SAME])
            SEL = em.tile("SEL", [128, 2, 128])
            em.op("pool", lambda g: g.memset(SEL[:], 0.0), writes=[SEL])
            em.op("pool", lambda g: g.memset(SEL[0:64, 0, :], 1.0), writes=[SEL])
            em.op("pool", lambda g: g.memset(SEL[64:128, 1, :], 1.0), writes=[SEL])
            TRI = [em.tile("TRI%d" % i, [128, 128]) for i in range(2)]
            NEGS = [em.tile("NEGS%d" % i, [128, 128]) for i in range(2)]
            NEGI = [em.tile("NEGI%d" % i, [128, 128]) for i in range(2)]
            for di, M in enumerate((MLE, MGE)):
                em.op("dve", lambda g, di=di, M=M: g.tensor_tensor(out=TRI[di][:], in0=M[:], in1=SAME[:], op=ALU.mult), reads=[M, SAME], writes=[TRI[di]])
                em.op("dve", lambda g, di=di: g.tensor_scalar(out=NEGI[di][:], in0=TRI[di][:], scalar1=-1.0, scalar2=1e9, op0=ALU.add, op1=ALU.mult),
                      reads=[TRI[di]], writes=[NEGI[di]])
                em.op("dve", lambda g, di=di: g.tensor_tensor(out=NEGS[di][:], in0=TRI[di][:], in1=ident[:], op=ALU.subtract), reads=[TRI[di], ident], writes=[NEGS[di]])
                em.op("dve", lambda g, di=di: g.tensor_scalar(out=NEGS[di][:], in0=NEGS[di][:], scalar1=-1.0, scalar2=1e9, op0=ALU.add, op1=ALU.mult),
                      reads=[NEGS[di]], writes=[NEGS[di]])
            cwt = em.tile("cwt", [128, 3, 24]); cwrow = em.tile("cwrow", [72, 128])
            em.dma("sp", lambda g: g.dma_start(out=cwrow[:], in_=conv_d.rearrange("j (g p) -> (j g) p", p=128)), writes=[cwrow])
            ps = nextps()
            em.tr(ps[:, 0:72], cwrow[:], ident, [cwrow], [ps], k=72)
            em.op("dve", lambda g: g.tensor_copy(out=cwt[:].rearrange("p j g -> p (j g)"), in_=ps[:, 0:72]), reads=[ps], writes=[cwt])
            gdw_bc = em.tile("gdw_bc", [128, 128])
            em.dma("sp", lambda g: g.dma_start(out=gdw_bc[:], in_=gdnw_d.partition_broadcast(128)), writes=[gdw_bc])
            alg = em.tile("alg", [128, 2, 8]); dtb = em.tile("dtb", [128, 2, 8])
            for di, (a_, d_) in enumerate(((alf_d, dtf_d), (alb_d, dtb_d))):
                em.dma("sp", lambda g, di=di, a_=a_: g.dma_start(out=alg[:, di, :], in_=a_.partition_broadcast(128)), writes=[alg], shared=True)
                em.dma("sp", lambda g, di=di, d_=d_: g.dma_start(out=dtb[:, di, :], in_=d_.partition_broadcast(128)), writes=[dtb], shared=True)
            em.op("act", lambda g: g.activation(out=alg[:], in_=alg[:], func=AF.Exp), reads=[alg], writes=[alg])
            em.op("dve", lambda g: g.tensor_scalar(out=alg[:], in0=alg[:], scalar1=-1.0, scalar2=None, op0=ALU.mult), reads=[alg], writes=[alg])
            gt = em.tile("gt", [128, NT, 32])
            em.dma("sp", lambda g: g.dma_start(out=gt[:], in_=ptm[:, 3072:3104].rearrange("(n p) c -> p n c", p=128)), reads=[B_ptm], writes=[gt])
            names = ["gcol", "lnb", "allg", "cumc", "ncum", "r1", "er1", "ecum", "send", "beta", "decb"]
            G = {}
            for di in range(2):
                for nm in names:
                    shp = [128, NT, 32] if nm == "allg" else ([128, NT, 16] if nm == "decb" else [128, NT, 8])
                    G[nm, di] = em.tile("%s%d" % (nm, di), shp)
                gcol, lnb, allg = G["gcol", di], G["lnb", di], G["allg", di]
                em.op("dve", lambda g, di=di, gcol=gcol: g.tensor_tensor(out=gcol[:], in0=gt[:, :, di * 8:(di + 1) * 8],
                                                                        in1=dtb[:, di:di + 1, :].to_broadcast([128, NT, 8]), op=ALU.add), reads=[gt, dtb], writes=[gcol])
                em.op("act", lambda g, gcol=gcol: g.activation(out=gcol[:], in_=gcol[:], func=AF.Exp), reads=[gcol], writes=[gcol])
                em.op("act", lambda g, gcol=gcol: g.activation(out=gcol[:], in_=gcol[:], func=AF.Ln, bias=1.0), reads=[gcol], writes=[gcol])
                em.op("dve", lambda g, di=di, gcol=gcol: g.tensor_tensor(out=gcol[:], in0=gcol[:], in1=alg[:, di:di + 1, :].to_broadcast([128, NT, 8]), op=ALU.mult),
                      reads=[gcol, alg], writes=[gcol])
                em.op("act", lambda g, di=di, lnb=lnb: g.activation(out=lnb[:], in_=gt[:, :, 16 + di * 8:16 + (di + 1) * 8], func=AF.Exp, scale=-1.0), reads=[gt], writes=[lnb])
                em.op("act", lambda g, lnb=lnb: g.activation(out=lnb[:], in_=lnb[:], func=AF.Ln, bias=1.0), reads=[lnb], writes=[lnb])
                em.op("dve", lambda g, lnb=lnb: g.tensor_scalar(out=lnb[:], in0=lnb[:], scalar1=-1.0, scalar2=None, op0=ALU.mult), reads=[lnb], writes=[lnb])
                for n in range(NT):
                    ps = nextps()
                    em.mm(ps[:, 0:8], TRI[di][:], gcol[:, n, :], True, True, [TRI[di], gcol], [ps])
                    em.mm(ps[:, 8:16], SAME[:], gcol[:, n, :], True, True, [SAME, gcol], [ps])
                    em.mm(ps[:, 16:24], SEL[:, 0, :], gcol[:, n, :], True, True, [SEL, gcol], [ps])
                    em.mm(ps[:, 24:32], SEL[:, 1, :], gcol[:, n, :], True, True, [SEL, gcol], [ps])
                    evac(allg[:, n, :], ps[:, 0:32], [ps], [allg])
                cumc, ncum, r1, er1, ecum, send, beta, decb = (G[k, di] for k in ("cumc", "ncum", "r1", "er1", "ecum", "send", "beta", "decb"))
                em.op("dve", lambda g, cumc=cumc, allg=allg: g.tensor_copy(out=cumc[:], in_=allg[:, :, 0:8]), reads=[allg], writes=[cumc])
                em.op("dve", lambda g, ncum=ncum, cumc=cumc: g.tensor_scalar(out=ncum[:], in0=cumc[:], scalar1=-1.0, scalar2=None, op0=ALU.mult), reads=[cumc], writes=[ncum])
                em.op("dve", lambda g, r1=r1, cumc=cumc, lnb=lnb: g.tensor_tensor(out=r1[:], in0=cumc[:], in1=lnb[:], op=ALU.add), reads=[cumc, lnb], writes=[r1])
                em.op("act", lambda g, er1=er1, r1=r1: g.activation(out=er1[:], in_=r1[:], func=AF.Exp), reads=[r1], writes=[er1])
                em.op("act", lambda g, ecum=ecum, cumc=cumc: g.activation(out=ecum[:], in_=cumc[:], func=AF.Exp), reads=[cumc], writes=[ecum])
                em.op("dve", lambda g, send=send, allg=allg, cumc=cumc: g.tensor_tensor(out=send[:], in0=allg[:, :, 8:16], in1=cumc[:], op=ALU.subtract), reads=[allg, cumc], writes=[send])
                em.op("act", lambda g, send=send: g.activation(out=send[:], in_=send[:], func=AF.Exp), reads=[send], writes=[send])
                em.op("act", lambda g, beta=beta, lnb=lnb: g.activation(out=beta[:], in_=lnb[:], func=AF.Exp), reads=[lnb], writes=[beta])
                em.op("act", lambda g, decb=decb, allg=allg: g.activation(out=decb[:], in_=allg[:, :, 16:32], func=AF.Exp), reads=[allg], writes=[decb])

            raw = em.tile("raw", [128, NTOK]); SQ = em.tile("SQ", [128, NTOK]); RI = em.tile("RI", [128, 512])
            YQ = em.tile("YQ", [128, NTOK]); YK = em.tile("YK", [128, NTOK]); YV = em.tile("YV", [128, NTOK])
            k_tm = em.tile("k_tm", [128, NT, 128]); vg_tm = em.tile("vg_tm", [128, NT, 128]); z_tm = em.tile("z_tm", [128, NTX, 128])
            o_fb = [em.tile("o_fb%d" % i, [128, NTX, 128]) for i in range(2)]
            mT_all = em.tile("mT_all2", [128, SEQ], F32R)
            jk = em.tile("jk2", [128, 128]); ssn = em.tile("ssn2", [128, NTX]); t1 = em.tile("t12", [128, 128]); sg = em.tile("sg2", [128, 128])
            WK = {}
            for di in range(2):
                for nm in ("VN", "O", "Sa", "Sb"):
                    WK[nm, di] = em.tile("%s_%d" % (nm, di), [128, 128])

            def conv_silu(Y, gi, h):
                cj = gi * 8 + h
                em.op("dve", lambda g: g.tensor_scalar(out=Y[:], in0=raw[:], scalar1=cwt[:, 1, cj:cj + 1], scalar2=None, op0=ALU.mult), reads=[raw, cwt], writes=[Y])
                segs = [(Y[:, 0:CTX].rearrange("p (r w) -> p r w", w=CTX), raw[:, 0:CTX].rearrange("p (r w) -> p r w", w=CTX), CTX),
                        (Y[:, CTX:NTOK].rearrange("p (r w) -> p r w", w=64), raw[:, CTX:NTOK].rearrange("p (r w) -> p r w", w=64), 64)]
                for (y3, a3, w) in segs:
                    em.op("dve", lambda g, y3=y3, a3=a3, w=w: g.scalar_tensor_tensor(out=y3[:, :, 1:w], in0=a3[:, :, 0:w - 1], scalar=cwt[:, 0, cj:cj + 1], in1=y3[:, :, 1:w],
                                                                                   op0=ALU.mult, op1=ALU.add), reads=[raw, cwt, Y], writes=[Y])
                    em.op("dve", lambda g, y3=y3, a3=a3, w=w: g.scalar_tensor_tensor(out=y3[:, :, 0:w - 1], in0=a3[:, :, 1:w], scalar=cwt[:, 2, cj:cj + 1], in1=y3[:, :, 0:w - 1],
                                                                                   op0=ALU.mult, op1=ALU.add), reads=[raw, cwt, Y], writes=[Y])
                em.op("act", lambda g: g.activation(out=Y[:], in_=Y[:], func=AF.Silu), reads=[Y], writes=[Y])

            def l2norm(Y, mult):
                em.op("dve", lambda g: g.tensor_tensor(out=SQ[:], in0=Y[:], in1=Y[:], op=ALU.mult), reads=[Y], writes=[SQ])
                for c0 in range(0, NTOK, 512):
                    w = min(512, NTOK - c0)
                    ps = nextps()
                    em.mm(ps[:, 0:w], ones[:], SQ[:, c0:c0 + w], True, True, [ones, SQ], [ps])
                    em.op("dve", lambda g, ps=ps, w=w: g.tensor_scalar(out=RI[:, 0:w], in0=ps[:, 0:w], scalar1=EPS, scalar2=None, op0=ALU.add), reads=[ps], writes=[RI])
                    em.op("act", lambda g, w=w: g.activation(out=RI[:, 0:w], in_=RI[:, 0:w], func=AF.Sqrt), reads=[RI], writes=[RI])
                    em.op("dve", lambda g, w=w: g.reciprocal(out=RI[:, 0:w], in_=RI[:, 0:w]), reads=[RI], writes=[RI])
                    em.op("dve", lambda g, c0=c0, w=w: g.scalar_tensor_tensor(out=Y[:, c0:c0 + w], in0=Y[:, c0:c0 + w], scalar=mult, in1=RI[:, 0:w], op0=ALU.mult, op1=ALU.mult),
                          reads=[Y, RI], writes=[Y])

            NSLOT = 2
            A_NAMES = ("DGr", "DGn", "DGc", "E1", "E2", "Bm", "Bt", "AT", "Pn", "Ptn", "R", "Rn", "vb", "kbd", "kend", "U", "WT")
            AW = {}
            for di in range(2):
                for sl_ in range(NSLOT):
                    for nm in A_NAMES:
                        AW[nm, di, sl_] = em.tile("%s_%d_%d" % (nm, di, sl_), [128, 128])
            ARES = {}

            def gdn_A(h, di, n, slot):
                W = lambda nm: AW[nm, di, slot]
                cumc, ncum, r1, er1, send, beta = (G[k, di] for k in ("cumc", "ncum", "r1", "er1", "send", "beta"))
                sl = slice(n * 128, (n + 1) * 128)
                isx = n >= 2
                col = lambda t: t[:, n, h:h + 1]
                psA = nextps()
                em.mm(psA[:, 0:128], YK[:, sl], YK[:, sl], True, True, [YK], [psA])
                if isx:
                    em.mm(psA[:, 128:256], YK[:, sl], YQ[:, sl], True, True, [YK, YQ], [psA])
                em.op("dve", lambda g: g.tensor_scalar(out=W("DGr")[:], in0=ident[:], scalar1=col(r1), scalar2=None, op0=ALU.mult), reads=[ident, r1], writes=[W("DGr")])
                em.op("dve", lambda g: g.tensor_scalar(out=W("DGn")[:], in0=ident[:], scalar1=col(ncum), scalar2=None, op0=ALU.mult), reads=[ident, ncum], writes=[W("DGn")])
                psD = nextps()
                em.mm(psD[:, 0:128], ones[:], W("DGr")[:], True, False, [ones, W("DGr")], [psD])
                em.mm(psD[:, 0:128], W("DGn")[:], ones[:], False, True, [ones, W("DGn")], [psD])
                if isx:
                    em.op("dve", lambda g: g.tensor_scalar(out=W("DGc")[:], in0=ident[:], scalar1=col(cumc), scalar2=None, op0=ALU.mult), reads=[ident, cumc], writes=[W("DGc")])
                    em.mm(psD[:, 128:256], ones[:], W("DGc")[:], True, False, [ones, W("DGc")], [psD])
                    em.mm(psD[:, 128:256], W("DGn")[:], ones[:], False, True, [ones, W("DGn")], [psD])
                yield
                em.op("dve", lambda g: g.tensor_tensor(out=W("E1")[:], in0=psD[:, 0:128], in1=NEGS[di][:], op=ALU.add), reads=[psD, NEGS[di]], writes=[W("E1")])
                em.op("act", lambda g: g.activation(out=W("E1")[:], in_=W("E1")[:], func=AF.Exp), reads=[W("E1")], writes=[W("E1")])
                em.op("dve", lambda g: g.tensor_tensor(out=W("Bm")[:], in0=psA[:, 0:128], in1=W("E1")[:], op=ALU.mult), reads=[psA, W("E1")], writes=[W("Bm")])
                if isx:
                    em.op("dve", lambda g: g.tensor_tensor(out=W("E2")[:], in0=psD[:, 128:256], in1=NEGI[di][:], op=ALU.add), reads=[psD, NEGI[di]], writes=[W("E2")])
                    em.op("act", lambda g: g.activation(out=W("E2")[:], in_=W("E2")[:], func=AF.Exp), reads=[W("E2")], writes=[W("E2")])
                    em.op("dve", lambda g: g.tensor_tensor(out=W("AT")[:], in0=psA[:, 128:256], in1=W("E2")[:], op=ALU.mult), reads=[psA, W("E2")], writes=[W("AT")])
                yield
                pst = nextps()
                em.tr(pst[:, 0:128], W("Bm")[:], ident, [W("Bm")], [pst])
                em.op("act", lambda g: g.copy(out=W("Bt")[:], in_=pst[:, 0:128]), reads=[pst], writes=[W("Bt")])
                em.op("dve", lambda g: g.tensor_tensor(out=W("R")[:], in0=ident[:], in1=W("Bm")[:], op=ALU.subtract), reads=[ident, W("Bm")], writes=[W("R")])
                P, Pt, Pn, Ptn, R, Rn = W("Bm"), W("Bt"), W("Pn"), W("Ptn"), W("R"), W("Rn")
                for lvl in range(5):
                    yield
                    ps2 = nextps()
                    em.mm(ps2[:, 0:128], P[:], Pt[:], True, True, [P, Pt], [ps2])
                    em.op("act", lambda g, ps2=ps2, Ptn=Ptn: g.copy(out=Ptn[:], in_=ps2[:, 0:128]), reads=[ps2], writes=[Ptn])
                    if lvl < 4:
                        em.mm(ps2[:, 128:256], Pt[:], P[:], True, True, [P, Pt], [ps2])
                        em.op("dve", lambda g, ps2=ps2, Pn=Pn: g.tensor_copy(out=Pn[:], in_=ps2[:, 128:256]), reads=[ps2], writes=[Pn])
                    yield
                    ps3 = nextps()
                    em.mm(ps3[:, 0:128], Ptn[:], R[:], True, True, [Ptn, R], [ps3])
                    em.op("dve", lambda g, ps3=ps3, R=R, Rn=Rn: g.tensor_tensor(out=Rn[:], in0=ps3[:, 0:128], in1=R[:], op=ALU.add), reads=[ps3, R], writes=[Rn])
                    P, Pn = Pn, P
                    Pt, Ptn = Ptn, Pt
                    R, Rn = Rn, R
                yield
                em.op("dve", lambda g: g.tensor_scalar(out=W("vb")[:], in0=vg_tm[:, n, :], scalar1=col(beta), scalar2=None, op0=ALU.mult), reads=[vg_tm, beta], writes=[W("vb")])
                em.op("dve", lambda g: g.tensor_scalar(out=W("kbd")[:], in0=k_tm[:, n, :], scalar1=col(er1), scalar2=None, op0=ALU.mult), reads=[k_tm, er1], writes=[W("kbd")])
                em.op("dve", lambda g: g.tensor_scalar(out=W("kend")[:], in0=k_tm[:, n, :], scalar1=col(send), scalar2=None, op0=ALU.mult), reads=[k_tm, send], writes=[W("kend")])
                psu = nextps()
                em.mm(psu[:, 0:128], R[:], W("vb")[:], True, True, [R, W("vb")], [psu])
                em.mm(psu[:, 128:256], W("kbd")[:], R[:], True, True, [R, W("kbd")], [psu])
                yield
                em.op("act", lambda g, psu=psu: g.copy(out=W("U")[:], in_=psu[:, 0:128]), reads=[psu], writes=[W("U")])
                em.op("dve", lambda g, psu=psu: g.tensor_copy(out=W("WT")[:], in_=psu[:, 128:256]), reads=[psu], writes=[W("WT")])
                ARES[di, n] = slot

            def gdn_B(h, di, order):
                ecum, decb = G["ecum", di], G["decb", di]
                Sc, Sn = WK["Sa", di], WK["Sb", di]
                VN, O = WK["VN", di], WK["O", di]
                em.op("pool", lambda g: g.memset(Sc[:], 0.0), writes=[Sc])
                corder = (0, 1) if di == 0 else (1, 0)
                o_out = o_fb[di]
                for idx, n in enumerate(order):
                    while (di, n) not in ARES:
                        yield "wait"
                    slot = ARES.pop((di, n))
                    W = lambda nm: AW[nm, di, slot]
                    sl = slice(n * 128, (n + 1) * 128)
                    isx = n >= 2
                    for c in corder:
                        rs = slice(c * 64, (c + 1) * 64)
                        psv = nextps()
                        em.mm(psv[:, 0:128], W("WT")[:], Sc[:], True, True, [W("WT"), Sc], [psv])
                        if isx:
                            em.mm(psv[:, 128:256], YQ[:, sl], Sc[:], True, True, [YQ, Sc], [psv])
                        yield
                        em.op("dve", lambda g, rs=rs, psv=psv: g.tensor_tensor(out=VN[rs, :], in0=W("U")[rs, :], in1=psv[rs, 0:128], op=ALU.subtract),
                              reads=[W("U"), psv], writes=[VN], shared=True)
                        if isx:
                            em.op("act", lambda g, rs=rs, psv=psv: g.activation(out=O[rs, :], in_=psv[rs, 128:256], func=AF.Identity, scale=ecum[rs, n, h:h + 1]),
                                  reads=[psv, ecum], writes=[O], shared=True)
                        pss = nextps()
                        em.mm(pss[:, 0:128], W("kend")[rs, :], VN[rs, :], True, True, [W("kend"), VN], [pss])
                        em.op("dve", lambda g, c=c, Sc=Sc, Sn=Sn: g.tensor_scalar(out=Sn[:], in0=Sc[:], scalar1=decb[:, n, c * 8 + h:c * 8 + h + 1], scalar2=None, op0=ALU.mult),
                              reads=[Sc, decb], writes=[Sn])
                        yield
                        em.op("dve", lambda g, pss=pss, Sn=Sn: g.tensor_tensor(out=Sn[:], in0=Sn[:], in1=pss[:, 0:128], op=ALU.add), reads=[pss, Sn], writes=[Sn])
                        Sc, Sn = Sn, Sc
                    if isx:
                        pso = nextps()
                        em.mm(pso[:, 0:128], W("AT")[:], VN[:], True, True, [W("AT"), VN], [pso])
                        yield
                        em.op("dve", lambda g, pso=pso: g.tensor_tensor(out=o_out[:, n - 2, :], in0=O[:], in1=pso[:, 0:128], op=ALU.add),
                              reads=[O, pso], writes=[o_out], shared=True)
                    yield ("done", idx)

            def run_gdn_head(h):
                st = []
                for di in range(2):
                    order = [0, 1] + list(range(2, NT)) if di == 0 else [1, 0] + list(range(NT - 1, 1, -1))
                    st.append(dict(order=order, a_next=0, a_act=[], b=gdn_B(h, di, order), b_done=0, alive=True))
                ARES.clear()
                while any(s_["alive"] for s_ in st):
                    for di, s_ in enumerate(st):
                        if not s_["alive"]:
                            continue
                        while s_["a_next"] < len(s_["order"]) and s_["a_next"] - s_["b_done"] < NSLOT:
                            n = s_["order"][s_["a_next"]]
                            s_["a_act"].append(gdn_A(h, di, n, s_["a_next"] % NSLOT))
                            s_["a_next"] += 1
                        for gen in list(s_["a_act"]):
                            try:
                                next(gen)
                            except StopIteration:
                                s_["a_act"].remove(gen)
                        try:
                            r_ = next(s_["b"])
                            if isinstance(r_, tuple) and r_[0] == "done":
                                s_["b_done"] = r_[1] + 1
                        except StopIteration:
                            s_["alive"] = False

            for h in range(ngd):
                for gi, Y in enumerate((YQ, YK, YV)):
                    r0 = 3072 + gi * 1024 + h * 128
                    em.dma("sp", lambda g, r0=r0: g.dma_start(out=raw[:], in_=pfm[r0:r0 + 128, :]), reads=[B_pfm], writes=[raw])
                    conv_silu(Y, gi, h)
                em.dma("pool", lambda g, h=h: g.dma_start(out=z_tm[:], in_=ptm[CTX:NTOK, 2048 + h * 128:2048 + (h + 1) * 128].rearrange("(n p) e -> p n e", p=128)),
                       reads=[B_ptm], writes=[z_tm])
                l2norm(YQ, 128 ** -0.5); l2norm(YK, 1.0)
                for n in range(NT):
                    for (Y, dst) in ((YK, k_tm), (YV, vg_tm)):
                        ps = nextps()
                        em.tr(ps[:, 0:128], Y[:, n * 128:(n + 1) * 128], ident, [Y], [ps])
                        evac(dst[:, n, :], ps[:, 0:128], [ps], [dst])
                if upto < 3:
                    for i in range(2):
                        em.op("pool", lambda g, i=i: g.memset(o_fb[i][:], 1.0), writes=[o_fb[i]])
                if upto == 2.5:
                    continue
                run_gdn_head(h)
                em.op("dve", lambda g: g.tensor_tensor(out=o_fb[0][:], in0=o_fb[0][:], in1=o_fb[1][:], op=ALU.add), reads=[o_fb[0], o_fb[1]], writes=[o_fb[0]])
                gated_norm_store(o_fb[0], z_tm, gdw_bc, 1024 + h * 128, jk, ssn, t1, sg, mT_all)
        if upto <= 3:
            em.barrier()
            print("instructions:", em.ninstr)
            return nc

        bc_blk = nc.gpsimd.to_reg(NBLK * 128 - 1); bc_w = nc.gpsimd.to_reg(NE * 128 * 4 - 1); bc_b = nc.gpsimd.to_reg(NE - 1)
        icols = [em.tile("icol%d" % i, [128, 1], I32) for i in range(8)]
        icnt = {"i": 0}

        def probe_ind(tag, src=None, ic_=None):
            import os
            if os.environ.get("PROBE_IND") != "1":
                return
            try:
                tt = icols[0] if ic_ is None else ic_
                src = zt if src is None else src
                nc.gpsimd.indirect_dma_start(out=xs_d[:, :], out_offset=bass.IndirectOffsetOnAxis(ap=tt[:, :], axis=0), in_=src[:, :], in_offset=None,
                                             bounds_check=bc_blk, oob_is_err=False).then_inc(em.esem["pool"].h, 16)
                print("PROBE_IND", tag, "ok")
            except Exception as ex:
                print("PROBE_IND", tag, "ERR", ex)

        def idxcol(src, c):
            t_ = icols[icnt["i"] % 8]; icnt["i"] += 1
            em.op("pool", lambda g: g.tensor_copy(out=t_[:], in_=src[:, c:c + 1]), reads=[src], writes=[t_])
            return t_

        desti = em.tile("desti", [128, NTX * TOPK], I32); gates = em.tile("gates", [128, NTX, TOPK])
        widx = em.tile("widx", [128, NBLK * 4], I32); bidx = em.tile("bidx", [128, NBLK], I32)
        zt = em.tile("zt", [128, D])
        em.op("pool", lambda g: g.memset(zt[:], 0.0), writes=[zt])
        probe_ind("before zero fill")
        for j in range(NBLK):
            em.dma("pool", lambda g, j=j: g.dma_start(out=xs_d[j * 128:(j + 1) * 128, :], in_=zt[:]), reads=[zt], writes=[B_xs], shared=True)
        probe_ind("after zero fill")
        with em.phase():
            probe_ind("in phase")
            gt1_bc = em.tile("gt1_bc", [128, D])
            em.dma("sp", lambda g: g.dma_start(out=gt1_bc[:], in_=modrows[0, 2 * D:3 * D].partition_broadcast(128)), reads=[B_modrows], writes=[gt1_bc])
            wrt = em.tile("wrt", [128, 16, NE]); brt = em.tile("brt", [1, NE])
            em.dma("sp", lambda g: g.dma_start(out=wrt[:], in_=w_rt.rearrange("(p q) e -> p q e", q=16)), writes=[wrt])
            em.dma("sp", lambda g: g.dma_start(out=brt[:], in_=b_rt.rearrange("(o n) -> o n", o=1)), writes=[brt])
            mg = em.tile("mg", [128, 16, 512], F32R); wring = [em.tile("wo%d" % i, [128, 16, 512], F32R) for i in range(2)]
            x1t = [em.tile("x1t%d" % i, [128, D]) for i in range(4)]
            tmpy = [em.tile("tmpy%d" % i, [128, 512]) for i in range(2)]
            junk = em.tile("junk3", [128, D]); ss = em.tile("ss3", [128, 1]); xn2 = em.tile("xn2t", [128, D]); h2T = em.tile("h2T", [128, 16, 128])
            lg = em.tile("lg", [128, NTX, NE]); mask_all = em.tile("mask_all", [128, NTX, NE]); rank = em.tile("rank", [128, NTX, NE])
            top8 = em.tile("top8", [128, NTX, 8])
            SLT = em.tile("SLT", [128, 128])
            em.op("dve", lambda g: g.tensor_tensor(out=SLT[:], in0=MLE[:], in1=ident[:], op=ALU.subtract), reads=[MLE, ident], writes=[SLT])
            wov = w_out.rearrange("(k p) c -> p k c", p=128); mxv = mixT.rearrange("(k p) t -> p k t", p=128)
            wi = 0; yi = 0
            for gI in range(4):
                tok0 = gI * 512
                em.dma("pool", lambda g, tok0=tok0: g.dma_start(out=mg[:], in_=mxv[:, :, tok0:tok0 + 512]), reads=[B_mixT], writes=[mg])
                for j in range(4):
                    em.dma("sp", lambda g, j=j, tok0=tok0: g.dma_start(out=x1t[j][:], in_=x_d[tok0 + j * 128:tok0 + (j + 1) * 128, :]), writes=[x1t[j]])
                for cg in range(4):
                    wt = wring[wi % 2]; wi += 1
                    em.dma("pool", lambda g, wt=wt, cg=cg: g.dma_start(out=wt[:], in_=wov[:, :, cg * 512:(cg + 1) * 512]), writes=[wt])
                    for j in range(4):
                        ps = nextps()
                        for k in range(16):
                            em.mm(ps[:, :], mg[:, k, j * 128:(j + 1) * 128], wt[:, k, :], k == 0, k == 15, [mg, wt], [ps], r=True)
                        ty = tmpy[yi % 2]; yi += 1
                        em.op("dve", lambda g, ps=ps, ty=ty, cg=cg: g.tensor_tensor(out=ty[:], in0=ps[:, :], in1=gt1_bc[:, cg * 512:(cg + 1) * 512], op=ALU.mult),
                              reads=[ps, gt1_bc], writes=[ty])
                        em.op("dve", lambda g, ty=ty, j=j, cg=cg: g.tensor_tensor(out=x1t[j][:, cg * 512:(cg + 1) * 512], in0=x1t[j][:, cg * 512:(cg + 1) * 512], in1=ty[:], op=ALU.add),
                              reads=[ty, x1t[j]], writes=[x1t[j]])
                for j in range(4):
                    t = gI * 4 + j
                    xt = x1t[j]
                    em.dma("sp", lambda g, xt=xt, t=t: g.dma_start(out=x1_d[t * 128:(t + 1) * 128, :], in_=xt[:]), reads=[xt], writes=[B_x1], shared=True)
                    em.op("act", lambda g, xt=xt: g.activation(out=junk[:], in_=xt[:], func=AF.Square, accum_out=ss[:]), reads=[xt], writes=[junk, ss])
                    rstd_from_ss(ss, 1, D)
                    em.op("dve", lambda g, xt=xt: g.tensor_scalar(out=xn2[:], in0=xt[:], scalar1=ss[:, 0:1], scalar2=None, op0=ALU.mult), reads=[xt, ss], writes=[xn2])
                    em.dma("sp", lambda g, t=t: g.dma_start(out=xn2_d[t * 128:(t + 1) * 128, :], in_=xn2[:]), reads=[xn2], writes=[B_xn2], shared=True)
                    for qq in range(4):
                        ps = nextps()
                        for q4 in range(4):
                            q = qq * 4 + q4
                            em.tr(ps[:, q4 * 128:(q4 + 1) * 128], xn2[:, q:D:16], ident, [xn2], [ps])
                        for q4 in range(4):
                            q = qq * 4 + q4
                            em.op("act", lambda g, q=q, q4=q4, ps=ps: g.activation(out=h2T[:, q, :], in_=ps[:, q4 * 128:(q4 + 1) * 128], func=AF.Identity,
                                                                                  scale=A2x[:, q:q + 1], bias=B2x[:, q:q + 1]), reads=[ps, A2x, B2x], writes=[h2T], shared=True)
                    ps = nextps()
                    for q in range(16):
                        em.mm(ps[:, 0:NE], h2T[:, q, :], wrt[:, q, :], q == 0, False, [h2T, wrt], [ps])
                    em.mm(ps[:, 0:NE], ones[0:1, 0:128], brt[0:1, :], False, True, [ones, brt], [ps])
                    em.op("dve", lambda g, ps=ps, t=t: g.tensor_copy(out=lg[:, t, :], in_=ps[:, 0:NE]), reads=[ps], writes=[lg], shared=True)
            probe_ind("before routing")
            nm = em.tile("nm", [128, 1]); e4 = em.tile("e4", [128, 4]); es = em.tile("es", [128, 1])
            for t in range(NTX):
                em.op("dve", lambda g, t=t: g.max(out=top8[:, t, :], in_=lg[:, t, :]), reads=[lg], writes=[top8], shared=True)
                em.op("dve", lambda g, t=t: g.tensor_scalar(out=mask_all[:, t, :], in0=lg[:, t, :], scalar1=top8[:, t, 3:4], scalar2=None, op0=ALU.is_ge),
                      reads=[lg, top8], writes=[mask_all], shared=True)
                em.op("dve", lambda g, t=t: g.tensor_scalar(out=nm[:], in0=top8[:, t, 0:1], scalar1=-1.0, scalar2=None, op0=ALU.mult), reads=[top8], writes=[nm])
                em.op("act", lambda g, t=t: g.activation(out=e4[:], in_=top8[:, t, 0:4], func=AF.Exp, bias=nm[:, 0:1], accum_out=es[:]), reads=[top8, nm], writes=[e4, es])
                em.op("dve", lambda g: g.reciprocal(out=es[:], in_=es[:]), reads=[es], writes=[es])
                em.op("dve", lambda g, t=t: g.tensor_scalar(out=gates[:, t, :], in0=e4[:], scalar1=es[:, 0:1], scalar2=None, op0=ALU.mult), reads=[e4, es], writes=[gates], shared=True)
                ps = nextps()
                em.mm(ps[:, 0:NE], SLT[:], mask_all[:, t, :], True, t == 0, [SLT, mask_all], [ps])
                for tp in range(t):
                    em.mm(ps[:, 0:NE], ones[:], mask_all[:, tp, :], False, tp == t - 1, [ones, mask_all], [ps])
                em.op("dve", lambda g, ps=ps, t=t: g.tensor_copy(out=rank[:, t, :], in_=ps[:, 0:NE]), reads=[ps], writes=[rank], shared=True)
            cntb = em.tile("cntb", [128, NE]); nblk = em.tile("nblk", [128, NE]); cmp = em.tile("cmp", [128, NE]); pend = em.tile("pend", [128, NE]); pst = em.tile("pst", [128, NE])
            ps = nextps()
            for t in range(NTX):
                em.mm(ps[:, 0:NE], ones[:], mask_all[:, t, :], t == 0, t == NTX - 1, [ones, mask_all], [ps])
            em.op("dve", lambda g: g.tensor_copy(out=cntb[:], in_=ps[:, 0:NE]), reads=[ps], writes=[cntb])
            em.op("dve", lambda g: g.tensor_scalar(out=nblk[:], in0=cntb[:], scalar1=0.5, scalar2=None, op0=ALU.is_gt), reads=[cntb], writes=[nblk])
            for m in range(1, 16):
                em.op("dve", lambda g, m=m: g.tensor_scalar(out=cmp[:], in0=cntb[:], scalar1=128.0 * m + 0.5, scalar2=None, op0=ALU.is_gt), reads=[cntb], writes=[cmp])
                em.op("dve", lambda g: g.tensor_tensor(out=nblk[:], in0=nblk[:], in1=cmp[:], op=ALU.add), reads=[nblk, cmp], writes=[nblk])
            em.op("dve", lambda g: g.tensor_tensor_scan(out=pend[:], data0=ones[:, 0:NE], data1=nblk[:], initial=0.0, op0=ALU.mult, op1=ALU.add), reads=[ones, nblk], writes=[pend])
            em.op("dve", lambda g: g.tensor_tensor(out=pst[:], in0=pend[:], in1=nblk[:], op=ALU.subtract), reads=[pend, nblk], writes=[pst])
            em.op("dve", lambda g: g.tensor_scalar(out=pst[:], in0=pst[:], scalar1=128.0, scalar2=None, op0=ALU.mult), reads=[pst], writes=[pst])
            em.op("dve", lambda g: g.tensor_scalar(out=pend[:], in0=pend[:], scalar1=128.0, scalar2=None, op0=ALU.mult), reads=[pend], writes=[pend])
            em.op("dve", lambda g: g.tensor_tensor(out=rank[:], in0=rank[:], in1=pst[:].unsqueeze(1).to_broadcast([128, NTX, NE]), op=ALU.add), reads=[rank, pst], writes=[rank])
            destf = em.tile("destf", [128, NTX, TOPK]); eqt = em.tile("eqt", [128, NE])
            for t in range(NTX):
                for k in range(TOPK):
                    em.op("dve", lambda g, t=t, k=k: g.tensor_scalar(out=eqt[:], in0=lg[:, t, :], scalar1=top8[:, t, k:k + 1], scalar2=None, op0=ALU.is_equal), reads=[lg, top8], writes=[eqt])
                    em.op("dve", lambda g, t=t: g.tensor_tensor(out=eqt[:], in0=eqt[:], in1=rank[:, t, :], op=ALU.mult), reads=[eqt, rank], writes=[eqt])
                    em.op("dve", lambda g, t=t, k=k: g.reduce_sum(out=destf[:, t, k:k + 1], in_=eqt[:], axis=mybir.AxisListType.X), reads=[eqt], writes=[destf], shared=True)
            em.op("dve", lambda g: g.tensor_copy(out=desti[:], in_=destf[:].rearrange("p t k -> p (t k)")), reads=[destf], writes=[desti])
            jv = em.tile("jv", [128, NBLK]); bexp = em.tile("bexp", [128, NBLK]); cmpj = em.tile("cmpj", [128, NBLK]); pio = em.tile("pio", [128, 1])
            em.op("pool", lambda g: g.iota(jv[:], pattern=[[128, NBLK]], base=0, channel_multiplier=0, allow_small_or_imprecise_dtypes=True), writes=[jv])
            em.op("pool", lambda g: g.iota(pio[:], pattern=[[0, 1]], base=0, channel_multiplier=1, allow_small_or_imprecise_dtypes=True), writes=[pio])
            em.op("pool", lambda g: g.memset(bexp[:], 0.0), writes=[bexp])
            for e_ in range(NE):
                em.op("dve", lambda g, e_=e_: g.tensor_scalar(out=cmpj[:], in0=jv[:], scalar1=pend[:, e_:e_ + 1], scalar2=None, op0=ALU.is_ge), reads=[jv, pend], writes=[cmpj])
                em.op("dve", lambda g: g.tensor_tensor(out=bexp[:], in0=bexp[:], in1=cmpj[:], op=ALU.add), reads=[bexp, cmpj], writes=[bexp])
            em.op("dve", lambda g: g.tensor_scalar(out=bexp[:], in0=bexp[:], scalar1=float(NE - 1), scalar2=None, op0=ALU.min), reads=[bexp], writes=[bexp])
            em.op("dve", lambda g: g.tensor_copy(out=bidx[:], in_=bexp[:]), reads=[bexp], writes=[bidx])
            em.op("dve", lambda g: g.tensor_scalar(out=bexp[:], in0=bexp[:], scalar1=128.0, scalar2=None, op0=ALU.mult), reads=[bexp], writes=[bexp])
            em.op("dve", lambda g: g.tensor_scalar(out=bexp[:], in0=bexp[:], scalar1=pio[:, 0:1], scalar2=None, op0=ALU.add), reads=[bexp, pio], writes=[bexp])
            bexp4 = em.tile("bexp4", [128, NBLK, 4])
            for quad in range(4):
                em.op("dve", lambda g, quad=quad: g.tensor_scalar(out=bexp4[:, :, quad], in0=bexp[:], scalar1=4.0, scalar2=float(quad), op0=ALU.mult, op1=ALU.add),
                      reads=[bexp], writes=[bexp4], shared=True)
            em.op("dve", lambda g: g.tensor_copy(out=widx[:], in_=bexp4[:].rearrange("p j q -> p (j q)")), reads=[bexp4], writes=[widx])
            if dbg:
                dump("lg", lg, [128, NTX, NE]); dump("destf", destf, [128, NTX, TOPK]); dump("gates", gates, [128, NTX, TOPK]); dump("bexp", bexp, [128, NBLK])
            probe_ind("before scatter")
            for t in range(NTX):
                em.dma("sp", lambda g, t=t: g.dma_start(out=xn2[:], in_=xn2_d[t * 128:(t + 1) * 128, :]), reads=[B_xn2], writes=[xn2])
                for k in range(TOPK):
                    probe_ind("pre-idxcol xn2", src=xn2)
                    ic = idxcol(desti, t * TOPK + k)
                    probe_ind("post-idxcol zt", ic_=ic)
                    probe_ind("post-idxcol xn2", src=xn2, ic_=ic)
                    em.dma("pool", lambda g, ic=ic: g.indirect_dma_start(out=xs_d[:, :], out_offset=bass.IndirectOffsetOnAxis(ap=ic[:, :], axis=0),
                                                                        in_=xn2[:, :], in_offset=None, bounds_check=bc_blk, oob_is_err=False),
                           reads=[xn2, ic], writes=[B_xs], shared=True)
        if upto <= 4:
            em.barrier()
            print("instructions:", em.ninstr)
            return nc

        with em.phase():
            w2 = [w.rearrange("(e p q4 ql) c -> (e p q4) (ql c)", p=128, q4=4, ql=4) for w in (w_gate, w_up, w_down)]
            bsrc = (b_gate, b_up, b_down)
            wq = [em.tile("wq%d" % i, [128, 4 * D], F32R) for i in range(3)]
            xs_t = em.tile("xs_t", [128, D]); xsT = em.tile("xsT", [128, 16, 128]); actv = em.tile("actv", [128, D]); actT = em.tile("actT", [128, 16, 128])
            ysb = em.tile("ysb", [128, D]); gsb = em.tile("gsb", [128, 512]); usb = em.tile("usb", [128, 512]); sgm = em.tile("sgm", [128, 512])
            brow3 = [em.tile("brow3_%d" % i, [2, D], F32R) for i in range(3)]
            wqi = 0
            for j in range(NBLK):
                em.dma("sp", lambda g, j=j: g.dma_start(out=xs_t[:], in_=xs_d[j * 128:(j + 1) * 128, :]), reads=[B_xs], writes=[xs_t])
                bic = idxcol(bidx, j)
                for i in range(3):
                    em.dma("pool", lambda g, i=i: g.indirect_dma_start(out=brow3[i][0:2, :], out_offset=None, in_=bsrc[i][:, :],
                                                                     in_offset=bass.IndirectOffsetOnAxis(ap=bic[0:2, :], axis=0),
                                                                     bounds_check=bc_b, oob_is_err=False), reads=[bic], writes=[brow3[i]])
                for qq in range(4):
                    ps = nextps()
                    for q4 in range(4):
                        q = qq * 4 + q4
                        em.tr(ps[:, q4 * 128:(q4 + 1) * 128], xs_t[:, q:D:16], ident, [xs_t], [ps])
                    for q4 in range(4):
                        q = qq * 4 + q4
                        em.op("act", lambda g, q=q, q4=q4, ps=ps: g.activation(out=xsT[:, q, :].bitcast(F32R), in_=ps[:, q4 * 128:(q4 + 1) * 128], func=AF.Identity,
                                                                              scale=A2x[:, q:q + 1], bias=B2x[:, q:q + 1]), reads=[ps, A2x, B2x], writes=[xsT], shared=True)
                for quad in range(4):
                    wts = []
                    wic = idxcol(widx, j * 4 + quad)
                    for i in range(2):
                        wt = wq[wqi % 3]; wqi += 1
                        em.dma("pool", lambda g, wt=wt, i=i, j=j, quad=quad: g.indirect_dma_start(
                            out=wt[:, :], out_offset=None, in_=w2[i][:, :],
                            in_offset=bass.IndirectOffsetOnAxis(ap=wic[:, :], axis=0), bounds_check=bc_w, oob_is_err=False), reads=[wic], writes=[wt])
                        wts.append(wt)
                    for i in range(2):
                        for cg in range(4):
                            ps = PS[i * 4 + cg]
                            for ql in range(4):
                                q = quad * 4 + ql
                                em.mm(ps[:, :], xsT[:, q, :], wts[i][:, ql * D + cg * 512:ql * D + (cg + 1) * 512], q == 0, False, [xsT, wts[i]], [ps], r=True)
                for i in range(2):
                    for cg in range(4):
                        ps = PS[i * 4 + cg]
                        em.mm(ps[:, :], ones_r[0:1, 0:128], brow3[i][0:1, cg * 512:(cg + 1) * 512], False, True, [ones_r, brow3[i]], [ps], r=True)
                for cg in range(4):
                    cs_ = slice(cg * 512, (cg + 1) * 512)
                    em.op("dve", lambda g, cg=cg: g.tensor_scalar(out=gsb[:], in0=PS[cg][:, :], scalar1=LIMIT, scalar2=None, op0=ALU.min), reads=[PS[cg]], writes=[gsb])
                    em.op("act", lambda g: g.activation(out=sgm[:], in_=gsb[:], func=AF.Sigmoid, scale=ALPHA), reads=[gsb], writes=[sgm])
                    em.op("dve", lambda g, cg=cg: g.tensor_scalar(out=usb[:], in0=PS[4 + cg][:, :], scalar1=LIMIT, scalar2=-LIMIT, op0=ALU.min, op1=ALU.max), reads=[PS[4 + cg]], writes=[usb])
                    em.op("dve", lambda g: g.scalar_tensor_tensor(out=usb[:], in0=usb[:], scalar=1.0, in1=gsb[:], op0=ALU.add, op1=ALU.mult), reads=[usb, gsb], writes=[usb])
                    em.op("dve", lambda g, cs_=cs_: g.tensor_tensor(out=actv[:, cs_], in0=usb[:], in1=sgm[:], op=ALU.mult), reads=[usb, sgm], writes=[actv], shared=True)
                for qq in range(4):
                    ps = nextps()
                    for q4 in range(4):
                        q = qq * 4 + q4
                        em.tr(ps[:, q4 * 128:(q4 + 1) * 128], actv[:, q:D:16], ident, [actv], [ps])
                    for q4 in range(4):
                        q = qq * 4 + q4
                        evac(actT[:, q, :].bitcast(F32R), ps[:, q4 * 128:(q4 + 1) * 128], [ps], [actT])
                for quad in range(4):
                    wt = wq[wqi % 3]; wqi += 1
                    wic = idxcol(widx, j * 4 + quad)
                    em.dma("pool", lambda g, wt=wt, j=j, quad=quad: g.indirect_dma_start(
                        out=wt[:, :], out_offset=None, in_=w2[2][:, :],
                        in_offset=bass.IndirectOffsetOnAxis(ap=wic[:, :], axis=0), bounds_check=bc_w, oob_is_err=False), reads=[wic], writes=[wt])
                    for cg in range(4):
                        ps = PS[cg]
                        for ql in range(4):
                            q = quad * 4 + ql
                            em.mm(ps[:, :], actT[:, q, :], wt[:, ql * D + cg * 512:ql * D + (cg + 1) * 512], q == 0, False, [actT, wt], [ps], r=True)
                for cg in range(4):
                    em.mm(PS[cg][:, :], ones_r[0:1, 0:128], brow3[2][0:1, cg * 512:(cg + 1) * 512], False, True, [ones_r, brow3[2]], [PS[cg]], r=True)
                    evac(ysb[:, cg * 512:(cg + 1) * 512], PS[cg][:, :], [PS[cg]], [ysb])
                em.dma("sp", lambda g, j=j: g.dma_start(out=ys_d[j * 128:(j + 1) * 128, :], in_=ysb[:]), reads=[ysb], writes=[B_ys], shared=True)

        with em.phase():
            gt2_bc = em.tile("gt2_bc", [128, D]); now_bc = em.tile("now_bc", [128, D])
            em.dma("sp", lambda g: g.dma_start(out=gt2_bc[:], in_=modrows[0, 5 * D:6 * D].partition_broadcast(128)), reads=[B_modrows], writes=[gt2_bc])
            em.dma("sp", lambda g: g.dma_start(out=now_bc[:], in_=now_d.partition_broadcast(128)), writes=[now_bc])
            yk = [em.tile("yk%d" % i, [128, D]) for i in range(4)]
            x1b = [em.tile("x1b%d" % i, [128, D]) for i in range(2)]; acc = em.tile("acc", [128, D]); junk = em.tile("junk5", [128, D]); ss = em.tile("ss5", [128, 1])
            ob5 = [em.tile("ob5_%d" % i, [128, D]) for i in range(2)]
            for t in range(NTX):
                xb = x1b[t % 2]; ob = ob5[t % 2]
                em.dma("sp", lambda g, xb=xb, t=t: g.dma_start(out=xb[:], in_=x1_d[t * 128:(t + 1) * 128, :]), reads=[B_x1], writes=[xb])
                for k in range(TOPK):
                    ic = idxcol(desti, t * TOPK + k)
                    em.dma("pool", lambda g, k=k, ic=ic: g.indirect_dma_start(out=yk[k][:, :], out_offset=None, in_=ys_d[:, :],
                                                                            in_offset=bass.IndirectOffsetOnAxis(ap=ic[:, :], axis=0),
                                                                            bounds_check=bc_blk, oob_is_err=False), reads=[B_ys, ic], writes=[yk[k]])
                em.op("dve", lambda g, t=t: g.tensor_scalar(out=acc[:], in0=yk[0][:], scalar1=gates[:, t, 0:1], scalar2=None, op0=ALU.mult), reads=[yk[0], gates], writes=[acc])
                for k in range(1, TOPK):
                    em.op("dve", lambda g, t=t, k=k: g.scalar_tensor_tensor(out=acc[:], in0=yk[k][:], scalar=gates[:, t, k:k + 1], in1=acc[:], op0=ALU.mult, op1=ALU.add),
                          reads=[yk[k], gates, acc], writes=[acc])
                em.op("dve", lambda g: g.tensor_tensor(out=acc[:], in0=acc[:], in1=gt2_bc[:], op=ALU.mult), reads=[acc, gt2_bc], writes=[acc])
                em.op("dve", lambda g, xb=xb: g.tensor_tensor(out=acc[:], in0=acc[:], in1=xb[:], op=ALU.add), reads=[acc, xb], writes=[acc])
                em.op("act", lambda g: g.activation(out=junk[:], in_=acc[:], func=AF.Square, accum_out=ss[:]), reads=[acc], writes=[junk, ss])
                rstd_from_ss(ss, 1, D)
                em.op("dve", lambda g, ob=ob: g.scalar_tensor_tensor(out=ob[:], in0=acc[:], scalar=ss[:, 0:1], in1=now_bc[:], op0=ALU.mult, op1=ALU.mult),
                      reads=[acc, ss, now_bc], writes=[ob])
                em.dma("sp", lambda g, ob=ob, t=t: g.dma_start(out=y_d[t * 128:(t + 1) * 128, :], in_=ob[:]), reads=[ob], writes=[B_y], shared=True)
        em.barrier()
        print("instructions:", em.ninstr)
    return nc


_W_KEYS = ["w_ada", "b_ada", "norm_mix_w", "w_in", "hg_lb_f", "hg_lb_b", "hg_norm_w", "gd_conv_w", "gd_a_log_f", "gd_a_log_b",
           "gd_dt_bias_f", "gd_dt_bias_b", "gd_norm_w", "w_out", "norm_ffn_w", "w_router", "b_router", "w_gate", "b_gate",
           "w_up", "b_up", "w_down", "b_down"]


def kernel(**inputs):
    f32 = lambda a: np.ascontiguousarray(np.asarray(a, dtype=np.float32))
    x = f32(inputs["x"]); c = f32(inputs["c"]); ctx = f32(inputs["ctx"])
    nb = x.shape[0]
    ne = int(np.asarray(inputs["w_router"]).shape[-1])
    shared = {"c_ctx": f32(inputs["c_ctx"]), "norm_out_w": f32(inputs["norm_out_w"])}
    for k in _W_KEYS:
        a = f32(inputs[k])[0]
        if k in ("w_gate", "w_up", "w_down"):
            a = a.reshape(ne * D, D)
        shared[k] = np.ascontiguousarray(a)
    shared["hg_lb_f"] = f32(inputs["hg_lb_f"]); shared["hg_lb_b"] = f32(inputs["hg_lb_b"])
    nc = build_program(NE=ne)
    in_maps = []
    for b in range(nb):
        m = dict(shared)
        m["x"] = x[b]; m["c"] = c[b]; m["ctx"] = ctx[b]
        in_maps.append(m)
    res = run_bass_kernel_spmd(nc, in_maps, core_ids=list(range(nb)))
    return np.stack([r["y"] for r in res.results], axis=0).astype(np.float32)
```

```python
import contextlib
import numpy as np
import concourse.bass as bass
import concourse.mybir as mybir
from concourse.bass_utils import run_bass_kernel_spmd

F32 = mybir.dt.float32
I32 = mybir.dt.int32
F32R = mybir.dt.float32r
AF = mybir.ActivationFunctionType
ALU = mybir.AluOpType

D = 2048
SEQ = 2048
CTX = 256
NTOK = SEQ + CTX
NT = NTOK // 128
NTX = SEQ // 128
HGW = 1024
GDW = 1024
NH = 8
IN_DIM = 9248
TOPK = 4
EPS = 1e-6
LIMIT = 7.0
ALPHA = 1.702
SAME_ENG_GAP = 2


class Sem:
    def __init__(self, handle, name):
        self.h = handle
        self.name = name
        self.count = 0


class Buf:
    def __init__(self, name):
        self.name = name
        self.ws = {}
        self.r = {}
        self.ld = None
        self.st = None


class T(Buf):
    def __init__(self, em, name, shape, dtype=F32, psum=False):
        super().__init__(name)
        self.is_tile = True
        self.is_psum = psum
        if psum:
            self.t = em.stack.enter_context(em.nc.psum_tensor(name, list(shape), dtype))
        else:
            self.t = em.stack.enter_context(em.nc.sbuf_tensor(name, list(shape), dtype))

    def __getitem__(self, idx):
        return self.t[idx]


class Emitter:
    def __init__(self, nc, stack, n_dma_sems=88):
        self.nc = nc
        self.gstack = stack
        self.stack = stack
        self.eng = {"pe": nc.tensor, "act": nc.scalar, "dve": nc.vector, "pool": nc.gpsimd, "sp": nc.sync}
        self.esem = {k: Sem(stack.enter_context(nc.semaphore("e_" + k)), k) for k in self.eng}
        self.seen = {k: {} for k in self.eng}
        self.pool = [Sem(stack.enter_context(nc.semaphore("d%d" % i)), "d%d" % i) for i in range(n_dma_sems)]
        self.used = []
        self.ninstr = 0
        self.phase_tiles = []
        self.log = None

    def get_sem(self):
        s = self.pool.pop()
        self.used.append(s)
        return s

    def tile(self, name, shape, dtype=F32):
        t = T(self, name, shape, dtype)
        self.phase_tiles.append(t)
        return t

    def psum(self, name, shape, dtype=F32):
        return T(self, name, shape, dtype, psum=True)

    def _waits(self, e, reads, writes, shared=False):
        need = {}

        def add(tk):
            if tk is None:
                return
            sem, val, is_dma = tk
            if is_dma:
                val = sem.count
            if e == "pe" and sem is self.esem["pe"]:
                return
            if sem is self.esem.get(e) and sem.count - val >= SAME_ENG_GAP:
                return
            if need.get(sem, 0) < val:
                need[sem] = val

        for b in reads:
            for tk in b.ws.values():
                add(tk)
            if getattr(b, "is_psum", False):
                for tk in b.r.values():
                    if tk[0] is not self.esem.get(e):
                        add(tk)
        for b in writes:
            if not shared:
                for tk in b.ws.values():
                    add(tk)
            for tk in b.r.values():
                add(tk)
        for sem, val in need.items():
            if self.seen[e].get(sem, 0) < val:
                self.eng[e].wait_ge(sem.h, val)
                self.seen[e][sem] = val
                self.ninstr += 1
                if self.log is not None:
                    self.log.append((e, "wait", sem.name, val))

    def _record(self, tk, reads, writes, shared):
        sem = tk[0]
        for b in reads:
            b.r[sem] = tk
        for b in writes:
            if shared:
                b.ws[sem] = tk
            else:
                b.ws = {sem: tk}
                b.r = {}

    def op(self, e, fn, reads=(), writes=(), shared=False):
        self._waits(e, reads, writes, shared)
        ins = fn(self.eng[e])
        sem = self.esem[e]
        sem.count += 1
        ins.then_inc(sem.h, 1)
        tk = (sem, sem.count, False)
        if self.log is not None:
            self.log.append((e, "inc", sem.name, 1))
        self._record(tk, reads, writes, shared)
        self.ninstr += 1
        return ins

    def dma(self, q, fn, reads=(), writes=(), shared=False):
        self._waits(q, reads, writes, shared)
        sem = None
        for b in writes:
            if isinstance(b, T):
                if b.ld is None:
                    b.ld = self.get_sem()
                sem = b.ld
                break
        if sem is None:
            for b in reads:
                if isinstance(b, T):
                    if b.st is None:
                        b.st = self.get_sem()
                    sem = b.st
                    break
        assert sem is not None
        ins = fn(self.eng[q])
        sem.count += 16
        ins.then_inc(sem.h, 16)
        tk = (sem, sem.count, True)
        if self.log is not None:
            self.log.append((q, "inc", sem.name, 16))
        self._record(tk, reads, writes, shared)
        self.ninstr += 1
        return ins

    def barrier(self):
        sems = list(self.esem.values()) + list(self.used)
        for e in self.eng:
            for s in sems:
                if s is self.esem[e] or s.count == 0:
                    continue
                if self.seen[e].get(s, 0) < s.count:
                    self.eng[e].wait_ge(s.h, s.count)
                    self.seen[e][s] = s.count
                    self.ninstr += 1
                    if self.log is not None:
                        self.log.append((e, "wait", s.name, s.count))

    @contextlib.contextmanager
    def phase(self):
        old_stack, old_tiles = self.stack, self.phase_tiles
        st = contextlib.ExitStack()
        self.stack = st
        self.phase_tiles = []
        try:
            with st:
                yield
                self.barrier()
                for t in self.phase_tiles:
                    for s in (t.ld, t.st):
                        if s is not None:
                            self.used.remove(s)
                            self.pool.append(s)
        finally:
            self.stack = old_stack
            self.phase_tiles = old_tiles

    def mm(self, out, lhsT, rhs, start, stop, reads, writes, shared=False, r=False):
        if r:
            if lhsT.dtype != F32R:
                lhsT = lhsT.bitcast(F32R)
            if rhs.dtype != F32R:
                rhs = rhs.bitcast(F32R)
        return self.op("pe", lambda g: g.matmul(out, lhsT, rhs, start=start, stop=stop), reads, writes, shared)

    def tr(self, out, in_, ident, reads, writes, shared=False, k=128):
        return self.op("pe", lambda g: g.transpose(out, in_, ident[0:k, 0:k]), list(reads) + [ident], writes, shared)


def build_program(NE=32, dbg=False, upto=99, nhg=NH, ngd=NH):
    NBLK = (SEQ * TOPK) // 128 + NE
    nc = bass.Bass("TRN2", target_bir_lowering=False)

    def din(name, shape, dt=F32):
        return nc.dram_tensor(name, list(shape), dt, kind="ExternalInput").ap()

    def dscr(name, shape, dt=F32, out=False):
        return nc.dram_tensor(name, list(shape), dt, kind="ExternalOutput" if (out or dbg) else "Internal").ap()

    x_d = din("x", [SEQ, D]); ctx_d = din("ctx", [CTX, D]); c_d = din("c", [D]); cctx_d = din("c_ctx", [D])
    w_ada = din("w_ada", [D, 6 * D], F32R); b_ada = din("b_ada", [6 * D], F32R); nmw_d = din("norm_mix_w", [D])
    w_in = din("w_in", [D, IN_DIM], F32R); lbf_d = din("hg_lb_f", [2, HGW]); lbb_d = din("hg_lb_b", [2, HGW])
    hgnw_d = din("hg_norm_w", [128]); conv_d = din("gd_conv_w", [3, 3 * GDW])
    alf_d = din("gd_a_log_f", [NH]); alb_d = din("gd_a_log_b", [NH])
    dtf_d = din("gd_dt_bias_f", [NH]); dtb_d = din("gd_dt_bias_b", [NH]); gdnw_d = din("gd_norm_w", [128])
    w_out = din("w_out", [D, D], F32R); nfw_d = din("norm_ffn_w", [D]); w_rt = din("w_router", [D, NE])
    b_rt = din("b_router", [NE]); w_gate = din("w_gate", [NE * D, D], F32R); b_gate = din("b_gate", [NE, D], F32R)
    w_up = din("w_up", [NE * D, D], F32R); b_up = din("b_up", [NE, D], F32R); w_down = din("w_down", [NE * D, D], F32R)
    b_down = din("b_down", [NE, D], F32R); now_d = din("norm_out_w", [D])
    y_d = nc.dram_tensor("y", [SEQ, D], F32, kind="ExternalOutput").ap()

    modrows = dscr("modrows", [2, 6 * D])
    pfm = dscr("pfm", [6144, NTOK])
    ptm = dscr("ptm", [NTOK, 3104])
    mixT = dscr("mixT", [D, SEQ], F32R)
    x1_d = dscr("x1", [SEQ, D])
    xn2_d = dscr("xn2", [SEQ, D])
    xs_d = dscr("xs", [NBLK * 128, D])
    ys_d = dscr("ys", [NBLK * 128, D])
    B_modrows = Buf("modrows"); B_pfm = Buf("pfm"); B_ptm = Buf("ptm"); B_mixT = Buf("mixT")
    B_x1 = Buf("x1"); B_xn2 = Buf("xn2"); B_xs = Buf("xs"); B_ys = Buf("ys"); B_y = Buf("y")

    gst = contextlib.ExitStack()
    with gst:
        em = Emitter(nc, gst)
        em.log = [] if dbg else None
        nc._em = em
        PS = [em.psum("ps%d" % i, [128, 512]) for i in range(8)]
        ident = em.tile("ident", [128, 128]); ones = em.tile("ones", [128, 128])
        em.op("pool", lambda g: g.memset(ones[:], 1.0), writes=[ones])
        ones_r = em.tile("ones_r", [1, 128])
        em.op("dve", lambda g: g.tensor_copy(out=ones_r[:].bitcast(F32R), in_=ones[0:1, :]), reads=[ones], writes=[ones_r])

        def aff_mask(out_t, cmp, sgn=1):
            em.op("pool", lambda g: g.affine_select(out=out_t[:], in_=ones[:], pattern=[[-sgn, 128]], compare_op=cmp,
                                                     fill=0.0, base=0, channel_multiplier=sgn), reads=[ones], writes=[out_t])

        aff_mask(ident, ALU.is_equal)
        cnt = {"rr": 0}
        dumps = {}

        def dump(name, tl, shape):
            if not dbg:
                return
            d = nc.dram_tensor("dbg_" + name, list(shape), F32, kind="ExternalOutput").ap()
            em.dma("sp", lambda g: g.dma_start(out=d, in_=tl[:]), reads=[tl], writes=[Buf("dbg_" + name)])

        def evac(out_ap, in_ap, reads, writes):
            cnt["rr"] += 1
            if cnt["rr"] % 2:
                em.op("act", lambda g: g.copy(out=out_ap, in_=in_ap), reads, writes)
            else:
                em.op("dve", lambda g: g.tensor_copy(out=out_ap, in_=in_ap), reads, writes)

        def rstd_from_ss(ss, n, width):
            em.op("dve", lambda g: g.tensor_scalar(out=ss[:, 0:n], in0=ss[:, 0:n], scalar1=1.0 / width, scalar2=EPS,
                                                   op0=ALU.mult, op1=ALU.add), reads=[ss], writes=[ss])
            em.op("act", lambda g: g.activation(out=ss[:, 0:n], in_=ss[:, 0:n], func=AF.Sqrt), reads=[ss], writes=[ss])
            em.op("dve", lambda g: g.reciprocal(out=ss[:, 0:n], in_=ss[:, 0:n]), reads=[ss], writes=[ss])

        A1x = em.tile("A1x", [128, 16]); B1x = em.tile("B1x", [128, 16])
        A1c = em.tile("A1c", [128, 16]); B1c = em.tile("B1c", [128, 16])
        A2x = em.tile("A2x", [128, 16]); B2x = em.tile("B2x", [128, 16])

        with em.phase():
            cs = em.tile("cs", [128, 16, 2]); craw = em.tile("craw", [128, 2, 16])
            em.dma("sp", lambda g: g.dma_start(out=craw[:, 0, :], in_=c_d.rearrange("(p q) -> p q", q=16)), writes=[craw], shared=True)
            em.dma("sp", lambda g: g.dma_start(out=craw[:, 1, :], in_=cctx_d.rearrange("(p q) -> p q", q=16)), writes=[craw], shared=True)
            for r in range(2):
                em.op("act", lambda g, r=r: g.activation(out=cs[:, :, r].bitcast(F32R), in_=craw[:, r, :], func=AF.Silu), reads=[craw], writes=[cs], shared=True)
            brow = em.tile("brow", [1, 6 * D], F32R)
            em.dma("pool", lambda g: g.dma_start(out=brow[:], in_=b_ada.rearrange("(o n) -> o n", o=1)), writes=[brow])
            wv = w_ada.rearrange("(p q) c -> p q c", q=16)
            wring = [em.tile("adaw%d" % i, [128, 16, 512], F32R) for i in range(3)]
            mrow = [em.tile("mrow%d" % i, [2, 512]) for i in range(2)]
            for s in range(24):
                wt = wring[s % 3]
                em.dma("pool", lambda g, wt=wt, s=s: g.dma_start(out=wt[:], in_=wv[:, :, s * 512:(s + 1) * 512]), writes=[wt])
                ps = PS[s % 2]
                for q in range(16):
                    em.mm(ps[0:2, :], cs[:, q, :], wt[:, q, :], q == 0, False, reads=[cs, wt], writes=[ps], r=True)
                em.mm(ps[0:2, :], ones_r[0:1, 0:2], brow[0:1, s * 512:(s + 1) * 512], False, True, reads=[ones_r, brow], writes=[ps], r=True)
                mr = mrow[s % 2]
                evac(mr[:], ps[0:2, :], [ps], [mr])
                em.dma("sp", lambda g, mr=mr, s=s: g.dma_start(out=modrows[:, s * 512:(s + 1) * 512], in_=mr[:]), reads=[mr], writes=[B_modrows], shared=True)
            tmp = em.tile("modtmp", [128, 6, 16]); nw = em.tile("nw", [128, 2, 16])
            em.dma("sp", lambda g: g.dma_start(out=nw[:, 0, :], in_=nmw_d.rearrange("(p q) -> p q", q=16)), writes=[nw], shared=True)
            em.dma("sp", lambda g: g.dma_start(out=nw[:, 1, :], in_=nfw_d.rearrange("(p q) -> p q", q=16)), writes=[nw], shared=True)

            def col(row, chunk, dst):
                em.dma("sp", lambda g: g.dma_start(out=dst, in_=modrows[row, chunk * D:(chunk + 1) * D].rearrange("(p q) -> p q", q=16)),
                       reads=[B_modrows], writes=[tmp], shared=True)

            col(0, 0, tmp[:, 0, :]); col(0, 1, tmp[:, 1, :]); col(1, 0, tmp[:, 2, :]); col(1, 1, tmp[:, 3, :])
            col(0, 3, tmp[:, 4, :]); col(0, 4, tmp[:, 5, :])

            def mkA(dst, sc_idx, nwi):
                em.op("dve", lambda g: g.scalar_tensor_tensor(out=dst[:], in0=tmp[:, sc_idx, :], scalar=1.0, in1=nw[:, nwi, :],
                                                             op0=ALU.add, op1=ALU.mult), reads=[tmp, nw], writes=[dst])

            mkA(A1x, 1, 0); mkA(A1c, 3, 0); mkA(A2x, 5, 1)
            em.op("dve", lambda g: g.tensor_copy(out=B1x[:], in_=tmp[:, 0, :]), reads=[tmp], writes=[B1x])
            em.op("dve", lambda g: g.tensor_copy(out=B1c[:], in_=tmp[:, 2, :]), reads=[tmp], writes=[B1c])
            em.op("dve", lambda g: g.tensor_copy(out=B2x[:], in_=tmp[:, 4, :]), reads=[tmp], writes=[B2x])

        FM = [(0, 0), (512, 512), (1024, 1024), (1536, 1536), (2048, 2048), (2560, 2560),
              (5120, 3072), (5632, 3584), (6144, 4096), (6656, 4608), (7168, 5120), (7680, 5632)]
        TM = [(3072, 0, 512), (3584, 512, 512), (4096, 1024, 512), (4608, 1536, 512),
              (8192, 2048, 512), (8704, 2560, 512), (9216, 3072, 32)]
        with em.phase():
            wv = w_in.rearrange("(p q) c -> p q c", q=16)
            hxT = em.tile("hxT", [128, 16, 512])
            xt2 = [em.tile("xt%d" % i, [128, D]) for i in range(2)]
            xn = em.tile("xn", [128, D]); junk = em.tile("junk1", [128, D]); ss = em.tile("ss1", [128, 1])
            wring = [em.tile("winw%d" % i, [128, 16, 512], F32R) for i in range(3)]
            ob = [em.tile("ob%d" % i, [128, 512]) for i in range(4)]
            groups = [(0, 2, ctx_d, A1c, B1c)] + [(2 + 4 * g, 4, x_d, A1x, B1x) for g in range(4)]
            ti = 0; wi = 0; oi = 0; pi = 0
            for (t0, ntile, src, A1, B1) in groups:
                ntok = ntile * 128
                for j in range(ntile):
                    row0 = (t0 + j) * 128 - (0 if src is ctx_d else CTX)
                    xt = xt2[ti % 2]; ti += 1
                    em.dma("sp", lambda g, xt=xt, row0=row0, src=src: g.dma_start(out=xt[:], in_=src[row0:row0 + 128, :]), writes=[xt])
                    em.op("act", lambda g, xt=xt: g.activation(out=junk[:], in_=xt[:], func=AF.Square, accum_out=ss[:]), reads=[xt], writes=[junk, ss])
                    rstd_from_ss(ss, 1, D)
                    em.op("dve", lambda g, xt=xt: g.tensor_scalar(out=xn[:], in0=xt[:], scalar1=ss[:, 0:1], scalar2=None, op0=ALU.mult),
                          reads=[xt, ss], writes=[xn])
                    for qq in range(4):
                        ps = PS[pi % 8]; pi += 1
                        for q4 in range(4):
                            q = qq * 4 + q4
                            em.tr(ps[:, q4 * 128:(q4 + 1) * 128], xn[:, q:D:16], ident, [xn], [ps])
                        for q4 in range(4):
                            q = qq * 4 + q4
                            em.op("act", lambda g, q=q, q4=q4, ps=ps, j=j, A1=A1, B1=B1: g.activation(
                                out=hxT[:, q, j * 128:(j + 1) * 128].bitcast(F32R), in_=ps[:, q4 * 128:(q4 + 1) * 128], func=AF.Identity,
                                scale=A1[:, q:q + 1], bias=B1[:, q:q + 1]), reads=[ps, A1, B1], writes=[hxT], shared=True)
                tok0 = t0 * 128
                for (wc, prow) in FM:
                    wt = wring[wi % 3]; wi += 1
                    em.dma("pool", lambda g, wt=wt, wc=wc: g.dma_start(out=wt[:], in_=wv[:, :, wc:wc + 512]), writes=[wt])
                    for sub in range(4):
                        ps = PS[pi % 8]; pi += 1
                        for q in range(16):
                            em.mm(ps[:, 0:ntok], wt[:, q, sub * 128:(sub + 1) * 128], hxT[:, q, 0:ntok], q == 0, q == 15, [wt, hxT], [ps], r=True)
                        o = ob[oi % 4]; oi += 1
                        evac(o[:, 0:ntok], ps[:, 0:ntok], [ps], [o])
                        em.dma("sp", lambda g, o=o, prow=prow, sub=sub, tok0=tok0, ntok=ntok: g.dma_start(
                            out=pfm[prow + sub * 128:prow + (sub + 1) * 128, tok0:tok0 + ntok], in_=o[:, 0:ntok]), reads=[o], writes=[B_pfm], shared=True)
                for (wc, pcol, wd) in TM:
                    wt = wring[wi % 3]; wi += 1
                    em.dma("pool", lambda g, wt=wt, wc=wc, wd=wd: g.dma_start(out=wt[:, :, 0:wd], in_=wv[:, :, wc:wc + wd]), writes=[wt])
                    for j in range(ntile):
                        ps = PS[pi % 8]; pi += 1
                        for q in range(16):
                            em.mm(ps[:, 0:wd], hxT[:, q, j * 128:(j + 1) * 128], wt[:, q, 0:wd], q == 0, q == 15, [wt, hxT], [ps], r=True)
                        o = ob[oi % 4]; oi += 1
                        evac(o[:, 0:wd], ps[:, 0:wd], [ps], [o])
                        em.dma("sp", lambda g, o=o, pcol=pcol, wd=wd, r0=tok0 + j * 128: g.dma_start(
                            out=ptm[r0:r0 + 128, pcol:pcol + wd], in_=o[:, 0:wd]), reads=[o], writes=[B_ptm], shared=True)

        if upto <= 1:
            em.barrier()
            print("instructions:", em.ninstr)
            return nc

        pcnt = {"i": 0}

        def nextps():
            pcnt["i"] += 1
            return PS[pcnt["i"] % 8]

        MLE = em.tile("MLE", [128, 128]); MGE = em.tile("MGE", [128, 128])
        aff_mask(MLE, ALU.is_ge, -1); aff_mask(MGE, ALU.is_ge, 1)

        def gated_norm_store(o_acc, g_tm, w_bc, feat0, jk, ssn, t1, sg, mT_all):
            for n in range(NTX):
                em.op("act", lambda g, n=n: g.activation(out=jk[:], in_=o_acc[:, n, :], func=AF.Square, accum_out=ssn[:, n:n + 1]),
                      reads=[o_acc], writes=[jk, ssn], shared=True)
            rstd_from_ss(ssn, NTX, 128)
            for n in range(NTX):
                em.op("dve", lambda g, n=n: g.scalar_tensor_tensor(out=t1[:], in0=o_acc[:, n, :], scalar=ssn[:, n:n + 1], in1=w_bc[:],
                                                                  op0=ALU.mult, op1=ALU.mult), reads=[o_acc, ssn, w_bc], writes=[t1])
                em.op("act", lambda g, n=n: g.activation(out=sg[:], in_=g_tm[:, n, :], func=AF.Silu), reads=[g_tm], writes=[sg])
                em.op("dve", lambda g: g.tensor_tensor(out=t1[:], in0=t1[:], in1=sg[:], op=ALU.mult), reads=[t1, sg], writes=[t1])
                ps = nextps()
                em.tr(ps[:, 0:128], t1[:], ident, [t1], [ps])
                evac(mT_all[:, n * 128:(n + 1) * 128], ps[:, 0:128], [ps], [mT_all])
            em.dma("pool", lambda g: g.dma_start(out=mixT[feat0:feat0 + 128, :], in_=mT_all[:]), reads=[mT_all], writes=[B_mixT], shared=True)

        with em.phase():
            lbt = em.tile("lbt", [128, 2, 2, 8]); lb = em.tile("lb", [128, 2, 8]); oml = em.tile("oml", [128, 2, 8])
            lbrow = em.tile("lbrow", [32, 128])
            for di, src in enumerate((lbf_d, lbb_d)):
                em.dma("sp", lambda g, di=di, src=src: g.dma_start(out=lbrow[di * 16:(di + 1) * 16, :], in_=src.rearrange("l (h p) -> (l h) p", p=128)),
                       writes=[lbrow], shared=True)
            ps = nextps()
            em.tr(ps[:, 0:32], lbrow[:], ident, [lbrow], [ps], k=32)
            em.op("dve", lambda g: g.tensor_copy(out=lbt[:].rearrange("p d l h -> p (d l h)"), in_=ps[:, 0:32]), reads=[ps], writes=[lbt])
            em.op("dve", lambda g: g.tensor_tensor(out=lb[:], in0=lbt[:, :, 0, :], in1=lbt[:, :, 1, :], op=ALU.subtract), reads=[lbt], writes=[lb])
            em.op("act", lambda g: g.activation(out=lb[:], in_=lb[:], func=AF.Sigmoid), reads=[lb], writes=[lb])
            em.op("dve", lambda g: g.tensor_scalar(out=oml[:], in0=lb[:], scalar1=-1.0, scalar2=1.0, op0=ALU.mult, op1=ALU.add), reads=[lb], writes=[oml])
            hgw_bc = em.tile("hgw_bc", [128, 128])
            em.dma("sp", lambda g: g.dma_start(out=hgw_bc[:], in_=hgnw_d.partition_broadcast(128)), writes=[hgw_bc])
            rst = em.tile("rst", [128, NTOK])
            em.op("pool", lambda g: g.memset(rst[:], 1.0), writes=[rst])
            em.op("pool", lambda g: g.memset(rst[:, 0:NTOK:128], 0.0), writes=[rst])
            qT = em.tile("qT", [128, NTOK]); zz = em.tile("zz", [128, NTOK]); v_tm = em.tile("v_tm", [128, NT, 128])
            g_tm = em.tile("g_tm", [128, NTX, 128])
            Ft = em.tile("Ft", [128, NTOK]); LF = em.tile("LF", [128, NTOK]); Kt = em.tile("Kt", [128, NTOK])
            CUM = em.tile("CUM", [128, NTOK]); CUMB = em.tile("CUMB", [128, NTOK]); At = em.tile("At", [128, NTOK])
            EQ = em.tile("EQ", [128, NTOK]); EK = em.tile("EK", [128, NTOK]); QD = em.tile("QD", [128, NTOK])
            dec = em.tile("dec", [128, NT]); gend = em.tile("gend", [128, NT])
            S2 = [em.tile("S%d" % i, [128, 128]) for i in range(2)]
            scT = [em.tile("scT%d" % i, [128, 128]) for i in range(2)]; kit = [em.tile("kit%d" % i, [128, 128]) for i in range(2)]
            o_acc = em.tile("o_acc", [128, NTX, 128]); mT_all = em.tile("mT_all", [128, SEQ], F32R)
            jk = em.tile("jk", [128, 128]); ssn = em.tile("ssn", [128, NTX]); t1 = em.tile("t1", [128, 128]); sg = em.tile("sg", [128, 128])
            v3 = lambda t: t[:].rearrange("p (n w) -> p n w", w=128)
            for h in range(nhg):
                em.dma("sp", lambda g, h=h: g.dma_start(out=qT[:], in_=pfm[h * 128:(h + 1) * 128, :]), reads=[B_pfm], writes=[qT])
                em.dma("pool", lambda g, h=h: g.dma_start(out=v_tm[:], in_=ptm[:, h * 128:(h + 1) * 128].rearrange("(n p) e -> p n e", p=128)),
                       reads=[B_ptm], writes=[v_tm])
                em.dma("pool", lambda g, h=h: g.dma_start(out=g_tm[:], in_=ptm[CTX:NTOK, 1024 + h * 128:1024 + (h + 1) * 128].rearrange("(n p) e -> p n e", p=128)),
                       reads=[B_ptm], writes=[g_tm])
                for di in range(2):
                    zrow = 1024 + di * 1024 + h * 128
                    em.dma("sp", lambda g, zrow=zrow: g.dma_start(out=zz[:], in_=pfm[zrow:zrow + 128, :]), reads=[B_pfm], writes=[zz])
                    em.op("act", lambda g: g.activation(out=Ft[:], in_=zz[:], func=AF.Sigmoid), reads=[zz], writes=[Ft])
                    em.op("dve", lambda g, di=di, h=h: g.tensor_scalar(out=Ft[:], in0=Ft[:], scalar1=oml[:, di, h:h + 1], scalar2=lb[:, di, h:h + 1],
                                                                       op0=ALU.mult, op1=ALU.add), reads=[Ft, oml, lb], writes=[Ft])
                    em.op("act", lambda g: g.activation(out=LF[:], in_=Ft[:], func=AF.Ln), reads=[Ft], writes=[LF])
                    em.op("dve", lambda g: g.tensor_scalar(out=Kt[:], in0=Ft[:], scalar1=-1.0, scalar2=1.0, op0=ALU.mult, op1=ALU.add), reads=[Ft], writes=[Kt])
                    em.op("dve", lambda g: g.tensor_tensor_scan(out=CUM[:], data0=rst[:], data1=LF[:], initial=0.0, op0=ALU.mult, op1=ALU.add),
                          reads=[rst, LF], writes=[CUM])
                    if di == 0:
                        cum = CUM; last = 127
                    else:
                        em.op("dve", lambda g: g.tensor_tensor(out=CUMB[:], in0=LF[:], in1=CUM[:], op=ALU.subtract), reads=[LF, CUM], writes=[CUMB])
                        em.op("dve", lambda g: g.tensor_tensor(out=v3(CUMB), in0=v3(CUMB), in1=v3(CUM)[:, :, 127:128].to_broadcast([128, NT, 128]), op=ALU.add),
                              reads=[CUMB, CUM], writes=[CUMB])
                        cum = CUMB; last = 0
                    em.op("dve", lambda g, cum=cum: g.tensor_tensor(out=v3(At), in0=v3(cum), in1=v3(cum)[:, :, 64:65].to_broadcast([128, NT, 128]), op=ALU.subtract),
                          reads=[cum], writes=[At])
                    em.op("act", lambda g, last=last: g.activation(out=gend[:], in_=v3(At)[:, :, last], func=AF.Exp), reads=[At], writes=[gend])
                    em.op("act", lambda g, cum=cum, last=last: g.activation(out=dec[:], in_=v3(cum)[:, :, last], func=AF.Exp), reads=[cum], writes=[dec])
                    em.op("act", lambda g: g.activation(out=EQ[:], in_=At[:], func=AF.Exp), reads=[At], writes=[EQ])
                    em.op("dve", lambda g: g.tensor_tensor(out=EQ[:], in0=EQ[:], in1=qT[:], op=ALU.mult), reads=[EQ, qT], writes=[EQ])
                    em.op("act", lambda g: g.activation(out=EK[:], in_=At[:], func=AF.Exp, scale=-1.0), reads=[At], writes=[EK])
                    em.op("dve", lambda g: g.tensor_tensor(out=EK[:], in0=EK[:], in1=Kt[:], op=ALU.mult), reads=[EK, Kt], writes=[EK])
                    em.op("act", lambda g, cum=cum: g.activation(out=QD[:], in_=cum[:], func=AF.Exp), reads=[cum], writes=[QD])
                    em.op("dve", lambda g: g.tensor_tensor(out=QD[:], in0=QD[:], in1=qT[:], op=ALU.mult), reads=[QD, qT], writes=[QD])
                    order = [0, 1] + list(range(2, NT)) if di == 0 else [1, 0] + list(range(NT - 1, 1, -1))
                    MASK = MLE if di == 0 else MGE
                    si = 0
                    em.op("pool", lambda g: g.memset(S2[0][:], 0.0), writes=[S2[0]])
                    for it, n in enumerate(order):
                        Sc = S2[si % 2]; Sn = S2[(si + 1) % 2]; si += 1
                        sl = slice(n * 128, (n + 1) * 128)
                        if n >= 2:
                            ps = nextps(); sc = scT[it % 2]
                            em.mm(ps[:, 0:128], EK[:, sl], EQ[:, sl], True, True, [EK, EQ], [ps])
                            em.op("dve", lambda g, ps=ps, sc=sc: g.tensor_tensor(out=sc[:], in0=ps[:, 0:128], in1=MASK[:], op=ALU.mult), reads=[ps, MASK], writes=[sc])
                            po = nextps()
                            em.mm(po[:, 0:128], sc[:], v_tm[:, n, :], True, False, [sc, v_tm], [po])
                            em.mm(po[:, 0:128], QD[:, sl], Sc[:], False, True, [QD, Sc], [po])
                            if di == 0:
                                em.op("act", lambda g, po=po, n=n: g.copy(out=o_acc[:, n - 2, :], in_=po[:, 0:128]), reads=[po], writes=[o_acc], shared=True)
                            else:
                                em.op("dve", lambda g, po=po, n=n: g.tensor_tensor(out=o_acc[:, n - 2, :], in0=o_acc[:, n - 2, :], in1=po[:, 0:128], op=ALU.add),
                                      reads=[po, o_acc], writes=[o_acc], shared=True)
                        if it == len(order) - 1:
                            break
                        pt = nextps(); kt_ = kit[it % 2]
                        em.tr(pt[:, 0:128], EK[:, sl], ident, [EK], [pt])
                        em.op("act", lambda g, pt=pt, kt_=kt_: g.copy(out=kt_[:], in_=pt[:, 0:128]), reads=[pt], writes=[kt_])
                        pu = nextps()
                        em.mm(pu[:, 0:128], kt_[:], v_tm[:, n, :], True, True, [kt_, v_tm], [pu])
                        em.op("dve", lambda g, Sc=Sc, Sn=Sn, n=n: g.tensor_scalar(out=Sn[:], in0=Sc[:], scalar1=dec[:, n:n + 1], scalar2=None, op0=ALU.mult),
                              reads=[Sc, dec], writes=[Sn])
                        em.op("dve", lambda g, pu=pu, Sn=Sn, n=n: g.scalar_tensor_tensor(out=Sn[:], in0=pu[:, 0:128], scalar=gend[:, n:n + 1], in1=Sn[:],
                                                                                       op0=ALU.mult, op1=ALU.add), reads=[pu, gend, Sn], writes=[Sn])
                    S2 = S2 if si % 2 == 0 else S2[::-1]
                gated_norm_store(o_acc, g_tm, hgw_bc, h * 128, jk, ssn, t1, sg, mT_all)
        if upto <= 2:
            em.barrier()
            print("instructions:", em.ninstr)
            return nc

        with em.phase():
            SAME = em.tile("SAME", [128, 128])
            em.op("pool", lambda g: g.memset(SAME[:], 0.0), writes=[SAME])
            em.op("pool", lambda g: g.memset(SAME[0:64, 0:64], 1.0), writes=[SAME])
            em.op("pool", lambda g: g.memset(SAME[64:128, 64:128], 1.0), writes=[SAME])
            SEL = em.tile("SEL", [128, 2, 128])
            em.op("pool", lambda g: g.memset(SEL[:], 0.0), writes=[SEL])
            em.op("pool", lambda g: g.memset(SEL[0:64, 0, :], 1.0), writes=[SEL])
            em.op("pool", lambda g: g.memset(SEL[64:128, 1, :], 1.0), writes=[SEL])
            TRI = [em.tile("TRI%d" % i, [128, 128]) for i in range(2)]
            NEGS = [em.tile("NEGS%d" % i, [128, 128]) for i in range(2)]
            NEGI = [em.tile("NEGI%d" % i, [128, 128]) for i in range(2)]
            for di, M in enumerate((MLE, MGE)):
                em.op("dve", lambda g, di=di, M=M: g.tensor_tensor(out=TRI[di][:], in0=M[:], in1=SAME[:], op=ALU.mult), reads=[M, SAME], writes=[TRI[di]])
                em.op("dve", lambda g, di=di: g.tensor_scalar(out=NEGI[di][:], in0=TRI[di][:], scalar1=-1.0, scalar2=1e9, op0=ALU.add, op1=ALU.mult),
                      reads=[TRI[di]], writes=[NEGI[di]])
                em.op("dve", lambda g, di=di: g.tensor_tensor(out=NEGS[di][:], in0=TRI[di][:], in1=ident[:], op=ALU.subtract), reads=[TRI[di], ident], writes=[NEGS[di]])
                em.op("dve", lambda g, di=di: g.tensor_scalar(out=NEGS[di][:], in0=NEGS[di][:], scalar1=-1.0, scalar2=1e9, op0=ALU.add, op1=ALU.mult),
                      reads=[NEGS[di]], writes=[NEGS[di]])
            cwt = em.tile("cwt", [128, 3, 24]); cwrow = em.tile("cwrow", [72, 128])
            em.dma("sp", lambda g: g.dma_start(out=cwrow[:], in_=conv_d.rearrange("j (g p) -> (j g) p", p=128)), writes=[cwrow])
            ps = nextps()
            em.tr(ps[:, 0:72], cwrow[:], ident, [cwrow], [ps], k=72)
            em.op("dve", lambda g: g.tensor_copy(out=cwt[:].rearrange("p j g -> p (j g)"), in_=ps[:, 0:72]), reads=[ps], writes=[cwt])
            gdw_bc = em.tile("gdw_bc", [128, 128])
            em.dma("sp", lambda g: g.dma_start(out=gdw_bc[:], in_=gdnw_d.partition_broadcast(128)), writes=[gdw_bc])
            alg = em.tile("alg", [128, 2, 8]); dtb = em.tile("dtb", [128, 2, 8])
            for di, (a_, d_) in enumerate(((alf_d, dtf_d), (alb_d, dtb_d))):
                em.dma("sp", lambda g, di=di, a_=a_: g.dma_start(out=alg[:, di, :], in_=a_.partition_broadcast(128)), writes=[alg], shared=True)
                em.dma("sp", lambda g, di=di, d_=d_: g.dma_start(out=dtb[:, di, :], in_=d_.partition_broadcast(128)), writes=[dtb], shared=True)
            em.op("act", lambda g: g.activation(out=alg[:], in_=alg[:], func=AF.Exp), reads=[alg], writes=[alg])
            em.op("dve", lambda g: g.tensor_scalar(out=alg[:], in0=alg[:], scalar1=-1.0, scalar2=None, op0=ALU.mult), reads=[alg], writes=[alg])
            gt = em.tile("gt", [128, NT, 32])
            em.dma("sp", lambda g: g.dma_start(out=gt[:], in_=ptm[:, 3072:3104].rearrange("(n p) c -> p n c", p=128)), reads=[B_ptm], writes=[gt])
            names = ["gcol", "lnb", "allg", "cumc", "ncum", "r1", "er1", "ecum", "send", "beta", "decb"]
            G = {}
            for di in range(2):
                for nm in names:
                    shp = [128, NT, 32] if nm == "allg" else ([128, NT, 16] if nm == "decb" else [128, NT, 8])
                    G[nm, di] = em.tile("%s%d" % (nm, di), shp)
                gcol, lnb, allg = G["gcol", di], G["lnb", di], G["allg", di]
                em.op("dve", lambda g, di=di, gcol=gcol: g.tensor_tensor(out=gcol[:], in0=gt[:, :, di * 8:(di + 1) * 8],
                                                                        in1=dtb[:, di:di + 1, :].to_broadcast([128, NT, 8]), op=ALU.add), reads=[gt, dtb], writes=[gcol])
                em.op("act", lambda g, gcol=gcol: g.activation(out=gcol[:], in_=gcol[:], func=AF.Exp), reads=[gcol], writes=[gcol])
                em.op("act", lambda g, gcol=gcol: g.activation(out=gcol[:], in_=gcol[:], func=AF.Ln, bias=1.0), reads=[gcol], writes=[gcol])
                em.op("dve", lambda g, di=di, gcol=gcol: g.tensor_tensor(out=gcol[:], in0=gcol[:], in1=alg[:, di:di + 1, :].to_broadcast([128, NT, 8]), op=ALU.mult),
                      reads=[gcol, alg], writes=[gcol])
                em.op("act", lambda g, di=di, lnb=lnb: g.activation(out=lnb[:], in_=gt[:, :, 16 + di * 8:16 + (di + 1) * 8], func=AF.Exp, scale=-1.0), reads=[gt], writes=[lnb])
                em.op("act", lambda g, lnb=lnb: g.activation(out=lnb[:], in_=lnb[:], func=AF.Ln, bias=1.0), reads=[lnb], writes=[lnb])
                em.op("dve", lambda g, lnb=lnb: g.tensor_scalar(out=lnb[:], in0=lnb[:], scalar1=-1.0, scalar2=None, op0=ALU.mult), reads=[lnb], writes=[lnb])
                for n in range(NT):
                    ps = nextps()
                    em.mm(ps[:, 0:8], TRI[di][:], gcol[:, n, :], True, True, [TRI[di], gcol], [ps])
                    em.mm(ps[:, 8:16], SAME[:], gcol[:, n, :], True, True, [SAME, gcol], [ps])
                    em.mm(ps[:, 16:24], SEL[:, 0, :], gcol[:, n, :], True, True, [SEL, gcol], [ps])
                    em.mm(ps[:, 24:32], SEL[:, 1, :], gcol[:, n, :], True, True, [SEL, gcol], [ps])
                    evac(allg[:, n, :], ps[:, 0:32], [ps], [allg])
                cumc, ncum, r1, er1, ecum, send, beta, decb = (G[k, di] for k in ("cumc", "ncum", "r1", "er1", "ecum", "send", "beta", "decb"))
                em.op("dve", lambda g, cumc=cumc, allg=allg: g.tensor_copy(out=cumc[:], in_=allg[:, :, 0:8]), reads=[allg], writes=[cumc])
                em.op("dve", lambda g, ncum=ncum, cumc=cumc: g.tensor_scalar(out=ncum[:], in0=cumc[:], scalar1=-1.0, scalar2=None, op0=ALU.mult), reads=[cumc], writes=[ncum])
                em.op("dve", lambda g, r1=r1, cumc=cumc, lnb=lnb: g.tensor_tensor(out=r1[:], in0=cumc[:], in1=lnb[:], op=ALU.add), reads=[cumc, lnb], writes=[r1])
                em.op("act", lambda g, er1=er1, r1=r1: g.activation(out=er1[:], in_=r1[:], func=AF.Exp), reads=[r1], writes=[er1])
                em.op("act", lambda g, ecum=ecum, cumc=cumc: g.activation(out=ecum[:], in_=cumc[:], func=AF.Exp), reads=[cumc], writes=[ecum])
                em.op("dve", lambda g, send=send, allg=allg, cumc=cumc: g.tensor_tensor(out=send[:], in0=allg[:, :, 8:16], in1=cumc[:], op=ALU.subtract), reads=[allg, cumc], writes=[send])
                em.op("act", lambda g, send=send: g.activation(out=send[:], in_=send[:], func=AF.Exp), reads=[send], writes=[send])
                em.op("act", lambda g, beta=beta, lnb=lnb: g.activation(out=beta[:], in_=lnb[:], func=AF.Exp), reads=[lnb], writes=[beta])
                em.op("act", lambda g, decb=decb, allg=allg: g.activation(out=decb[:], in_=allg[:, :, 16:32], func=AF.Exp), reads=[allg], writes=[decb])

            raw = em.tile("raw", [128, NTOK]); SQ = em.tile("SQ", [128, NTOK]); RI = em.tile("RI", [128, 512])
            YQ = em.tile("YQ", [128, NTOK]); YK = em.tile("YK", [128, NTOK]); YV = em.tile("YV", [128, NTOK])
            k_tm = em.tile("k_tm", [128, NT, 128]); vg_tm = em.tile("vg_tm", [128, NT, 128]); z_tm = em.tile("z_tm", [128, NTX, 128])
            o_fb = [em.tile("o_fb%d" % i, [128, NTX, 128]) for i in range(2)]
            mT_all = em.tile("mT_all2", [128, SEQ], F32R)
            jk = em.tile("jk2", [128, 128]); ssn = em.tile("ssn2", [128, NTX]); t1 = em.tile("t12", [128, 128]); sg = em.tile("sg2", [128, 128])
            WK = {}
            for di in range(2):
                for nm in ("VN", "O", "Sa", "Sb"):
                    WK[nm, di] = em.tile("%s_%d" % (nm, di), [128, 128])

            def conv_silu(Y, gi, h):
                cj = gi * 8 + h
                em.op("dve", lambda g: g.tensor_scalar(out=Y[:], in0=raw[:], scalar1=cwt[:, 1, cj:cj + 1], scalar2=None, op0=ALU.mult), reads=[raw, cwt], writes=[Y])
                segs = [(Y[:, 0:CTX].rearrange("p (r w) -> p r w", w=CTX), raw[:, 0:CTX].rearrange("p (r w) -> p r w", w=CTX), CTX),
                        (Y[:, CTX:NTOK].rearrange("p (r w) -> p r w", w=64), raw[:, CTX:NTOK].rearrange("p (r w) -> p r w", w=64), 64)]
                for (y3, a3, w) in segs:
                    em.op("dve", lambda g, y3=y3, a3=a3, w=w: g.scalar_tensor_tensor(out=y3[:, :, 1:w], in0=a3[:, :, 0:w - 1], scalar=cwt[:, 0, cj:cj + 1], in1=y3[:, :, 1:w],
                                                                                   op0=ALU.mult, op1=ALU.add), reads=[raw, cwt, Y], writes=[Y])
                    em.op("dve", lambda g, y3=y3, a3=a3, w=w: g.scalar_tensor_tensor(out=y3[:, :, 0:w - 1], in0=a3[:, :, 1:w], scalar=cwt[:, 2, cj:cj + 1], in1=y3[:, :, 0:w - 1],
                                                                                   op0=ALU.mult, op1=ALU.add), reads=[raw, cwt, Y], writes=[Y])
                em.op("act", lambda g: g.activation(out=Y[:], in_=Y[:], func=AF.Silu), reads=[Y], writes=[Y])

            def l2norm(Y, mult):
                em.op("dve", lambda g: g.tensor_tensor(out=SQ[:], in0=Y[:], in1=Y[:], op=ALU.mult), reads=[Y], writes=[SQ])
                for c0 in range(0, NTOK, 512):
                    w = min(512, NTOK - c0)
                    ps = nextps()
                    em.mm(ps[:, 0:w], ones[:], SQ[:, c0:c0 + w], True, True, [ones, SQ], [ps])
                    em.op("dve", lambda g, ps=ps, w=w: g.tensor_scalar(out=RI[:, 0:w], in0=ps[:, 0:w], scalar1=EPS, scalar2=None, op0=ALU.add), reads=[ps], writes=[RI])
                    em.op("act", lambda g, w=w: g.activation(out=RI[:, 0:w], in_=RI[:, 0:w], func=AF.Sqrt), reads=[RI], writes=[RI])
                    em.op("dve", lambda g, w=w: g.reciprocal(out=RI[:, 0:w], in_=RI[:, 0:w]), reads=[RI], writes=[RI])
                    em.op("dve", lambda g, c0=c0, w=w: g.scalar_tensor_tensor(out=Y[:, c0:c0 + w], in0=Y[:, c0:c0 + w], scalar=mult, in1=RI[:, 0:w], op0=ALU.mult, op1=ALU.mult),
                          reads=[Y, RI], writes=[Y])

            NSLOT = 2
            A_NAMES = ("DGr", "DGn", "DGc", "E1", "E2", "Bm", "Bt", "AT", "Pn", "Ptn", "R", "Rn", "vb", "kbd", "kend", "U", "WT")
            AW = {}
            for di in range(2):
                for sl_ in range(NSLOT):
                    for nm in A_NAMES:
                        AW[nm, di, sl_] = em.tile("%s_%d_%d" % (nm, di, sl_), [128, 128])
            ARES = {}

            def gdn_A(h, di, n, slot):
                W = lambda nm: AW[nm, di, slot]
                cumc, ncum, r1, er1, send, beta = (G[k, di] for k in ("cumc", "ncum", "r1", "er1", "send", "beta"))
                sl = slice(n * 128, (n + 1) * 128)
                isx = n >= 2
                col = lambda t: t[:, n, h:h + 1]
                psA = nextps()
                em.mm(psA[:, 0:128], YK[:, sl], YK[:, sl], True, True, [YK], [psA])
                if isx:
                    em.mm(psA[:, 128:256], YK[:, sl], YQ[:, sl], True, True, [YK, YQ], [psA])
                em.op("dve", lambda g: g.tensor_scalar(out=W("DGr")[:], in0=ident[:], scalar1=col(r1), scalar2=None, op0=ALU.mult), reads=[ident, r1], writes=[W("DGr")])
                em.op("dve", lambda g: g.tensor_scalar(out=W("DGn")[:], in0=ident[:], scalar1=col(ncum), scalar2=None, op0=ALU.mult), reads=[ident, ncum], writes=[W("DGn")])
                psD = nextps()
                em.mm(psD[:, 0:128], ones[:], W("DGr")[:], True, False, [ones, W("DGr")], [psD])
                em.mm(psD[:, 0:128], W("DGn")[:], ones[:], False, True, [ones, W("DGn")], [psD])
                if isx:
                    em.op("dve", lambda g: g.tensor_scalar(out=W("DGc")[:], in0=ident[:], scalar1=col(cumc), scalar2=None, op0=ALU.mult), reads=[ident, cumc], writes=[W("DGc")])
                    em.mm(psD[:, 128:256], ones[:], W("DGc")[:], True, False, [ones, W("DGc")], [psD])
                    em.mm(psD[:, 128:256], W("DGn")[:], ones[:], False, True, [ones, W("DGn")], [psD])
                yield
                em.op("dve", lambda g: g.tensor_tensor(out=W("E1")[:], in0=psD[:, 0:128], in1=NEGS[di][:], op=ALU.add), reads=[psD, NEGS[di]], writes=[W("E1")])
                em.op("act", lambda g: g.activation(out=W("E1")[:], in_=W("E1")[:], func=AF.Exp), reads=[W("E1")], writes=[W("E1")])
                em.op("dve", lambda g: g.tensor_tensor(out=W("Bm")[:], in0=psA[:, 0:128], in1=W("E1")[:], op=ALU.mult), reads=[psA, W("E1")], writes=[W("Bm")])
                if isx:
                    em.op("dve", lambda g: g.tensor_tensor(out=W("E2")[:], in0=psD[:, 128:256], in1=NEGI[di][:], op=ALU.add), reads=[psD, NEGI[di]], writes=[W("E2")])
                    em.op("act", lambda g: g.activation(out=W("E2")[:], in_=W("E2")[:], func=AF.Exp), reads=[W("E2")], writes=[W("E2")])
                    em.op("dve", lambda g: g.tensor_tensor(out=W("AT")[:], in0=psA[:, 128:256], in1=W("E2")[:], op=ALU.mult), reads=[psA, W("E2")], writes=[W("AT")])
                yield
                pst = nextps()
                em.tr(pst[:, 0:128], W("Bm")[:], ident, [W("Bm")], [pst])
                em.op("act", lambda g: g.copy(out=W("Bt")[:], in_=pst[:, 0:128]), reads=[pst], writes=[W("Bt")])
                em.op("dve", lambda g: g.tensor_tensor(out=W("R")[:], in0=ident[:], in1=W("Bm")[:], op=ALU.subtract), reads=[ident, W("Bm")], writes=[W("R")])
                P, Pt, Pn, Ptn, R, Rn = W("Bm"), W("Bt"), W("Pn"), W("Ptn"), W("R"), W("Rn")
                for lvl in range(5):
                    yield
                    ps2 = nextps()
                    em.mm(ps2[:, 0:128], P[:], Pt[:], True, True, [P, Pt], [ps2])
                    em.op("act", lambda g, ps2=ps2, Ptn=Ptn: g.copy(out=Ptn[:], in_=ps2[:, 0:128]), reads=[ps2], writes=[Ptn])
                    if lvl < 4:
                        em.mm(ps2[:, 128:256], Pt[:], P[:], True, True, [P, Pt], [ps2])
                        em.op("dve", lambda g, ps2=ps2, Pn=Pn: g.tensor_copy(out=Pn[:], in_=ps2[:, 128:256]), reads=[ps2], writes=[Pn])
                    yield
                    ps3 = nextps()
                    em.mm(ps3[:, 0:128], Ptn[:], R[:], True, True, [Ptn, R], [ps3])
                    em.op("dve", lambda g, ps3=ps3, R=R, Rn=Rn: g.tensor_tensor(out=Rn[:], in0=ps3[:, 0:128], in1=R[:], op=ALU.add), reads=[ps3, R], writes=[Rn])
                    P, Pn = Pn, P
                    Pt, Ptn = Ptn, Pt
                    R, Rn = Rn, R
                yield
                em.op("dve", lambda g: g.tensor_scalar(out=W("vb")[:], in0=vg_tm[:, n, :], scalar1=col(beta), scalar2=None, op0=ALU.mult), reads=[vg_tm, beta], writes=[W("vb")])
                em.op("dve", lambda g: g.tensor_scalar(out=W("kbd")[:], in0=k_tm[:, n, :], scalar1=col(er1), scalar2=None, op0=ALU.mult), reads=[k_tm, er1], writes=[W("kbd")])
                em.op("dve", lambda g: g.tensor_scalar(out=W("kend")[:], in0=k_tm[:, n, :], scalar1=col(send), scalar2=None, op0=ALU.mult), reads=[k_tm, send], writes=[W("kend")])
                psu = nextps()
                em.mm(psu[:, 0:128], R[:], W("vb")[:], True, True, [R, W("vb")], [psu])
                em.mm(psu[:, 128:256], W("kbd")[:], R[:], True, True, [R, W("kbd")], [psu])
                yield
                em.op("act", lambda g, psu=psu: g.copy(out=W("U")[:], in_=psu[:, 0:128]), reads=[psu], writes=[W("U")])
                em.op("dve", lambda g, psu=psu: g.tensor_copy(out=W("WT")[:], in_=psu[:, 128:256]), reads=[psu], writes=[W("WT")])
                ARES[di, n] = slot

            def gdn_B(h, di, order):
                ecum, decb = G["ecum", di], G["decb", di]
                Sc, Sn = WK["Sa", di], WK["Sb", di]
                VN, O = WK["VN", di], WK["O", di]
                em.op("pool", lambda g: g.memset(Sc[:], 0.0), writes=[Sc])
                corder = (0, 1) if di == 0 else (1, 0)
                o_out = o_fb[di]
                for idx, n in enumerate(order):
                    while (di, n) not in ARES:
                        yield "wait"
                    slot = ARES.pop((di, n))
                    W = lambda nm: AW[nm, di, slot]
                    sl = slice(n * 128, (n + 1) * 128)
                    isx = n >= 2
                    for c in corder:
                        rs = slice(c * 64, (c + 1) * 64)
                        psv = nextps()
                        em.mm(psv[:, 0:128], W("WT")[:], Sc[:], True, True, [W("WT"), Sc], [psv])
                        if isx:
                            em.mm(psv[:, 128:256], YQ[:, sl], Sc[:], True, True, [YQ, Sc], [psv])
                        yield
                        em.op("dve", lambda g, rs=rs, psv=psv: g.tensor_tensor(out=VN[rs, :], in0=W("U")[rs, :], in1=psv[rs, 0:128], op=ALU.subtract),
                              reads=[W("U"), psv], writes=[VN], shared=True)
                        if isx:
                            em.op("act", lambda g, rs=rs, psv=psv: g.activation(out=O[rs, :], in_=psv[rs, 128:256], func=AF.Identity, scale=ecum[rs, n, h:h + 1]),
                                  reads=[psv, ecum], writes=[O], shared=True)
                        pss = nextps()
                        em.mm(pss[:, 0:128], W("kend")[rs, :], VN[rs, :], True, True, [W("kend"), VN], [pss])
                        em.op("dve", lambda g, c=c, Sc=Sc, Sn=Sn: g.tensor_scalar(out=Sn[:], in0=Sc[:], scalar1=decb[:, n, c * 8 + h:c * 8 + h + 1], scalar2=None, op0=ALU.mult),
                              reads=[Sc, decb], writes=[Sn])
                        yield
                        em.op("dve", lambda g, pss=pss, Sn=Sn: g.tensor_tensor(out=Sn[:], in0=Sn[:], in1=pss[:, 0:128], op=ALU.add), reads=[pss, Sn], writes=[Sn])
                        Sc, Sn = Sn, Sc
                    if isx:
                        pso = nextps()
                        em.mm(pso[:, 0:128], W("AT")[:], VN[:], True, True, [W("AT"), VN], [pso])
                        yield
                        em.op("dve", lambda g, pso=pso: g.tensor_tensor(out=o_out[:, n - 2, :], in0=O[:], in1=pso[:, 0:128], op=ALU.add),
                              reads=[O, pso], writes=[o_out], shared=True)
                    yield ("done", idx)

            def run_gdn_head(h):
                st = []
                for di in range(2):
                    order = [0, 1] + list(range(2, NT)) if di == 0 else [1, 0] + list(range(NT - 1, 1, -1))
                    st.append(dict(order=order, a_next=0, a_act=[], b=gdn_B(h, di, order), b_done=0, alive=True))
                ARES.clear()
                while any(s_["alive"] for s_ in st):
                    for di, s_ in enumerate(st):
                        if not s_["alive"]:
                            continue
                        while s_["a_next"] < len(s_["order"]) and s_["a_next"] - s_["b_done"] < NSLOT:
                            n = s_["order"][s_["a_next"]]
                            s_["a_act"].append(gdn_A(h, di, n, s_["a_next"] % NSLOT))
                            s_["a_next"] += 1
                        for gen in list(s_["a_act"]):
                            try:
                                next(gen)
                            except StopIteration:
                                s_["a_act"].remove(gen)
                        try:
                            r_ = next(s_["b"])
                            if isinstance(r_, tuple) and r_[0] == "done":
                                s_["b_done"] = r_[1] + 1
                        except StopIteration:
                            s_["alive"] = False

            for h in range(ngd):
                for gi, Y in enumerate((YQ, YK, YV)):
                    r0 = 3072 + gi * 1024 + h * 128
                    em.dma("sp", lambda g, r0=r0: g.dma_start(out=raw[:], in_=pfm[r0:r0 + 128, :]), reads=[B_pfm], writes=[raw])
                    conv_silu(Y, gi, h)
                em.dma("pool", lambda g, h=h: g.dma_start(out=z_tm[:], in_=ptm[CTX:NTOK, 2048 + h * 128:2048 + (h + 1) * 128].rearrange("(n p) e -> p n e", p=128)),
                       reads=[B_ptm], writes=[z_tm])
                l2norm(YQ, 128 ** -0.5); l2norm(YK, 1.0)
                for n in range(NT):
                    for (Y, dst) in ((YK, k_tm), (YV, vg_tm)):
                        ps = nextps()
                        em.tr(ps[:, 0:128], Y[:, n * 128:(n + 1) * 128], ident, [Y], [ps])
                        evac(dst[:, n, :], ps[:, 0:128], [ps], [dst])
                if upto < 3:
                    for i in range(2):
                        em.op("pool", lambda g, i=i: g.memset(o_fb[i][:], 1.0), writes=[o_fb[i]])
                if upto == 2.5:
                    continue
                run_gdn_head(h)
                em.op("dve", lambda g: g.tensor_tensor(out=o_fb[0][:], in0=o_fb[0][:], in1=o_fb[1][:], op=ALU.add), reads=[o_fb[0], o_fb[1]], writes=[o_fb[0]])
                gated_norm_store(o_fb[0], z_tm, gdw_bc, 1024 + h * 128, jk, ssn, t1, sg, mT_all)
        if upto <= 3:
            em.barrier()
            print("instructions:", em.ninstr)
            return nc

        bc_blk = nc.gpsimd.to_reg(NBLK * 128 - 1); bc_w = nc.gpsimd.to_reg(NE * 128 * 4 - 1); bc_b = nc.gpsimd.to_reg(NE - 1)
        icols = [em.tile("icol%d" % i, [128, 1], I32) for i in range(8)]
        icnt = {"i": 0}

        def probe_ind(tag, src=None, ic_=None):
            import os
            if os.environ.get("PROBE_IND") != "1":
                return
            try:
                tt = icols[0] if ic_ is None else ic_
                src = zt if src is None else src
                nc.gpsimd.indirect_dma_start(out=xs_d[:, :], out_offset=bass.IndirectOffsetOnAxis(ap=tt[:, :], axis=0), in_=src[:, :], in_offset=None,
                                             bounds_check=bc_blk, oob_is_err=False).then_inc(em.esem["pool"].h, 16)
                print("PROBE_IND", tag, "ok")
            except Exception as ex:
                print("PROBE_IND", tag, "ERR", ex)

        def idxcol(src, c):
            t_ = icols[icnt["i"] % 8]; icnt["i"] += 1
            em.op("pool", lambda g: g.tensor_copy(out=t_[:], in_=src[:, c:c + 1]), reads=[src], writes=[t_])
            return t_

        desti = em.tile("desti", [128, NTX * TOPK], I32); gates = em.tile("gates", [128, NTX, TOPK])
        widx = em.tile("widx", [128, NBLK * 4], I32); bidx = em.tile("bidx", [128, NBLK], I32)
        zt = em.tile("zt", [128, D])
        em.op("pool", lambda g: g.memset(zt[:], 0.0), writes=[zt])
        probe_ind("before zero fill")
        for j in range(NBLK):
            em.dma("pool", lambda g, j=j: g.dma_start(out=xs_d[j * 128:(j + 1) * 128, :], in_=zt[:]), reads=[zt], writes=[B_xs], shared=True)
        probe_ind("after zero fill")
        with em.phase():
            probe_ind("in phase")
            gt1_bc = em.tile("gt1_bc", [128, D])
            em.dma("sp", lambda g: g.dma_start(out=gt1_bc[:], in_=modrows[0, 2 * D:3 * D].partition_broadcast(128)), reads=[B_modrows], writes=[gt1_bc])
            wrt = em.tile("wrt", [128, 16, NE]); brt = em.tile("brt", [1, NE])
            em.dma("sp", lambda g: g.dma_start(out=wrt[:], in_=w_rt.rearrange("(p q) e -> p q e", q=16)), writes=[wrt])
            em.dma("sp", lambda g: g.dma_start(out=brt[:], in_=b_rt.rearrange("(o n) -> o n", o=1)), writes=[brt])
            mg = em.tile("mg", [128, 16, 512], F32R); wring = [em.tile("wo%d" % i, [128, 16, 512], F32R) for i in range(2)]
            x1t = [em.tile("x1t%d" % i, [128, D]) for i in range(4)]
            tmpy = [em.tile("tmpy%d" % i, [128, 512]) for i in range(2)]
            junk = em.tile("junk3", [128, D]); ss = em.tile("ss3", [128, 1]); xn2 = em.tile("xn2t", [128, D]); h2T = em.tile("h2T", [128, 16, 128])
            lg = em.tile("lg", [128, NTX, NE]); mask_all = em.tile("mask_all", [128, NTX, NE]); rank = em.tile("rank", [128, NTX, NE])
            top8 = em.tile("top8", [128, NTX, 8])
            SLT = em.tile("SLT", [128, 128])
            em.op("dve", lambda g: g.tensor_tensor(out=SLT[:], in0=MLE[:], in1=ident[:], op=ALU.subtract), reads=[MLE, ident], writes=[SLT])
            wov = w_out.rearrange("(k p) c -> p k c", p=128); mxv = mixT.rearrange("(k p) t -> p k t", p=128)
            wi = 0; yi = 0
            for gI in range(4):
                tok0 = gI * 512
                em.dma("pool", lambda g, tok0=tok0: g.dma_start(out=mg[:], in_=mxv[:, :, tok0:tok0 + 512]), reads=[B_mixT], writes=[mg])
                for j in range(4):
                    em.dma("sp", lambda g, j=j, tok0=tok0: g.dma_start(out=x1t[j][:], in_=x_d[tok0 + j * 128:tok0 + (j + 1) * 128, :]), writes=[x1t[j]])
                for cg in range(4):
                    wt = wring[wi % 2]; wi += 1
                    em.dma("pool", lambda g, wt=wt, cg=cg: g.dma_start(out=wt[:], in_=wov[:, :, cg * 512:(cg + 1) * 512]), writes=[wt])
                    for j in range(4):
                        ps = nextps()
                        for k in range(16):
                            em.mm(ps[:, :], mg[:, k, j * 128:(j + 1) * 128], wt[:, k, :], k == 0, k == 15, [mg, wt], [ps], r=True)
                        ty = tmpy[yi % 2]; yi += 1
                        em.op("dve", lambda g, ps=ps, ty=ty, cg=cg: g.tensor_tensor(out=ty[:], in0=ps[:, :], in1=gt1_bc[:, cg * 512:(cg + 1) * 512], op=ALU.mult),
                              reads=[ps, gt1_bc], writes=[ty])
                        em.op("dve", lambda g, ty=ty, j=j, cg=cg: g.tensor_tensor(out=x1t[j][:, cg * 512:(cg + 1) * 512], in0=x1t[j][:, cg * 512:(cg + 1) * 512], in1=ty[:], op=ALU.add),
                              reads=[ty, x1t[j]], writes=[x1t[j]])
                for j in range(4):
                    t = gI * 4 + j
                    xt = x1t[j]
                    em.dma("sp", lambda g, xt=xt, t=t: g.dma_start(out=x1_d[t * 128:(t + 1) * 128, :], in_=xt[:]), reads=[xt], writes=[B_x1], shared=True)
                    em.op("act", lambda g, xt=xt: g.activation(out=junk[:], in_=xt[:], func=AF.Square, accum_out=ss[:]), reads=[xt], writes=[junk, ss])
                    rstd_from_ss(ss, 1, D)
                    em.op("dve", lambda g, xt=xt: g.tensor_scalar(out=xn2[:], in0=xt[:], scalar1=ss[:, 0:1], scalar2=None, op0=ALU.mult), reads=[xt, ss], writes=[xn2])
                    em.dma("sp", lambda g, t=t: g.dma_start(out=xn2_d[t * 128:(t + 1) * 128, :], in_=xn2[:]), reads=[xn2], writes=[B_xn2], shared=True)
                    for qq in range(4):
                        ps = nextps()
                        for q4 in range(4):
                            q = qq * 4 + q4
                            em.tr(ps[:, q4 * 128:(q4 + 1) * 128], xn2[:, q:D:16], ident, [xn2], [ps])
                        for q4 in range(4):
                            q = qq * 4 + q4
                            em.op("act", lambda g, q=q, q4=q4, ps=ps: g.activation(out=h2T[:, q, :], in_=ps[:, q4 * 128:(q4 + 1) * 128], func=AF.Identity,
                                                                                  scale=A2x[:, q:q + 1], bias=B2x[:, q:q + 1]), reads=[ps, A2x, B2x], writes=[h2T], shared=True)
                    ps = nextps()
                    for q in range(16):
                        em.mm(ps[:, 0:NE], h2T[:, q, :], wrt[:, q, :], q == 0, False, [h2T, wrt], [ps])
                    em.mm(ps[:, 0:NE], ones[0:1, 0:128], brt[0:1, :], False, True, [ones, brt], [ps])
                    em.op("dve", lambda g, ps=ps, t=t: g.tensor_copy(out=lg[:, t, :], in_=ps[:, 0:NE]), reads=[ps], writes=[lg], shared=True)
            probe_ind("before routing")
            nm = em.tile("nm", [128, 1]); e4 = em.tile("e4", [128, 4]); es = em.tile("es", [128, 1])
            for t in range(NTX):
                em.op("dve", lambda g, t=t: g.max(out=top8[:, t, :], in_=lg[:, t, :]), reads=[lg], writes=[top8], shared=True)
                em.op("dve", lambda g, t=t: g.tensor_scalar(out=mask_all[:, t, :], in0=lg[:, t, :], scalar1=top8[:, t, 3:4], scalar2=None, op0=ALU.is_ge),
                      reads=[lg, top8], writes=[mask_all], shared=True)
                em.op("dve", lambda g, t=t: g.tensor_scalar(out=nm[:], in0=top8[:, t, 0:1], scalar1=-1.0, scalar2=None, op0=ALU.mult), reads=[top8], writes=[nm])
                em.op("act", lambda g, t=t: g.activation(out=e4[:], in_=top8[:, t, 0:4], func=AF.Exp, bias=nm[:, 0:1], accum_out=es[:]), reads=[top8, nm], writes=[e4, es])
                em.op("dve", lambda g: g.reciprocal(out=es[:], in_=es[:]), reads=[es], writes=[es])
                em.op("dve", lambda g, t=t: g.tensor_scalar(out=gates[:, t, :], in0=e4[:], scalar1=es[:, 0:1], scalar2=None, op0=ALU.mult), reads=[e4, es], writes=[gates], shared=True)
                ps = nextps()
                em.mm(ps[:, 0:NE], SLT[:], mask_all[:, t, :], True, t == 0, [SLT, mask_all], [ps])
                for tp in range(t):
                    em.mm(ps[:, 0:NE], ones[:], mask_all[:, tp, :], False, tp == t - 1, [ones, mask_all], [ps])
                em.op("dve", lambda g, ps=ps, t=t: g.tensor_copy(out=rank[:, t, :], in_=ps[:, 0:NE]), reads=[ps], writes=[rank], shared=True)
            cntb = em.tile("cntb", [128, NE]); nblk = em.tile("nblk", [128, NE]); cmp = em.tile("cmp", [128, NE]); pend = em.tile("pend", [128, NE]); pst = em.tile("pst", [128, NE])
            ps = nextps()
            for t in range(NTX):
                em.mm(ps[:, 0:NE], ones[:], mask_all[:, t, :], t == 0, t == NTX - 1, [ones, mask_all], [ps])
            em.op("dve", lambda g: g.tensor_copy(out=cntb[:], in_=ps[:, 0:NE]), reads=[ps], writes=[cntb])
            em.op("dve", lambda g: g.tensor_scalar(out=nblk[:], in0=cntb[:], scalar1=0.5, scalar2=None, op0=ALU.is_gt), reads=[cntb], writes=[nblk])
            for m in range(1, 16):
                em.op("dve", lambda g, m=m: g.tensor_scalar(out=cmp[:], in0=cntb[:], scalar1=128.0 * m + 0.5, scalar2=None, op0=ALU.is_gt), reads=[cntb], writes=[cmp])
                em.op("dve", lambda g: g.tensor_tensor(out=nblk[:], in0=nblk[:], in1=cmp[:], op=ALU.add), reads=[nblk, cmp], writes=[nblk])
            em.op("dve", lambda g: g.tensor_tensor_scan(out=pend[:], data0=ones[:, 0:NE], data1=nblk[:], initial=0.0, op0=ALU.mult, op1=ALU.add), reads=[ones, nblk], writes=[pend])
            em.op("dve", lambda g: g.tensor_tensor(out=pst[:], in0=pend[:], in1=nblk[:], op=ALU.subtract), reads=[pend, nblk], writes=[pst])
            em.op("dve", lambda g: g.tensor_scalar(out=pst[:], in0=pst[:], scalar1=128.0, scalar2=None, op0=ALU.mult), reads=[pst], writes=[pst])
            em.op("dve", lambda g: g.tensor_scalar(out=pend[:], in0=pend[:], scalar1=128.0, scalar2=None, op0=ALU.mult), reads=[pend], writes=[pend])
            em.op("dve", lambda g: g.tensor_tensor(out=rank[:], in0=rank[:], in1=pst[:].unsqueeze(1).to_broadcast([128, NTX, NE]), op=ALU.add), reads=[rank, pst], writes=[rank])
            destf = em.tile("destf", [128, NTX, TOPK]); eqt = em.tile("eqt", [128, NE])
            for t in range(NTX):
                for k in range(TOPK):
                    em.op("dve", lambda g, t=t, k=k: g.tensor_scalar(out=eqt[:], in0=lg[:, t, :], scalar1=top8[:, t, k:k + 1], scalar2=None, op0=ALU.is_equal), reads=[lg, top8], writes=[eqt])
                    em.op("dve", lambda g, t=t: g.tensor_tensor(out=eqt[:], in0=eqt[:], in1=rank[:, t, :], op=ALU.mult), reads=[eqt, rank], writes=[eqt])
                    em.op("dve", lambda g, t=t, k=k: g.reduce_sum(out=destf[:, t, k:k + 1], in_=eqt[:], axis=mybir.AxisListType.X), reads=[eqt], writes=[destf], shared=True)
            em.op("dve", lambda g: g.tensor_copy(out=desti[:], in_=destf[:].rearrange("p t k -> p (t k)")), reads=[destf], writes=[desti])
            jv = em.tile("jv", [128, NBLK]); bexp = em.tile("bexp", [128, NBLK]); cmpj = em.tile("cmpj", [128, NBLK]); pio = em.tile("pio", [128, 1])
            em.op("pool", lambda g: g.iota(jv[:], pattern=[[128, NBLK]], base=0, channel_multiplier=0, allow_small_or_imprecise_dtypes=True), writes=[jv])
            em.op("pool", lambda g: g.iota(pio[:], pattern=[[0, 1]], base=0, channel_multiplier=1, allow_small_or_imprecise_dtypes=True), writes=[pio])
            em.op("pool", lambda g: g.memset(bexp[:], 0.0), writes=[bexp])
            for e_ in range(NE):
                em.op("dve", lambda g, e_=e_: g.tensor_scalar(out=cmpj[:], in0=jv[:], scalar1=pend[:, e_:e_ + 1], scalar2=None, op0=ALU.is_ge), reads=[jv, pend], writes=[cmpj])
                em.op("dve", lambda g: g.tensor_tensor(out=bexp[:], in0=bexp[:], in1=cmpj[:], op=ALU.add), reads=[bexp, cmpj], writes=[bexp])
            em.op("dve", lambda g: g.tensor_scalar(out=bexp[:], in0=bexp[:], scalar1=float(NE - 1), scalar2=None, op0=ALU.min), reads=[bexp], writes=[bexp])
            em.op("dve", lambda g: g.tensor_copy(out=bidx[:], in_=bexp[:]), reads=[bexp], writes=[bidx])
            em.op("dve", lambda g: g.tensor_scalar(out=bexp[:], in0=bexp[:], scalar1=128.0, scalar2=None, op0=ALU.mult), reads=[bexp], writes=[bexp])
            em.op("dve", lambda g: g.tensor_scalar(out=bexp[:], in0=bexp[:], scalar1=pio[:, 0:1], scalar2=None, op0=ALU.add), reads=[bexp, pio], writes=[bexp])
            bexp4 = em.tile("bexp4", [128, NBLK, 4])
            for quad in range(4):
                em.op("dve", lambda g, quad=quad: g.tensor_scalar(out=bexp4[:, :, quad], in0=bexp[:], scalar1=4.0, scalar2=float(quad), op0=ALU.mult, op1=ALU.add),
                      reads=[bexp], writes=[bexp4], shared=True)
            em.op("dve", lambda g: g.tensor_copy(out=widx[:], in_=bexp4[:].rearrange("p j q -> p (j q)")), reads=[bexp4], writes=[widx])
            if dbg:
                dump("lg", lg, [128, NTX, NE]); dump("destf", destf, [128, NTX, TOPK]); dump("gates", gates, [128, NTX, TOPK]); dump("bexp", bexp, [128, NBLK])
            probe_ind("before scatter")
            for t in range(NTX):
                em.dma("sp", lambda g, t=t: g.dma_start(out=xn2[:], in_=xn2_d[t * 128:(t + 1) * 128, :]), reads=[B_xn2], writes=[xn2])
                for k in range(TOPK):
                    probe_ind("pre-idxcol xn2", src=xn2)
                    ic = idxcol(desti, t * TOPK + k)
                    probe_ind("post-idxcol zt", ic_=ic)
                    probe_ind("post-idxcol xn2", src=xn2, ic_=ic)
                    em.dma("pool", lambda g, ic=ic: g.indirect_dma_start(out=xs_d[:, :], out_offset=bass.IndirectOffsetOnAxis(ap=ic[:, :], axis=0),
                                                                        in_=xn2[:, :], in_offset=None, bounds_check=bc_blk, oob_is_err=False),
                           reads=[xn2, ic], writes=[B_xs], shared=True)
        if upto <= 4:
            em.barrier()
            print("instructions:", em.ninstr)
            return nc

        with em.phase():
            w2 = [w.rearrange("(e p q4 ql) c -> (e p q4) (ql c)", p=128, q4=4, ql=4) for w in (w_gate, w_up, w_down)]
            bsrc = (b_gate, b_up, b_down)
            wq = [em.tile("wq%d" % i, [128, 4 * D], F32R) for i in range(3)]
            xs_t = em.tile("xs_t", [128, D]); xsT = em.tile("xsT", [128, 16, 128]); actv = em.tile("actv", [128, D]); actT = em.tile("actT", [128, 16, 128])
            ysb = em.tile("ysb", [128, D]); gsb = em.tile("gsb", [128, 512]); usb = em.tile("usb", [128, 512]); sgm = em.tile("sgm", [128, 512])
            brow3 = [em.tile("brow3_%d" % i, [2, D], F32R) for i in range(3)]
            wqi = 0
            for j in range(NBLK):
                em.dma("sp", lambda g, j=j: g.dma_start(out=xs_t[:], in_=xs_d[j * 128:(j + 1) * 128, :]), reads=[B_xs], writes=[xs_t])
                bic = idxcol(bidx, j)
                for i in range(3):
                    em.dma("pool", lambda g, i=i: g.indirect_dma_start(out=brow3[i][0:2, :], out_offset=None, in_=bsrc[i][:, :],
                                                                     in_offset=bass.IndirectOffsetOnAxis(ap=bic[0:2, :], axis=0),
                                                                     bounds_check=bc_b, oob_is_err=False), reads=[bic], writes=[brow3[i]])
                for qq in range(4):
                    ps = nextps()
                    for q4 in range(4):
                        q = qq * 4 + q4
                        em.tr(ps[:, q4 * 128:(q4 + 1) * 128], xs_t[:, q:D:16], ident, [xs_t], [ps])
                    for q4 in range(4):
                        q = qq * 4 + q4
                        em.op("act", lambda g, q=q, q4=q4, ps=ps: g.activation(out=xsT[:, q, :].bitcast(F32R), in_=ps[:, q4 * 128:(q4 + 1) * 128], func=AF.Identity,
                                                                              scale=A2x[:, q:q + 1], bias=B2x[:, q:q + 1]), reads=[ps, A2x, B2x], writes=[xsT], shared=True)
                for quad in range(4):
                    wts = []
                    wic = idxcol(widx, j * 4 + quad)
                    for i in range(2):
                        wt = wq[wqi % 3]; wqi += 1
                        em.dma("pool", lambda g, wt=wt, i=i, j=j, quad=quad: g.indirect_dma_start(
                            out=wt[:, :], out_offset=None, in_=w2[i][:, :],
                            in_offset=bass.IndirectOffsetOnAxis(ap=wic[:, :], axis=0), bounds_check=bc_w, oob_is_err=False), reads=[wic], writes=[wt])
                        wts.append(wt)
                    for i in range(2):
                        for cg in range(4):
                            ps = PS[i * 4 + cg]
                            for ql in range(4):
                                q = quad * 4 + ql
                                em.mm(ps[:, :], xsT[:, q, :], wts[i][:, ql * D + cg * 512:ql * D + (cg + 1) * 512], q == 0, False, [xsT, wts[i]], [ps], r=True)
                for i in range(2):
                    for cg in range(4):
                        ps = PS[i * 4 + cg]
                        em.mm(ps[:, :], ones_r[0:1, 0:128], brow3[i][0:1, cg * 512:(cg + 1) * 512], False, True, [ones_r, brow3[i]], [ps], r=True)
                for cg in range(4):
                    cs_ = slice(cg * 512, (cg + 1) * 512)
                    em.op("dve", lambda g, cg=cg: g.tensor_scalar(out=gsb[:], in0=PS[cg][:, :], scalar1=LIMIT, scalar2=None, op0=ALU.min), reads=[PS[cg]], writes=[gsb])
                    em.op("act", lambda g: g.activation(out=sgm[:], in_=gsb[:], func=AF.Sigmoid, scale=ALPHA), reads=[gsb], writes=[sgm])
                    em.op("dve", lambda g, cg=cg: g.tensor_scalar(out=usb[:], in0=PS[4 + cg][:, :], scalar1=LIMIT, scalar2=-LIMIT, op0=ALU.min, op1=ALU.max), reads=[PS[4 + cg]], writes=[usb])
                    em.op("dve", lambda g: g.scalar_tensor_tensor(out=usb[:], in0=usb[:], scalar=1.0, in1=gsb[:], op0=ALU.add, op1=ALU.mult), reads=[usb, gsb], writes=[usb])
                    em.op("dve", lambda g, cs_=cs_: g.tensor_tensor(out=actv[:, cs_], in0=usb[:], in1=sgm[:], op=ALU.mult), reads=[usb, sgm], writes=[actv], shared=True)
                for qq in range(4):
                    ps = nextps()
                    for q4 in range(4):
                        q = qq * 4 + q4
                        em.tr(ps[:, q4 * 128:(q4 + 1) * 128], actv[:, q:D:16], ident, [actv], [ps])
                    for q4 in range(4):
                        q = qq * 4 + q4
                        evac(actT[:, q, :].bitcast(F32R), ps[:, q4 * 128:(q4 + 1) * 128], [ps], [actT])
                for quad in range(4):
                    wt = wq[wqi % 3]; wqi += 1
                    wic = idxcol(widx, j * 4 + quad)
                    em.dma("pool", lambda g, wt=wt, j=j, quad=quad: g.indirect_dma_start(
                        out=wt[:, :], out_offset=None, in_=w2[2][:, :],
                        in_offset=bass.IndirectOffsetOnAxis(ap=wic[:, :], axis=0), bounds_check=bc_w, oob_is_err=False), reads=[wic], writes=[wt])
                    for cg in range(4):
                        ps = PS[cg]
                        for ql in range(4):
                            q = quad * 4 + ql
                            em.mm(ps[:, :], actT[:, q, :], wt[:, ql * D + cg * 512:ql * D + (cg + 1) * 512], q == 0, False, [actT, wt], [ps], r=True)
                for cg in range(4):
                    em.mm(PS[cg][:, :], ones_r[0:1, 0:128], brow3[2][0:1, cg * 512:(cg + 1) * 512], False, True, [ones_r, brow3[2]], [PS[cg]], r=True)
                    evac(ysb[:, cg * 512:(cg + 1) * 512], PS[cg][:, :], [PS[cg]], [ysb])
                em.dma("sp", lambda g, j=j: g.dma_start(out=ys_d[j * 128:(j + 1) * 128, :], in_=ysb[:]), reads=[ysb], writes=[B_ys], shared=True)

        with em.phase():
            gt2_bc = em.tile("gt2_bc", [128, D]); now_bc = em.tile("now_bc", [128, D])
            em.dma("sp", lambda g: g.dma_start(out=gt2_bc[:], in_=modrows[0, 5 * D:6 * D].partition_broadcast(128)), reads=[B_modrows], writes=[gt2_bc])
            em.dma("sp", lambda g: g.dma_start(out=now_bc[:], in_=now_d.partition_broadcast(128)), writes=[now_bc])
            yk = [em.tile("yk%d" % i, [128, D]) for i in range(4)]
            x1b = [em.tile("x1b%d" % i, [128, D]) for i in range(2)]; acc = em.tile("acc", [128, D]); junk = em.tile("junk5", [128, D]); ss = em.tile("ss5", [128, 1])
            ob5 = [em.tile("ob5_%d" % i, [128, D]) for i in range(2)]
            for t in range(NTX):
                xb = x1b[t % 2]; ob = ob5[t % 2]
                em.dma("sp", lambda g, xb=xb, t=t: g.dma_start(out=xb[:], in_=x1_d[t * 128:(t + 1) * 128, :]), reads=[B_x1], writes=[xb])
                for k in range(TOPK):
                    ic = idxcol(desti, t * TOPK + k)
                    em.dma("pool", lambda g, k=k, ic=ic: g.indirect_dma_start(out=yk[k][:, :], out_offset=None, in_=ys_d[:, :],
                                                                            in_offset=bass.IndirectOffsetOnAxis(ap=ic[:, :], axis=0),
                                                                            bounds_check=bc_blk, oob_is_err=False), reads=[B_ys, ic], writes=[yk[k]])
                em.op("dve", lambda g, t=t: g.tensor_scalar(out=acc[:], in0=yk[0][:], scalar1=gates[:, t, 0:1], scalar2=None, op0=ALU.mult), reads=[yk[0], gates], writes=[acc])
                for k in range(1, TOPK):
                    em.op("dve", lambda g, t=t, k=k: g.scalar_tensor_tensor(out=acc[:], in0=yk[k][:], scalar=gates[:, t, k:k + 1], in1=acc[:], op0=ALU.mult, op1=ALU.add),
                          reads=[yk[k], gates, acc], writes=[acc])
                em.op("dve", lambda g: g.tensor_tensor(out=acc[:], in0=acc[:], in1=gt2_bc[:], op=ALU.mult), reads=[acc, gt2_bc], writes=[acc])
                em.op("dve", lambda g, xb=xb: g.tensor_tensor(out=acc[:], in0=acc[:], in1=xb[:], op=ALU.add), reads=[acc, xb], writes=[acc])
                em.op("act", lambda g: g.activation(out=junk[:], in_=acc[:], func=AF.Square, accum_out=ss[:]), reads=[acc], writes=[junk, ss])
                rstd_from_ss(ss, 1, D)
                em.op("dve", lambda g, ob=ob: g.scalar_tensor_tensor(out=ob[:], in0=acc[:], scalar=ss[:, 0:1], in1=now_bc[:], op0=ALU.mult, op1=ALU.mult),
                      reads=[acc, ss, now_bc], writes=[ob])
                em.dma("sp", lambda g, ob=ob, t=t: g.dma_start(out=y_d[t * 128:(t + 1) * 128, :], in_=ob[:]), reads=[ob], writes=[B_y], shared=True)
        em.barrier()
        print("instructions:", em.ninstr)
    return nc


_W_KEYS = ["w_ada", "b_ada", "norm_mix_w", "w_in", "hg_lb_f", "hg_lb_b", "hg_norm_w", "gd_conv_w", "gd_a_log_f", "gd_a_log_b",
           "gd_dt_bias_f", "gd_dt_bias_b", "gd_norm_w", "w_out", "norm_ffn_w", "w_router", "b_router", "w_gate", "b_gate",
           "w_up", "b_up", "w_down", "b_down"]


def kernel(**inputs):
    f32 = lambda a: np.ascontiguousarray(np.asarray(a, dtype=np.float32))
    x = f32(inputs["x"]); c = f32(inputs["c"]); ctx = f32(inputs["ctx"])
    nb = x.shape[0]
    ne = int(np.asarray(inputs["w_router"]).shape[-1])
    shared = {"c_ctx": f32(inputs["c_ctx"]), "norm_out_w": f32(inputs["norm_out_w"])}
    for k in _W_KEYS:
        a = f32(inputs[k])[0]
        if k in ("w_gate", "w_up", "w_down"):
            a = a.reshape(ne * D, D)
        shared[k] = np.ascontiguousarray(a)
    shared["hg_lb_f"] = f32(inputs["hg_lb_f"]); shared["hg_lb_b"] = f32(inputs["hg_lb_b"])
    nc = build_program(NE=ne)
    in_maps = []
    for b in range(nb):
        m = dict(shared)
        m["x"] = x[b]; m["c"] = c[b]; m["ctx"] = ctx[b]
        in_maps.append(m)
    res = run_bass_kernel_spmd(nc, in_maps, core_ids=list(range(nb)))
    return np.stack([r["y"] for r in res.results], axis=0).astype(np.float32)
```
